# Optimizing a Trainium2 kernel written in Bass

```python
import math
import jax
import jax.numpy as jnp
from jax import lax
import numpy as np

D_MODEL = 1024
BATCH = 8
SEQ = 2048
DEPTH = 4

GRID_W = 64
CTX_LEN = 256
N_BRANCH = 3
BRANCH_W = D_MODEL

MLA_HEADS = D_MODEL // 128
QK_NOPE = 128
QK_ROPE = 64
V_DIM = 128
Q_LORA = D_MODEL // 2
KV_LORA = D_MODEL // 4
ROPE_FREQS = QK_ROPE // 4
ROPE_THETA = 10000.0
ATTN_SCALE = (QK_NOPE + QK_ROPE) ** -0.5
ATTN_Q_BLOCK = 128

SSD_HEADDIM = 64
SSD_INNER = D_MODEL
SSD_HEADS = SSD_INNER // SSD_HEADDIM
SSD_GROUPS = 4
SSD_STATE = 128
SSD_CHUNK = 128
SSD_CONV_CH = SSD_INNER + 2 * SSD_GROUPS * SSD_STATE

CONV_W = 4
CONV_PAD_L = CONV_W // 2
CONV_PAD_R = CONV_W - 1 - CONV_PAD_L

LRU_WIDTH = D_MODEL
LRU_BW = 64
LRU_BLOCKS = LRU_WIDTH // LRU_BW
LRU_C = 8.0

N_GROUPS = 4
EXPERTS_PER_GROUP = 8
N_EXPERTS = N_GROUPS * EXPERTS_PER_GROUP
TOP_K = 2
EXPERT_HIDDEN = D_MODEL // 2
EXPERT_BLOCK = 128

ALPHA = (2 * DEPTH) ** 0.25
BETA = (8 * DEPTH) ** -0.25
LN_EPS = 1e-5
RMS_EPS = 1e-6

IN_SECTIONS = (Q_LORA, KV_LORA, QK_ROPE, SSD_INNER, SSD_CONV_CH, 2 * SSD_HEADS, LRU_WIDTH, LRU_WIDTH, N_BRANCH * D_MODEL)
IN_COLS = sum(IN_SECTIONS)

kernel_name = 'hybrid_mla_ssd_rglru_hmoe_diffusion'


def layer_norm(t, gain=None, bias=None):
    tf = t.astype(jnp.float32)
    mu = jnp.mean(tf, -1, keepdims=True)
    var = jnp.mean(jnp.square(tf - mu), -1, keepdims=True)
    y = ((tf - mu) * lax.rsqrt(var + LN_EPS)).astype(t.dtype)
    if gain is None:
        return y
    return y * gain + bias


def rms_norm(t, gain):
    tf = t.astype(jnp.float32)
    y = tf * lax.rsqrt(jnp.mean(jnp.square(tf), -1, keepdims=True) + RMS_EPS)
    return y.astype(t.dtype) * gain


def modulate(t, shift, scale):
    return layer_norm(t) * (1 + scale) + shift


def split_in(p):
    out, start = [], 0
    for size in IN_SECTIONS:
        out.append(p[..., start:start + size])
        start += size
    return out


def dwconv_centred(t, w, bias):
    ch = t.shape[-1]
    y = lax.conv_general_dilated(t, w[:, None, :].astype(t.dtype), window_strides=(1,),
                                 padding=[(CONV_PAD_L, CONV_PAD_R)],
                                 dimension_numbers=('NWC', 'WIO', 'NWC'), feature_group_count=ch)
    return y + bias


def axial_rope(t, cos, sin):
    shp = t.shape
    t = t.reshape(shp[:-1] + (2, 2, ROPE_FREQS))
    t1, t2 = t[..., 0, :], t[..., 1, :]
    out = jnp.stack([t1 * cos - t2 * sin, t2 * cos + t1 * sin], axis=-2)
    return out.reshape(shp)


def mla_q(cq_raw, norm_w, w_uq, rope):
    b, l, _ = cq_raw.shape
    q = (rms_norm(cq_raw, norm_w) @ w_uq).reshape(b, l, MLA_HEADS, QK_NOPE + QK_ROPE)
    qn, qr = q[..., :QK_NOPE], q[..., QK_NOPE:]
    if rope is not None:
        qr = axial_rope(qr, rope[0][:, None], rope[1][:, None])
    return qn, qr


def mla_kv(ckv_raw, kr, norm_w, w_ukv, rope):
    b, l, _ = ckv_raw.shape
    kv = (rms_norm(ckv_raw, norm_w) @ w_ukv).reshape(b, l, MLA_HEADS, QK_NOPE + V_DIM)
    kn, v = kv[..., :QK_NOPE], kv[..., QK_NOPE:]
    if rope is not None:
        kr = axial_rope(kr, rope[0], rope[1])
    return kn, v, kr


def mla_attend(qn, qr, kn, kr, v):
    b, lq, h, _ = qn.shape
    nb = lq // ATTN_Q_BLOCK

    def block(args):
        qnb, qrb = args
        s = jnp.einsum('bqhd,bkhd->bhqk', qnb, kn) + jnp.einsum('bqhr,bkr->bhqk', qrb, kr)
        p = jax.nn.softmax(s.astype(jnp.float32) * ATTN_SCALE, axis=-1).astype(v.dtype)
        return jnp.einsum('bhqk,bkhd->bqhd', p, v)

    qn_b = qn.reshape(b, nb, ATTN_Q_BLOCK, h, QK_NOPE).swapaxes(0, 1)
    qr_b = qr.reshape(b, nb, ATTN_Q_BLOCK, h, QK_ROPE).swapaxes(0, 1)
    out = lax.map(block, (qn_b, qr_b))
    return out.swapaxes(0, 1).reshape(b, lq, h * V_DIM)


def ssd_chunked(x, dt, a_neg, bm, cm, h0, need_y):
    b, l, nh, p = x.shape
    g, n = bm.shape[2], bm.shape[3]
    e = nh // g
    nc = l // SSD_CHUNK
    xg = (x * dt[..., None]).reshape(b, nc, SSD_CHUNK, g, e, p)
    acum = jnp.cumsum((dt * a_neg).reshape(b, nc, SSD_CHUNK, g, e), axis=2)
    bc = bm.reshape(b, nc, SSD_CHUNK, g, n)
    cc = cm.reshape(b, nc, SSD_CHUNK, g, n)
    decay_to_end = jnp.exp(acum[:, :, -1:] - acum)
    states = jnp.einsum('bcsgn,bcsge,bcsgep->bcgepn', bc, decay_to_end, xg)
    chunk_decay = jnp.exp(acum[:, :, -1])

    def step(hc, inp):
        s, dcy = inp
        return dcy[..., None, None] * hc + s, hc

    h_final, h_prev = lax.scan(step, h0, (jnp.moveaxis(states, 1, 0), jnp.moveaxis(chunk_decay, 1, 0)))
    if not need_y:
        return None, h_final
    h_prev = jnp.moveaxis(h_prev, 0, 1)
    seg = acum[:, :, :, None] - acum[:, :, None, :]
    lower = jnp.tril(jnp.ones((SSD_CHUNK, SSD_CHUNK), bool))[:, :, None, None]
    lmat = jnp.exp(jnp.where(lower, seg, -jnp.inf))
    cb = jnp.einsum('bcqgn,bcsgn->bcqsg', cc, bc)
    y_diag = jnp.einsum('bcqsg,bcqsge,bcsgep->bcqgep', cb, lmat, xg)
    y_off = jnp.einsum('bcqgn,bcgepn,bcqge->bcqgep', cc, h_prev, jnp.exp(acum))
    return (y_diag + y_off).reshape(b, l, nh, p), h_final


def ssd_mixer(z, xbc, dt_raw, conv_w, conv_b, a_log, dt_bias, d_skip, norm_w, init_states, need_y):
    b, l, _ = xbc.shape
    xbc = jax.nn.silu(dwconv_centred(xbc, conv_w, conv_b))
    xs = xbc[..., :SSD_INNER].reshape(b, l, SSD_HEADS, SSD_HEADDIM)
    bm = xbc[..., SSD_INNER:SSD_INNER + SSD_GROUPS * SSD_STATE].reshape(b, l, SSD_GROUPS, SSD_STATE)
    cm = xbc[..., SSD_INNER + SSD_GROUPS * SSD_STATE:].reshape(b, l, SSD_GROUPS, SSD_STATE)
    ys, finals = [], []
    for d in range(2):
        flip = (lambda t: jnp.flip(t, axis=1)) if d == 1 else (lambda t: t)
        dt = jax.nn.softplus(dt_raw[..., d * SSD_HEADS:(d + 1) * SSD_HEADS] + dt_bias[d])
        a_neg = -jnp.exp(a_log[d])
        if init_states is None:
            h0 = jnp.zeros((b, SSD_GROUPS, SSD_HEADS // SSD_GROUPS, SSD_HEADDIM, SSD_STATE), xs.dtype)
        else:
            h0 = init_states[d]
        y, h_final = ssd_chunked(flip(xs), flip(dt), a_neg, flip(bm), flip(cm), h0, need_y)
        finals.append(h_final)
        if need_y:
            ys.append(flip(y))
    if not need_y:
        return None, finals
    y = ys[0] + ys[1] + xs * d_skip[:, None]
    y = (y.reshape(b, l, SSD_INNER) * jax.nn.silu(z)).reshape(b, l, SSD_GROUPS, SSD_INNER // SSD_GROUPS)
    y = rms_norm(y, norm_w.reshape(SSD_GROUPS, SSD_INNER // SSD_GROUPS))
    return y.reshape(b, l, SSD_INNER), finals


def _lin_combine(left, right):
    a1, b1 = left
    a2, b2 = right
    return a1 * a2, a2 * b1 + b2


def rglru_mixer(xr, gy, conv_w, conv_b, wa, ba, wx, bx, lam, init_states, need_y):
    b, l, _ = xr.shape
    xr = dwconv_centred(xr, conv_w, conv_b)
    hs, finals = [], []
    for d in range(2):
        xd = jnp.flip(xr, axis=1) if d == 1 else xr
        xblk = xd.reshape(b, l, LRU_BLOCKS, LRU_BW)
        r = jax.nn.sigmoid(jnp.einsum('blkc,kcd->blkd', xblk, wa[d]).reshape(b, l, LRU_WIDTH) + ba[d])
        i = jax.nn.sigmoid(jnp.einsum('blkc,kcd->blkd', xblk, wx[d]).reshape(b, l, LRU_WIDTH) + bx[d])
        log_a = -LRU_C * r * jax.nn.softplus(-lam[d])
        a = jnp.exp(log_a)
        bt = jnp.sqrt(-jnp.expm1(2 * log_a)) * (i * xd)
        a_cum, h = lax.associative_scan(_lin_combine, (a, bt), axis=1)
        if init_states is not None:
            h = h + a_cum * init_states[d][:, None, :]
        finals.append(h[:, -1])
        if need_y:
            hs.append(jnp.flip(h, axis=1) if d == 1 else h)
    if not need_y:
        return None, finals
    return (hs[0] + hs[1]) * jax.nn.gelu(gy), finals


def merge_branches(branches, gate_logits, w_branch, w_out):
    b, l, _ = gate_logits.shape
    stacked = jnp.stack(branches, axis=2)
    proj = jnp.einsum('blkw,kwd->blkd', stacked, w_branch)
    gates = jax.nn.sigmoid(gate_logits.reshape(b, l, N_BRANCH, D_MODEL))
    return jnp.sum(gates * proj, axis=2) @ w_out


def expert_dispatch(t, eid, ewt, w1, w3, w2):
    n, d = t.shape
    m = n * TOP_K
    flat_e = eid.reshape(m)
    order = jnp.argsort(flat_e)
    e_sorted = flat_e[order]
    tok_sorted = (order // TOP_K).astype(jnp.int32)
    wt_sorted = ewt.reshape(m)[order]
    counts = jnp.zeros((N_EXPERTS,), jnp.int32).at[flat_e].add(1)
    padded = (counts + EXPERT_BLOCK - 1) // EXPERT_BLOCK * EXPERT_BLOCK
    start = jnp.cumsum(counts) - counts
    pend = jnp.cumsum(padded)
    pstart = pend - padded
    dest = pstart[e_sorted] + jnp.arange(m, dtype=jnp.int32) - start[e_sorted]
    n_blocks = -(-m // EXPERT_BLOCK) + N_EXPERTS
    row_tok = jnp.full((n_blocks * EXPERT_BLOCK,), n, jnp.int32).at[dest].set(tok_sorted)
    t_pad = jnp.concatenate([t, jnp.zeros((1, d), t.dtype)], axis=0)
    xin = t_pad[row_tok].reshape(n_blocks, EXPERT_BLOCK, d)
    blk_e = jnp.minimum(jnp.searchsorted(pend, jnp.arange(n_blocks, dtype=jnp.int32) * EXPERT_BLOCK, side='right'), N_EXPERTS - 1)

    def run(args):
        xb, e = args
        hid = jax.nn.silu(xb @ w1[e]) * (xb @ w3[e])
        return hid @ w2[e]

    yout = lax.map(run, (xin, blk_e)).reshape(n_blocks * EXPERT_BLOCK, d)
    contrib = yout[dest] * wt_sorted[:, None].astype(yout.dtype)
    return jax.ops.segment_sum(contrib, tok_sorted, num_segments=n)


def hier_moe(t, wg, bg, we, be, w1, w3, w2):
    n = t.shape[0]
    glog = (t @ wg + bg).astype(jnp.float32)
    gprob = jax.nn.softmax(glog, axis=-1)
    _, gsel = lax.top_k(glog, 1)
    gval = jnp.take_along_axis(gprob, gsel, axis=-1)
    elog = (t @ we + be).astype(jnp.float32).reshape(n, N_GROUPS, EXPERTS_PER_GROUP)
    elog_g = jnp.take_along_axis(elog, gsel[:, :, None], axis=1)[:, 0]
    top_v, top_i = lax.top_k(elog_g, TOP_K)
    ewt = jax.nn.softmax(top_v, axis=-1) * gval
    eid = gsel * EXPERTS_PER_GROUP + top_i
    return expert_dispatch(t, eid, ewt, w1, w3, w2)


def setup_inputs(seed: int = 0) -> dict:
    key = jax.random.key(seed)
    ks = iter(jax.random.split(key, 48))
    f32 = jnp.float32

    def nrm(shape, scale):
        return jax.random.normal(next(ks), shape, f32) * scale

    def gain(shape):
        return 1.0 + nrm(shape, 0.01)

    L = DEPTH
    inp = {}
    inp['x'] = nrm((BATCH, SEQ, D_MODEL), 1.0)
    inp['c'] = nrm((BATCH, D_MODEL), 1.0)
    inp['ctx'] = nrm((BATCH, CTX_LEN, D_MODEL), 1.0)
    inp['c_ctx'] = nrm((D_MODEL,), 1.0)
    inp['w_mod'] = nrm((L, D_MODEL, 6 * D_MODEL), 0.5 * D_MODEL ** -0.5)
    inp['b_mod'] = nrm((L, 6 * D_MODEL), 0.01)
    inp['w_in'] = nrm((L, D_MODEL, IN_COLS), D_MODEL ** -0.5)
    inp['q_norm_w'] = gain((L, Q_LORA))
    inp['kv_norm_w'] = gain((L, KV_LORA))
    inp['w_uq'] = nrm((L, Q_LORA, MLA_HEADS * (QK_NOPE + QK_ROPE)), Q_LORA ** -0.5)
    inp['w_ukv'] = nrm((L, KV_LORA, MLA_HEADS * (QK_NOPE + V_DIM)), KV_LORA ** -0.5)
    inp['ssd_conv_w'] = nrm((L, CONV_W, SSD_CONV_CH), CONV_W ** -0.5)
    inp['ssd_conv_b'] = nrm((L, SSD_CONV_CH), 0.01)
    inp['ssd_a_log'] = jnp.log(jax.random.uniform(next(ks), (L, 2, SSD_HEADS), f32, 1.0, 16.0))
    dt0 = jnp.exp(jax.random.uniform(next(ks), (L, 2, SSD_HEADS), f32, math.log(1e-3), math.log(1e-1)))
    inp['ssd_dt_bias'] = dt0 + jnp.log(-jnp.expm1(-dt0))
    inp['ssd_d'] = gain((L, SSD_HEADS))
    inp['ssd_norm_w'] = gain((L, SSD_INNER))
    inp['lru_conv_w'] = nrm((L, CONV_W, LRU_WIDTH), CONV_W ** -0.5)
    inp['lru_conv_b'] = nrm((L, LRU_WIDTH), 0.01)
    inp['lru_wa'] = nrm((L, 2, LRU_BLOCKS, LRU_BW, LRU_BW), LRU_BW ** -0.5)
    inp['lru_ba'] = nrm((L, 2, LRU_WIDTH), 0.01)
    inp['lru_wx'] = nrm((L, 2, LRU_BLOCKS, LRU_BW, LRU_BW), LRU_BW ** -0.5)
    inp['lru_bx'] = nrm((L, 2, LRU_WIDTH), 0.01)
    a0 = jax.random.uniform(next(ks), (L, 2, LRU_WIDTH), f32, 0.9, 0.999)
    s0 = a0 ** (1.0 / LRU_C)
    inp['lru_lambda'] = jnp.log(s0) - jnp.log1p(-s0)
    inp['w_branch'] = nrm((L, N_BRANCH, BRANCH_W, D_MODEL), BRANCH_W ** -0.5)
    inp['w_out'] = nrm((L, D_MODEL, D_MODEL), BETA * D_MODEL ** -0.5)
    inp['ln1_g'] = gain((L, D_MODEL))
    inp['ln1_b'] = nrm((L, D_MODEL), 0.01)
    inp['ln2_g'] = gain((L, D_MODEL))
    inp['ln2_b'] = nrm((L, D_MODEL), 0.01)
    inp['router_wg'] = nrm((L, D_MODEL, N_GROUPS), D_MODEL ** -0.5)
    inp['router_bg'] = nrm((L, N_GROUPS), 0.01)
    inp['router_we'] = nrm((L, D_MODEL, N_EXPERTS), D_MODEL ** -0.5)
    inp['router_be'] = nrm((L, N_EXPERTS), 0.01)
    inp['exp_w1'] = nrm((L, N_EXPERTS, D_MODEL, EXPERT_HIDDEN), D_MODEL ** -0.5)
    inp['exp_w3'] = nrm((L, N_EXPERTS, D_MODEL, EXPERT_HIDDEN), D_MODEL ** -0.5)
    inp['exp_w2'] = nrm((L, N_EXPERTS, EXPERT_HIDDEN, D_MODEL), BETA * EXPERT_HIDDEN ** -0.5)
    return inp


def reference(x, c, ctx, c_ctx, w_mod, b_mod, w_in, q_norm_w, kv_norm_w, w_uq, w_ukv,
              ssd_conv_w, ssd_conv_b, ssd_a_log, ssd_dt_bias, ssd_d, ssd_norm_w,
              lru_conv_w, lru_conv_b, lru_wa, lru_ba, lru_wx, lru_bx, lru_lambda,
              w_branch, w_out, ln1_g, ln1_b, ln2_g, ln2_b,
              router_wg, router_bg, router_we, router_be, exp_w1, exp_w3, exp_w2):
    bsz, seq, _ = x.shape
    n_ctx = ctx.shape[1]
    rows = seq // GRID_W
    row_pos = jnp.repeat(jnp.arange(rows, dtype=jnp.float32), GRID_W)
    col_pos = (jnp.arange(rows * GRID_W) % GRID_W).astype(jnp.float32)
    inv_freq = ROPE_THETA ** (-jnp.arange(ROPE_FREQS, dtype=jnp.float32) / ROPE_FREQS)
    ang = jnp.stack([row_pos[:, None] * inv_freq, col_pos[:, None] * inv_freq], axis=1)
    rope = (jnp.cos(ang).astype(x.dtype), jnp.sin(ang).astype(x.dtype))
    silu_c = jax.nn.silu(c)
    silu_cc = jax.nn.silu(c_ctx)
    h, hc = x, ctx
    for layer in range(DEPTH):
        last = layer == DEPTH - 1
        sh1, sc1, g1, sh2, sc2, g2 = [m[:, None, :] for m in jnp.split(silu_c @ w_mod[layer] + b_mod[layer], 6, axis=-1)]
        sh1c, sc1c, g1c, sh2c, sc2c, g2c = jnp.split(silu_cc @ w_mod[layer] + b_mod[layer], 6, axis=-1)
        cq_l, ckv_l, kr_l, z_l, xbc_l, dt_l, lx_l, lg_l, gate_l = split_in(modulate(h, sh1, sc1) @ w_in[layer])
        cq_c, ckv_c, kr_c, z_c, xbc_c, dt_c, lx_c, lg_c, gate_c = split_in(modulate(hc, sh1c, sc1c) @ w_in[layer])
        kn_c, v_c, kr_c = mla_kv(ckv_c, kr_c, kv_norm_w[layer], w_ukv[layer], None)
        kn_l, v_l, kr_l = mla_kv(ckv_l, kr_l, kv_norm_w[layer], w_ukv[layer], rope)
        qn_l, qr_l = mla_q(cq_l, q_norm_w[layer], w_uq[layer], rope)
        att_l = mla_attend(qn_l, qr_l, jnp.concatenate([kn_c, kn_l], axis=1),
                           jnp.concatenate([kr_c, kr_l], axis=1), jnp.concatenate([v_c, v_l], axis=1))
        ssd_p = (ssd_conv_w[layer], ssd_conv_b[layer], ssd_a_log[layer], ssd_dt_bias[layer], ssd_d[layer], ssd_norm_w[layer])
        y_ssd_c, st_ssd = ssd_mixer(z_c, xbc_c, dt_c, *ssd_p, None, not last)
        y_ssd_l, _ = ssd_mixer(z_l, xbc_l, dt_l, *ssd_p, st_ssd, True)
        lru_p = (lru_conv_w[layer], lru_conv_b[layer], lru_wa[layer], lru_ba[layer], lru_wx[layer], lru_bx[layer], lru_lambda[layer])
        y_lru_c, st_lru = rglru_mixer(lx_c, lg_c, *lru_p, None, not last)
        y_lru_l, _ = rglru_mixer(lx_l, lg_l, *lru_p, st_lru, True)
        mix_l = merge_branches((att_l, y_ssd_l, y_lru_l), gate_l, w_branch[layer], w_out[layer])
        h = layer_norm(ALPHA * h + g1 * mix_l, ln1_g[layer], ln1_b[layer])
        if not last:
            qn_c, qr_c = mla_q(cq_c, q_norm_w[layer], w_uq[layer], None)
            att_c = mla_attend(qn_c, qr_c, kn_c, kr_c, v_c)
            mix_c = merge_branches((att_c, y_ssd_c, y_lru_c), gate_c, w_branch[layer], w_out[layer])
            hc = layer_norm(ALPHA * hc + g1c * mix_c, ln1_g[layer], ln1_b[layer])
        moe_p = (router_wg[layer], router_bg[layer], router_we[layer], router_be[layer], exp_w1[layer], exp_w3[layer], exp_w2[layer])
        u2 = modulate(h, sh2, sc2).reshape(bsz * seq, D_MODEL)
        if last:
            f_l = hier_moe(u2, *moe_p).reshape(bsz, seq, D_MODEL)
        else:
            u2c = modulate(hc, sh2c, sc2c).reshape(bsz * n_ctx, D_MODEL)
            f = hier_moe(jnp.concatenate([u2c, u2], axis=0), *moe_p)
            f_c = f[:bsz * n_ctx].reshape(bsz, n_ctx, D_MODEL)
            f_l = f[bsz * n_ctx:].reshape(bsz, seq, D_MODEL)
            hc = layer_norm(ALPHA * hc + g2c * f_c, ln2_g[layer], ln2_b[layer])
        h = layer_norm(ALPHA * h + g2 * f_l, ln2_g[layer], ln2_b[layer])
    return h
```

```python
import math
from contextlib import ExitStack
import numpy as np
import concourse.bass as bass
import concourse.mybir as mybir
from concourse.bass_utils import run_bass_kernel_spmd

F32 = mybir.dt.float32
BF16 = mybir.dt.bfloat16
AF = mybir.ActivationFunctionType
ALU = mybir.AluOpType
AX = mybir.AxisListType.X

D = 1024
T = 2304
NT = 18
L = 4
TB = [(0, 512), (512, 1024), (1024, 1536), (1536, 2048), (2048, 2304)]
SEQS = [(0, 256), (256, 2304)]
ALPHA = 8.0 ** 0.25
LN_EPS = 1e-5
RMS_EPS = 1e-6
ATTN_SCALE = 192.0 ** -0.5
C_CQ, C_CKV, C_KR, C_Z, C_XBC, C_DT, C_LX, C_LG, C_GATE = 0, 512, 768, 832, 1856, 3904, 3936, 4960, 5984
NCOL = 9056
V_BMOD, V_QNW, V_KVNW, V_SCW, V_SCB, V_LCW, V_LCB, V_LBA, V_LBX, V_LLAM, V_SNW = 0, 96, 100, 102, 166, 182, 214, 222, 238, 254, 270
NV = 278
R_LN1G, R_LN1B, R_LN2G, R_LN2B, R_ALOG, R_DTB, R_DSK, R_RB = 0, 1024, 2048, 3072, 4096, 4128, 4160, 4176
NR = 4212


class Tile:
    __slots__ = ("t", "name", "last_w", "reads", "dsem", "dcount")

    def __init__(self, t, name):
        self.t = t
        self.name = name
        self.last_w = None
        self.reads = []
        self.dsem = None
        self.dcount = 0

    def __getitem__(self, k):
        return self.t[k]


class MK:
    def __init__(self, nc):
        self.nc = nc
        self.E = {"pe": nc.tensor, "act": nc.scalar, "dve": nc.vector, "pool": nc.gpsimd, "sp": nc.sync}
        self.sem = {k: nc.alloc_semaphore("s_" + k) for k in self.E}
        self.cnt = {k: 0 for k in self.E}
        self.waited = {}
        self.sem_pool = []
        self.stage_tiles = []
        self.dma_active = []
        self.uid = 0
        self.n_inst = 0
        self.n_wait = 0

    def tile(self, es, name, shape, dt):
        self.uid += 1
        t = es.enter_context(self.nc.sbuf_tensor("%s_%d" % (name, self.uid), list(shape), dt))
        tl = Tile(t, name)
        self.stage_tiles.append(tl)
        return tl

    def tracker(self, name):
        return Tile(None, name)

    def _wait(self, eng, tok):
        if tok is None:
            return
        sem, val, src = tok
        if src == eng and eng == "pe":
            return
        key = (eng, id(sem))
        if self.waited.get(key, 0) >= val:
            return
        self.waited[key] = val
        self.E[eng].wait_ge(sem, val)
        self.n_wait += 1

    def _deps(self, eng, reads, writes):
        for b in reads:
            self._wait(eng, b.last_w)
        for b in writes:
            self._wait(eng, b.last_w)
            for r in b.reads:
                self._wait(eng, r)

    def op(self, eng, fn, reads=(), writes=()):
        self._deps(eng, reads, writes)
        inst = fn(self.E[eng])
        self.cnt[eng] += 1
        inst.then_inc(self.sem[eng], 1)
        tok = (self.sem[eng], self.cnt[eng], eng)
        for b in reads:
            b.reads.append(tok)
        for b in writes:
            b.last_w = tok
            b.reads = []
        self.n_inst += 1
        return inst

    def dma(self, q, out, in_, sbuf, load=True):
        if sbuf.dsem is None:
            if self.sem_pool:
                sbuf.dsem, sbuf.dcount = self.sem_pool.pop()
            else:
                self.uid += 1
                sbuf.dsem, sbuf.dcount = self.nc.alloc_semaphore("d%d" % self.uid), 0
            self.dma_active.append(sbuf)
        if load:
            self._deps(q, [], [sbuf])
        else:
            self._deps(q, [sbuf], [])
        inst = self.E[q].dma_start(out=out, in_=in_)
        sbuf.dcount += 16
        inst.then_inc(sbuf.dsem, 16)
        tok = (sbuf.dsem, sbuf.dcount, "dma")
        if load:
            sbuf.last_w = tok
            sbuf.reads = []
        else:
            sbuf.reads.append(tok)
        self.n_inst += 1
        return inst

    def barrier(self):
        for b in self.dma_active:
            self._wait("sp", (b.dsem, b.dcount, "dma"))
        for k in self.E:
            if k != "sp" and self.cnt[k]:
                self._wait("sp", (self.sem[k], self.cnt[k], k))
        inst = self.E["sp"].nop()
        self.cnt["sp"] += 1
        inst.then_inc(self.sem["sp"], 1)
        tok = (self.sem["sp"], self.cnt["sp"], "sp")
        for k in self.E:
            if k != "sp":
                self._wait(k, tok)

    def end_stage(self, es):
        self.barrier()
        keep = []
        for tl in self.dma_active:
            if tl in self.stage_tiles:
                self.sem_pool.append((tl.dsem, tl.dcount))
                tl.dsem = None
            else:
                keep.append(tl)
        self.dma_active = keep
        self.stage_tiles = []
        es.close()


def build(n_layers=L, debug=False, stop_after=None):
    nc = bass.Bass("TRN2", target_bir_lowering=False)
    mk = MK(nc)

    def dram(name, shape, dt=F32, kind="ExternalInput"):
        return nc.dram_tensor(name, list(shape), dt, kind=kind).ap()

    skind = "ExternalOutput" if debug else "Internal"
    h0 = dram("h0", [T, D])
    cvec = dram("cvec", [128, 16])
    w_mod = dram("w_mod", [L, D, 6144])
    w_in = dram("w_in", [L, D, NCOL])
    w_krsw = dram("w_krsw", [L, D, 64])
    w_uqx = dram("w_uqx", [L, 512, 2048])
    w_ukv = dram("w_ukv", [L, 256, 2048])
    lru_bd = dram("lru_bd", [L, 32, 128, 128])
    w_branch = dram("w_branch", [L, 3, D, D])
    w_out = dram("w_out", [L, D, D])
    wr = dram("wr", [L, D, 36])
    exp_w1 = dram("exp_w1", [L, 32, D, 512])
    exp_w3 = dram("exp_w3", [L, 32, D, 512])
    exp_w2 = dram("exp_w2", [L, 32, 512, D])
    vecs_d = dram("vecs", [L, 128, NV])
    rows_d = dram("rows", [L, NR])
    consts_d = dram("consts", [4, 128, 128])
    cs_d = dram("cossin", [2, 64, T])
    y_out = dram("y", [2048, D], F32, "ExternalOutput")
    hd = dram("hd", [T, D], F32, skind)
    cqT = dram("cqT", [512, T], BF16, skind)
    ckvT = dram("ckvT", [256, T], BF16, skind)
    krT = dram("krT", [64, T], BF16, skind)
    bcT = dram("bcT", [1024, T], BF16, skind)
    xs_tm = dram("xs_tm", [T, D], BF16, skind)
    b_tm = dram("b_tm", [T, 512], BF16, skind)
    zs = dram("zs", [T, D], BF16, skind)
    dtr = dram("dtr", [T, 32], F32, skind)
    gateT = dram("gateT", [3072, T], BF16, skind)
    attT = dram("attT", [D, T], BF16, skind)
    yssdT = dram("yssdT", [D, T], BF16, skind)
    ylruT = dram("ylruT", [D, T], BF16, skind)
    hprev = dram("hprev", [2, NT, 128, D], BF16, skind)

    ges = ExitStack()
    PSt = ges.enter_context(nc.psum_tensor("psall", [128, 4096], F32))
    PS = PSt
    PB = [mk.tracker("pb%d" % i) for i in range(8)]

    def bank(i, n=512, off=0):
        return PS[:, i * 512 + off:i * 512 + off + n]

    def bankb(i):
        return PS[:, i * 512:(i + 1) * 512].bitcast(BF16)

    cst = mk.tile(ges, "cst", [128, 4, 128], F32)
    identf = cst[:, 0, :]
    triU = cst[:, 1, :]
    triL = cst[:, 2, :]
    onesf = cst[:, 3, :]
    cstb = mk.tile(ges, "cstb", [128, 2, 128], BF16)
    identb = cstb[:, 0, :]
    onesb = cstb[:, 1, :]
    vec = mk.tile(ges, "vec", [128, NV], F32)
    srow = mk.tile(ges, "srow", [128, NR - R_ALOG], F32)
    modT = mk.tile(ges, "modT", [128, 96], F32)
    scb = mk.tile(ges, "scb", [128, 16], BF16)
    cvt = mk.tile(ges, "cvt", [128, 16], F32)
    lcs = mk.tile(ges, "lcs", [128, 16], F32)
    qnws = mk.tile(ges, "qnws", [128, 6], F32)

    mk.dma("sp", cst[:], consts_d.rearrange("c p n -> p c n"), cst)
    mk.op("dve", lambda e: e.tensor_copy(out=cstb[:, 0, :], in_=cst[:, 0, :]), [cst], [cstb])
    mk.op("dve", lambda e: e.tensor_copy(out=cstb[:, 1, :], in_=cst[:, 3, :]), [cst], [cstb])
    mk.dma("sp", cvt[:], cvec, cvt)
    mk.op("act", lambda e: e.activation(out=scb[:], in_=cvt[:], func=AF.Silu), [cvt], [scb])

    def SROW(off, n):
        return srow[:, off - R_ALOG:off - R_ALOG + n]

    def modcol(sec, c, j):
        col = (sec * 8 + c) * 2 + j
        return modT[:, col:col + 1]

    rr = {"mm": 0, "ev": 0}

    def evac_eng():
        rr["ev"] += 1
        return "act" if rr["ev"] % 2 else "dve"

    def copy(eng, out, in_, r, w):
        if eng == "act":
            mk.op("act", lambda e: e.activation(out=out, in_=in_, func=AF.Identity), r, w)
        else:
            mk.op(eng, lambda e: e.tensor_copy(out=out, in_=in_), r, w)

    def stage_mod(l):
        es = ExitStack()
        mk.dma("sp", vec[:], vecs_d[l], vec)
        mk.dma("sp", srow[:], rows_d[l, R_ALOG:NR].partition_broadcast(128), srow)
        wmb = [mk.tile(es, "wmb", [128, 8, 512], BF16) for _ in range(2)]
        wv = w_mod[l].rearrange("(k p) n -> p k n", p=128)
        for cg in range(12):
            wb = wmb[cg % 2]
            mk.dma("pool", wb[:], wv[:, :, cg * 512:(cg + 1) * 512], wb)
            for f in range(4):
                fc = cg * 4 + f
                for k in range(8):
                    mk.op("pe", lambda e: e.matmul(bank(0, 2, fc * 2), lhsT=wb[:, k, f * 128:(f + 1) * 128],
                                                   rhs=scb[:, 2 * k:2 * k + 2], start=(k == 0), stop=(k == 7)),
                          [wb, scb], [PB[0]])
        mk.op("dve", lambda e: e.tensor_tensor(out=modT[:], in0=bank(0, 96), in1=vec[:, V_BMOD:V_BMOD + 96], op=ALU.add),
              [PB[0], vec], [modT])
        for sec in (1, 4):
            mk.op("dve", lambda e: e.tensor_scalar_add(out=modT[:, sec * 16:(sec + 1) * 16], in0=modT[:, sec * 16:(sec + 1) * 16],
                                                       scalar1=1.0), [modT], [modT])
        mk.op("act", lambda e: e.activation(out=lcs[:], in_=vec[:, V_LLAM:V_LLAM + 16], func=AF.Exp, scale=-1.0), [vec], [lcs])
        mk.op("act", lambda e: e.activation(out=lcs[:], in_=lcs[:], func=AF.Ln, bias=1.0, scale=1.0), [lcs], [lcs])
        mk.op("dve", lambda e: e.tensor_scalar_mul(out=lcs[:], in0=lcs[:], scalar1=-8.0), [lcs], [lcs])
        mk.op("dve", lambda e: e.tensor_scalar_mul(out=qnws[:, 0:4], in0=vec[:, V_QNW:V_QNW + 4], scalar1=math.sqrt(512.0)), [vec], [qnws])
        mk.op("dve", lambda e: e.tensor_scalar_mul(out=qnws[:, 4:6], in0=vec[:, V_KVNW:V_KVNW + 2], scalar1=math.sqrt(256.0)), [vec], [qnws])
        mk.end_stage(es)

    def build_grow(es, sec):
        grow = mk.tile(es, "grow", [128, 2, D], F32)
        dg = [mk.tile(es, "dg", [128, 128], F32) for _ in range(2)]
        n = 0
        for j in range(2):
            for c in range(8):
                d_ = dg[n % 2]
                pb = 6 + (n % 2)
                mk.op("dve", lambda e: e.tensor_scalar_mul(out=d_[:], in0=identf, scalar1=modcol(sec, c, j)), [cst, modT], [d_])
                mk.op("pe", lambda e: e.matmul(bank(pb, 128), lhsT=onesf, rhs=d_[:], start=True, stop=True), [cst, d_], [PB[pb]])
                copy("act", grow[:, j, c * 128:(c + 1) * 128], bank(pb, 128), [PB[pb]], [grow])
                n += 1
        return grow

    def layer_norm_stats(x_ap, xt, stt, mvt):
        mk.op("dve", lambda e: e.bn_stats(out=stt[:, 0:6], in_=x_ap[:, 0:512]), [xt], [stt])
        mk.op("dve", lambda e: e.bn_stats(out=stt[:, 6:12], in_=x_ap[:, 512:1024]), [xt], [stt])
        mk.op("dve", lambda e: e.bn_aggr(out=mvt[:, 2:4], in_=stt[:]), [stt], [mvt])
        mk.op("act", lambda e: e.activation(out=mvt[:, 0:1], in_=mvt[:, 3:4], func=AF.Sqrt, bias=LN_EPS, scale=1.0), [mvt], [mvt])
        mk.op("dve", lambda e: e.reciprocal(out=mvt[:, 0:1], in_=mvt[:, 0:1]), [mvt], [mvt])
        mk.op("dve", lambda e: e.tensor_scalar(out=mvt[:, 1:2], in0=mvt[:, 2:3], scalar1=mvt[:, 0:1], scalar2=-1.0,
                                               op0=ALU.mult, op1=ALU.mult), [mvt], [mvt])

    def stage_ln_mod(es, src, sec_shift, sec_scale, uT, t0, router=None):
        hts = [mk.tile(es, "ht", [128, D], F32) for _ in range(2)]
        xns = [mk.tile(es, "xn", [128, D], F32) for _ in range(2)]
        ufs = [mk.tile(es, "uf", [128, 8, 128], F32) for _ in range(2)]
        sts = [mk.tile(es, "st", [128, 12], F32) for _ in range(2)]
        mvs = [mk.tile(es, "mv", [128, 4], F32) for _ in range(2)]
        if router is not None:
            wrt, gw = router
            rt = [mk.tile(es, "rt", [128, 160], F32) for _ in range(2)]
        tiles = list(range(t0, NT))
        mk.dma("sp", hts[0][:], src[tiles[0] * 128:(tiles[0] + 1) * 128, :], hts[0])
        for i, t in enumerate(tiles):
            ht, xn, uf, st_, mv = hts[i % 2], xns[i % 2], ufs[i % 2], sts[i % 2], mvs[i % 2]
            if i + 1 < len(tiles):
                tn = tiles[i + 1]
                mk.dma("sp", hts[(i + 1) % 2][:], src[tn * 128:(tn + 1) * 128, :], hts[(i + 1) % 2])
            j = 1 if t < 2 else 0
            layer_norm_stats(ht, ht, st_, mv)
            mk.op("act", lambda e: e.activation(out=xn[:], in_=ht[:], func=AF.Identity, scale=mv[:, 0:1], bias=mv[:, 1:2]),
                  [ht, mv], [xn])
            for half in range(2):
                pb = 4 + (rr["mm"] % 2)
                rr["mm"] += 1
                for c4 in range(4):
                    c = half * 4 + c4
                    mk.op("pe", lambda e: e.transpose(out=bank(pb, 128, c4 * 128), in_=xn[:, c * 128:(c + 1) * 128], identity=identf),
                          [xn, cst], [PB[pb]])
                for c4 in range(4):
                    c = half * 4 + c4
                    mk.op("act", lambda e: e.activation(out=uf[:, c, :], in_=bank(pb, 128, c4 * 128), func=AF.Identity,
                                                        scale=modcol(sec_scale, c, j), bias=modcol(sec_shift, c, j)),
                          [PB[pb], modT], [uf])
            mk.op("dve", lambda e: e.tensor_copy(out=uT[:, :, t * 128:(t + 1) * 128], in_=uf[:]), [uf], [uT])
            if router is not None:
                r_ = rt[i % 2]
                for k in range(8):
                    mk.op("pe", lambda e: e.matmul(bank(6, 36), lhsT=uf[:, k, :], rhs=wrt[:, k, :], start=(k == 0), stop=(k == 7)),
                          [uf, wrt], [PB[6]])
                lg = r_[:, 0:36]
                mk.op("dve", lambda e: e.tensor_tensor(out=lg, in0=bank(6, 36), in1=SROW(R_RB, 36), op=ALU.add), [PB[6], srow], [r_])
                gm = r_[:, 40:41]
                mk.op("dve", lambda e: e.reduce_max(out=gm, in_=r_[:, 0:4], axis=AX), [r_], [r_])
                ngm = r_[:, 41:42]
                mk.op("dve", lambda e: e.tensor_scalar_mul(out=ngm, in0=gm, scalar1=-1.0), [r_], [r_])
                mk.op("dve", lambda e: e.memset(r_[:, 42:43], 0.0), [], [r_])
                mk.op("act", lambda e: e.activation(out=r_[:, 44:48], in_=r_[:, 0:4], func=AF.Exp, bias=ngm, scale=1.0,
                                                    accum_out=r_[:, 42:43]), [r_], [r_])
                gval = r_[:, 43:44]
                mk.op("dve", lambda e: e.reciprocal(out=gval, in_=r_[:, 42:43]), [r_], [r_])
                gmask = r_[:, 48:52]
                mk.op("dve", lambda e: e.tensor_scalar(out=gmask, in0=r_[:, 0:4], scalar1=gm, scalar2=None, op0=ALU.is_ge), [r_], [r_])
                pen = r_[:, 52:56]
                mk.op("dve", lambda e: e.tensor_scalar(out=pen, in0=gmask, scalar1=-1.0, scalar2=1e30, op0=ALU.add, op1=ALU.mult), [r_], [r_])
                ml = r_[:, 56:88]
                mk.op("dve", lambda e: e.tensor_tensor(out=ml.rearrange("p (g x) -> p g x", x=8),
                                                       in0=r_[:, 4:36].rearrange("p (g x) -> p g x", x=8),
                                                       in1=pen.unsqueeze(2).to_broadcast([128, 4, 8]), op=ALU.add), [r_], [r_])
                m1 = r_[:, 88:89]
                mk.op("dve", lambda e: e.reduce_max(out=m1, in_=ml, axis=AX), [r_], [r_])
                oh1 = r_[:, 96:128]
                mk.op("dve", lambda e: e.tensor_scalar(out=oh1, in0=ml, scalar1=m1, scalar2=None, op0=ALU.is_ge), [r_], [r_])
                ml2 = r_[:, 128:160]
                mk.op("dve", lambda e: e.scalar_tensor_tensor(out=ml2, in0=oh1, scalar=-1e30, in1=ml, op0=ALU.mult, op1=ALU.add), [r_], [r_])
                m2 = r_[:, 89:90]
                mk.op("dve", lambda e: e.reduce_max(out=m2, in_=ml2, axis=AX), [r_], [r_])
                dd = r_[:, 90:91]
                mk.op("dve", lambda e: e.tensor_tensor(out=dd, in0=m2, in1=m1, op=ALU.subtract), [r_], [r_])
                mk.op("act", lambda e: e.activation(out=dd, in_=dd, func=AF.Exp), [r_], [r_])
                mk.op("dve", lambda e: e.tensor_scalar_add(out=dd, in0=dd, scalar1=1.0), [r_], [r_])
                mk.op("dve", lambda e: e.reciprocal(out=dd, in_=dd), [r_], [r_])
                w1_ = r_[:, 91:92]
                mk.op("dve", lambda e: e.tensor_tensor(out=w1_, in0=dd, in1=gval, op=ALU.mult), [r_], [r_])
                w2_ = r_[:, 92:93]
                mk.op("dve", lambda e: e.tensor_tensor(out=w2_, in0=gval, in1=w1_, op=ALU.subtract), [r_], [r_])
                mk.op("dve", lambda e: e.tensor_scalar(out=ml, in0=ml2, scalar1=m2, scalar2=w2_, op0=ALU.is_ge, op1=ALU.mult), [r_], [r_])
                mk.op("dve", lambda e: e.scalar_tensor_tensor(out=gw[:, t, :], in0=oh1, scalar=w1_, in1=ml, op0=ALU.mult, op1=ALU.add),
                      [r_], [gw])

    def res_ln(t, mix_ap, mix_reads, h_src, grow, lnrow, dst_ap_fn, tmps):
        ht, vt, st_, mv = tmps
        j = 1 if t < 2 else 0
        mk.op("dve", lambda e: e.tensor_tensor(out=vt[:], in0=mix_ap, in1=grow[:, j, :], op=ALU.mult), list(mix_reads) + [grow], [vt])
        mk.op("dve", lambda e: e.scalar_tensor_tensor(out=vt[:], in0=ht[:], scalar=ALPHA, in1=vt[:], op0=ALU.mult, op1=ALU.add),
              [ht, vt], [vt])
        layer_norm_stats(vt, vt, st_, mv)
        mk.op("act", lambda e: e.activation(out=vt[:], in_=vt[:], func=AF.Identity, scale=mv[:, 0:1], bias=mv[:, 1:2]), [vt, mv], [vt])
        mk.op("pool", lambda e: e.tensor_tensor(out=vt[:], in0=vt[:], in1=lnrow[:, 0, :], op=ALU.mult), [vt, lnrow], [vt])
        mk.op("pool", lambda e: e.tensor_tensor(out=vt[:], in0=vt[:], in1=lnrow[:, 1, :], op=ALU.add), [vt, lnrow], [vt])
        mk.dma("sp", dst_ap_fn(t), vt[:], vt, load=False)

    def stage_inproj(l, src):
        es = ExitStack()
        uT = mk.tile(es, "uT", [128, 8, T], BF16)
        es1 = ExitStack()
        stage_ln_mod(es1, src, 0, 1, uT, 0)
        mk.end_stage(es1)
        wv = w_in[l].rearrange("(k p) n -> p k n", p=128)
        wgs = [mk.tile(es, "wg", [128, 8, 512], BF16) for _ in range(2)]
        keep = [uT] + wgs

        def sub_end(esx):
            mk.end_stage(esx)
            mk.stage_tiles.extend(keep)
            return ExitStack()

        mk.stage_tiles.extend(keep)
        es_outer = es
        es = ExitStack()
        wgi = [0]

        def load_w(col0, n, src_v=None):
            wg = wgs[wgi[0] % 2]
            wgi[0] += 1
            v = wv if src_v is None else src_v
            mk.dma("pool", wg[:, :, 0:n], v[:, :, col0:col0 + n], wg)
            return wg

        def proj_fm(wg, c0, M, a, b, pb):
            for k in range(8):
                mk.op("pe", lambda e: e.matmul(PS[0:M, pb * 512:pb * 512 + (b - a)], lhsT=wg[:, k, c0:c0 + M], rhs=uT[:, k, a:b],
                                               start=(k == 0), stop=(k == 7)), [wg, uT], [PB[pb]])

        def next_pb():
            rr["mm"] += 1
            return rr["mm"] % 4

        raws = [mk.tile(es, "raw", [128, 4, 512], BF16) for _ in range(2)]
        sqs = [mk.tile(es, "sq", [128, 512], BF16) for _ in range(2)]
        Rt = [mk.tile(es, "Rt", [128, 512], F32) for _ in range(2)]
        outb = [mk.tile(es, "outb", [128, 4, 512], BF16) for _ in range(2)]
        for (col0, nch, dim, nwoff, dst) in ((C_CQ, 4, 512, 0, cqT), (C_CKV, 2, 256, 4, ckvT)):
            wg = load_w(col0, nch * 128)
            for bi, (a, b) in enumerate(TB):
                n = b - a
                raw, R_, ob = raws[bi % 2], Rt[bi % 2], outb[bi % 2]
                for c in range(nch):
                    pb = next_pb()
                    proj_fm(wg, c * 128, 128, a, b, pb)
                    copy("act", raw[:, c, 0:n], bank(pb, n), [PB[pb]], [raw])
                    sq = sqs[c % 2]
                    mk.op("dve", lambda e: e.tensor_tensor(out=sq[:, 0:n], in0=bank(pb, n), in1=raw[:, c, 0:n], op=ALU.mult), [PB[pb], raw], [sq])
                    mk.op("pe", lambda e: e.matmul(bank(4, n), lhsT=onesb, rhs=sq[:, 0:n], start=(c == 0), stop=(c == nch - 1)),
                          [cstb, sq], [PB[4]])
                mk.op("act", lambda e: e.activation(out=R_[:, 0:n], in_=bank(4, n), func=AF.Sqrt, bias=dim * RMS_EPS, scale=1.0), [PB[4]], [R_])
                mk.op("dve", lambda e: e.reciprocal(out=R_[:, 0:n], in_=R_[:, 0:n]), [R_], [R_])
                for c in range(nch):
                    mk.op("dve", lambda e: e.scalar_tensor_tensor(out=ob[:, c, 0:n], in0=raw[:, c, 0:n], scalar=qnws[:, nwoff + c:nwoff + c + 1],
                                                                  in1=R_[:, 0:n], op0=ALU.mult, op1=ALU.mult), [raw, qnws, R_], [ob])
                mk.dma("sp", dst.rearrange("(c p) t -> p c t", p=128)[:, :, a:b], ob[:, 0:nch, 0:n], ob, load=False)
        cs = mk.tile(es, "cs", [64, 2, T], F32)
        mk.dma("sp", cs[:], cs_d.rearrange("c p t -> p c t"), cs)
        wg = load_w(C_KR, 64)
        wg2 = load_w(0, 64, w_krsw[l].rearrange("(k p) n -> p k n", p=128))
        krb = mk.tile(es, "krb", [64, T], BF16)
        kt1 = mk.tile(es, "kt1", [64, 512], F32)
        kt2 = mk.tile(es, "kt2", [64, 512], F32)
        for bi, (a, b) in enumerate(TB):
            n = b - a
            p1 = next_pb()
            proj_fm(wg, 0, 64, a, b, p1)
            p2 = next_pb()
            proj_fm(wg2, 0, 64, a, b, p2)
            mk.op("dve", lambda e: e.tensor_tensor(out=kt1[:, 0:n], in0=PS[0:64, p1 * 512:p1 * 512 + n], in1=cs[:, 0, a:b], op=ALU.mult), [PB[p1], cs], [kt1])
            mk.op("dve", lambda e: e.tensor_tensor(out=kt2[:, 0:n], in0=PS[0:64, p2 * 512:p2 * 512 + n], in1=cs[:, 1, a:b], op=ALU.mult), [PB[p2], cs], [kt2])
            mk.op("pool", lambda e: e.tensor_tensor(out=krb[:, a:b], in0=kt1[:, 0:n], in1=kt2[:, 0:n], op=ALU.add), [kt1, kt2], [krb])
        mk.dma("sp", krT, krb[:], krb, load=False)
        gbs = [mk.tile(es, "gb", [128, T], BF16) for _ in range(2)]
        for c in range(24):
            if c % 4 == 0:
                wg = load_w(C_GATE + c * 128, 512)
            gb = gbs[c % 2]
            for bi, (a, b) in enumerate(TB):
                pb = next_pb()
                proj_fm(wg, (c % 4) * 128, 128, a, b, pb)
                mk.op("act", lambda e: e.activation(out=gb[:, a:b], in_=bank(pb, b - a), func=AF.Sigmoid), [PB[pb]], [gb])
            mk.dma("sp", gateT[c * 128:(c + 1) * 128, :], gb[:], gb, load=False)
        es = sub_end(es)
        rawf = [mk.tile(es, "rawf", [128, T], F32) for _ in range(2)]
        accf = [mk.tile(es, "accf", [128, T], F32) for _ in range(2)]
        xbb = [mk.tile(es, "xbb", [128, T], BF16) for _ in range(2)]
        tmT = [mk.tile(es, "tmT", [128, NT, 128], BF16) for _ in range(2)]

        def conv(raw, acc, wcol, bcol):
            mk.op("act", lambda e: e.activation(out=acc[:], in_=raw[:], func=AF.Identity, scale=vec[:, wcol + 2:wcol + 3], bias=vec[:, bcol:bcol + 1]),
                  [raw, vec], [acc])
            for (s0, s1) in SEQS:
                for (eng, j, oa, ob_, ia, ib) in (("dve", 0, s0 + 2, s1, s0, s1 - 2), ("dve", 1, s0 + 1, s1, s0, s1 - 1), ("dve", 3, s0, s1 - 1, s0 + 1, s1)):
                    mk.op(eng, lambda e: e.scalar_tensor_tensor(out=acc[:, oa:ob_], in0=raw[:, ia:ib], scalar=vec[:, wcol + j:wcol + j + 1],
                                                                in1=acc[:, oa:ob_], op0=ALU.mult, op1=ALU.add), [raw, vec, acc], [acc])

        for c in range(16):
            if c % 4 == 0:
                wg = load_w(C_XBC + c * 128, 512)
            raw, acc, xb = rawf[c % 2], accf[c % 2], xbb[c % 2]
            for bi, (a, b) in enumerate(TB):
                pb = next_pb()
                proj_fm(wg, (c % 4) * 128, 128, a, b, pb)
                copy(evac_eng(), raw[:, a:b], bank(pb, b - a), [PB[pb]], [raw])
            conv(raw, acc, V_SCW + c * 4, V_SCB + c)
            mk.op("act", lambda e: e.activation(out=xb[:], in_=acc[:], func=AF.Silu), [acc], [xb])
            if c >= 8:
                mk.dma("sp", bcT[(c - 8) * 128:(c - 7) * 128, :], xb[:], xb, load=False)
            if c < 12:
                tm = tmT[c % 2]
                for g0 in range(0, NT, 8):
                    g1 = min(NT, g0 + 8)
                    pb = 4 + (g0 // 8) % 2
                    for t in range(g0, g1):
                        mk.op("pe", lambda e: e.transpose(out=bankb(pb)[:, (t - g0) * 128:(t - g0 + 1) * 128], in_=xb[:, t * 128:(t + 1) * 128], identity=identb),
                              [xb, cstb], [PB[pb]])
                    copy(evac_eng(), tm[:, g0:g1, :], bankb(pb)[:, 0:(g1 - g0) * 128].rearrange("p (t c) -> p t c", c=128), [PB[pb]], [tm])
                if c < 8:
                    mk.dma("sp", xs_tm.rearrange("(t p) c -> p t c", p=128)[:, :, c * 128:(c + 1) * 128], tm[:], tm, load=False)
                else:
                    mk.dma("sp", b_tm.rearrange("(t p) c -> p t c", p=128)[:, :, (c - 8) * 128:(c - 7) * 128], tm[:], tm, load=False)
        es = sub_end(es)
        rawf = [mk.tile(es, "rawf", [128, T], F32) for _ in range(2)]
        bd = mk.tile(es, "bd", [128, 32, 128], BF16)
        mk.dma("pool", bd[:], lru_bd[l].rearrange("m p n -> p m n"), bd)
        xc = mk.tile(es, "xc", [128, T], F32)
        xcb = mk.tile(es, "xcb", [128, T], BF16)
        ra = mk.tile(es, "ra", [128, T], F32)
        ib_ = mk.tile(es, "ib", [128, T], F32)
        tq = mk.tile(es, "tq", [128, T], F32)
        hh = [mk.tile(es, "hh", [128, T], F32) for _ in range(2)]
        gg = mk.tile(es, "gg", [128, T], F32)
        yb = [mk.tile(es, "yb", [128, T], BF16) for _ in range(2)]
        for c in range(8):
            if c % 4 == 0:
                wgx = load_w(C_LX + c * 128, 512)
                wgg = load_w(C_LG + c * 128, 512)
            raw = rawf[c % 2]
            for bi, (a, b) in enumerate(TB):
                pb = next_pb()
                proj_fm(wgx, (c % 4) * 128, 128, a, b, pb)
                copy(evac_eng(), raw[:, a:b], bank(pb, b - a), [PB[pb]], [raw])
            conv(raw, xc, V_LCW + c * 4, V_LCB + c)
            mk.op("pool", lambda e: e.tensor_copy(out=xcb[:], in_=xc[:]), [xc], [xcb])
            for d in range(2):
                for gi, (gt, boff) in enumerate(((ra, V_LBA), (ib_, V_LBX))):
                    m = (d * 2 + gi) * 8 + c
                    for bi, (a, b) in enumerate(TB):
                        pb = next_pb()
                        mk.op("pe", lambda e: e.matmul(bank(pb, b - a), lhsT=bd[:, m, :], rhs=xcb[:, a:b], start=True, stop=True), [bd, xcb], [PB[pb]])
                        mk.op("act", lambda e: e.activation(out=gt[:, a:b], in_=bank(pb, b - a), func=AF.Sigmoid,
                                                            bias=vec[:, boff + d * 8 + c:boff + d * 8 + c + 1], scale=1.0), [PB[pb], vec], [gt])
                mk.op("act", lambda e: e.activation(out=ra[:], in_=ra[:], func=AF.Exp, scale=lcs[:, d * 8 + c:d * 8 + c + 1]), [ra, lcs], [ra])
                mk.op("pool", lambda e: e.tensor_tensor(out=tq[:], in0=ra[:], in1=ra[:], op=ALU.mult), [ra], [tq])
                mk.op("act", lambda e: e.activation(out=tq[:], in_=tq[:], func=AF.Sqrt, bias=1.0, scale=-1.0), [tq], [tq])
                mk.op("dve", lambda e: e.tensor_tensor(out=ib_[:], in0=ib_[:], in1=tq[:], op=ALU.mult), [ib_, tq], [ib_])
                mk.op("dve", lambda e: e.tensor_tensor(out=ib_[:], in0=ib_[:], in1=xc[:], op=ALU.mult), [ib_, xc], [ib_])
                h_ = hh[d]
                if d == 0:
                    mk.op("dve", lambda e: e.tensor_tensor_scan(out=h_[:, 0:256], data0=ra[:, 0:256], data1=ib_[:, 0:256], initial=0.0,
                                                                op0=ALU.mult, op1=ALU.add), [ra, ib_], [h_])
                    mk.op("dve", lambda e: e.tensor_tensor_scan(out=h_[:, 256:T], data0=ra[:, 256:T], data1=ib_[:, 256:T], initial=h_[:, 255:256],
                                                                op0=ALU.mult, op1=ALU.add), [ra, ib_, h_], [h_])
                else:
                    mk.op("dve", lambda e: e.tensor_tensor_scan(out=h_[:, 0:256][:, ::-1], data0=ra[:, 0:256][:, ::-1], data1=ib_[:, 0:256][:, ::-1],
                                                                initial=0.0, op0=ALU.mult, op1=ALU.add), [ra, ib_], [h_])
                    mk.op("dve", lambda e: e.tensor_tensor_scan(out=h_[:, 256:T][:, ::-1], data0=ra[:, 256:T][:, ::-1], data1=ib_[:, 256:T][:, ::-1],
                                                                initial=h_[:, 0:1], op0=ALU.mult, op1=ALU.add), [ra, ib_, h_], [h_])
            for bi, (a, b) in enumerate(TB):
                pb = next_pb()
                proj_fm(wgg, (c % 4) * 128, 128, a, b, pb)
                mk.op("act", lambda e: e.activation(out=gg[:, a:b], in_=bank(pb, b - a), func=AF.Gelu_apprx_tanh), [PB[pb]], [gg])
            mk.op("pool", lambda e: e.tensor_tensor(out=hh[0][:], in0=hh[0][:], in1=hh[1][:], op=ALU.add), [hh[0], hh[1]], [hh[0]])
            y_ = yb[c % 2]
            mk.op("dve", lambda e: e.tensor_tensor(out=y_[:], in0=hh[0][:], in1=gg[:], op=ALU.mult), [hh[0], gg], [y_])
            mk.dma("sp", ylruT[c * 128:(c + 1) * 128, :], y_[:], y_, load=False)
        es = sub_end(es)
        wz = mk.tile(es, "wz", [128, 8, 1024], BF16)
        mk.dma("pool", wz[:], wv[:, :, C_Z:C_Z + 1024], wz)
        wdt = mk.tile(es, "wdt", [128, 8, 32], BF16)
        mk.dma("pool", wdt[:], wv[:, :, C_DT:C_DT + 32], wdt)
        dta = mk.tile(es, "dta", [128, NT, 32], F32)
        zts = [mk.tile(es, "zt", [128, D], BF16) for _ in range(2)]
        for t in range(NT):
            zt = zts[t % 2]
            for half in range(2):
                pb = next_pb()
                for k in range(8):
                    mk.op("pe", lambda e: e.matmul(bank(pb), lhsT=uT[:, k, t * 128:(t + 1) * 128], rhs=wz[:, k, half * 512:(half + 1) * 512],
                                                   start=(k == 0), stop=(k == 7)), [uT, wz], [PB[pb]])
                mk.op("act", lambda e: e.activation(out=zt[:, half * 512:(half + 1) * 512], in_=bank(pb), func=AF.Silu), [PB[pb]], [zt])
            mk.dma("sp", zs[t * 128:(t + 1) * 128, :], zt[:], zt, load=False)
            pb = 4 + t % 2
            for k in range(8):
                mk.op("pe", lambda e: e.matmul(bank(pb, 32), lhsT=uT[:, k, t * 128:(t + 1) * 128], rhs=wdt[:, k, :], start=(k == 0), stop=(k == 7)),
                      [uT, wdt], [PB[pb]])
            mk.op("dve", lambda e: e.tensor_copy(out=dta[:, t, :], in_=bank(pb, 32)), [PB[pb]], [dta])
        mk.dma("sp", dtr.rearrange("(t p) c -> p t c", p=128), dta[:], dta, load=False)
        mk.end_stage(es)
        mk.end_stage(es_outer)

    def stage_mla(l, last):
        es = ExitStack()
        cqn = mk.tile(es, "cqn", [128, 4, T], BF16)
        ckvn = mk.tile(es, "ckvn", [128, 2, T], BF16)
        krb = mk.tile(es, "krb", [64, T], BF16)
        cs = mk.tile(es, "cs", [64, 2, T], F32)
        mk.dma("sp", cqn[:], cqT.rearrange("(c p) t -> p c t", p=128), cqn)
        mk.dma("sp", ckvn[:], ckvT.rearrange("(c p) t -> p c t", p=128), ckvn)
        mk.dma("sp", krb[:], krT, krb)
        mk.dma("sp", cs[:], cs_d.rearrange("c p t -> p c t"), cs)
        wqs = [mk.tile(es, "wq", [128, 4, 256], BF16) for _ in range(2)]
        wks = [mk.tile(es, "wk", [128, 2, 256], BF16) for _ in range(2)]
        qn = mk.tile(es, "qn", [128, T], BF16)
        qr = mk.tile(es, "qr", [64, T], BF16)
        kn = mk.tile(es, "kn", [128, T], BF16)
        vh = mk.tile(es, "vh", [128, NT, 128], BF16)
        kt1 = mk.tile(es, "kt1", [64, 512], F32)
        kt2 = mk.tile(es, "kt2", [64, 512], F32)
        Ps = [mk.tile(es, "P", [128, T], BF16) for _ in range(2)]
        PTs = [mk.tile(es, "PT", [128, NT, 128], BF16) for _ in range(2)]
        sm = [mk.tile(es, "sm", [128, 16], F32) for _ in range(2)]
        PB7b = mk.tracker("pb7b")
        ob = [mk.tile(es, "ob", [128, 128], BF16) for _ in range(2)]
        aT = [mk.tile(es, "aT", [128, T], BF16) for _ in range(2)]
        wqv = w_uqx[l].rearrange("(k p) n -> p k n", p=128)
        wkv = w_ukv[l].rearrange("(k p) n -> p k n", p=128)
        t0 = 2 if last else 0
        it = 0
        for h in range(8):
            wq, wk = wqs[h % 2], wks[h % 2]
            mk.dma("pool", wq[:], wqv[:, :, h * 256:(h + 1) * 256], wq)
            mk.dma("pool", wk[:], wkv[:, :, h * 256:(h + 1) * 256], wk)
            for bi, (a, b) in enumerate(TB):
                n = b - a
                for k in range(4):
                    mk.op("pe", lambda e: e.matmul(bank(5, n), lhsT=wq[:, k, 0:128], rhs=cqn[:, k, a:b], start=(k == 0), stop=(k == 3)), [wq, cqn], [PB[5]])
                copy("act", qn[:, a:b], bank(5, n), [PB[5]], [qn])
                for k in range(4):
                    mk.op("pe", lambda e: e.matmul(PS[0:64, 6 * 512:6 * 512 + n], lhsT=wq[:, k, 128:192], rhs=cqn[:, k, a:b], start=(k == 0), stop=(k == 3)), [wq, cqn], [PB[6]])
                for k in range(4):
                    mk.op("pe", lambda e: e.matmul(PS[0:64, 7 * 512:7 * 512 + n], lhsT=wq[:, k, 192:256], rhs=cqn[:, k, a:b], start=(k == 0), stop=(k == 3)), [wq, cqn], [PB[7]])
                mk.op("dve", lambda e: e.tensor_tensor(out=kt1[:, 0:n], in0=PS[0:64, 6 * 512:6 * 512 + n], in1=cs[:, 0, a:b], op=ALU.mult), [PB[6], cs], [kt1])
                mk.op("dve", lambda e: e.tensor_tensor(out=kt2[:, 0:n], in0=PS[0:64, 7 * 512:7 * 512 + n], in1=cs[:, 1, a:b], op=ALU.mult), [PB[7], cs], [kt2])
                mk.op("pool", lambda e: e.tensor_tensor(out=qr[:, a:b], in0=kt1[:, 0:n], in1=kt2[:, 0:n], op=ALU.add), [kt1, kt2], [qr])
                for k in range(2):
                    mk.op("pe", lambda e: e.matmul(bank(5, n), lhsT=wk[:, k, 0:128], rhs=ckvn[:, k, a:b], start=(k == 0), stop=(k == 1)), [wk, ckvn], [PB[5]])
                copy("dve", kn[:, a:b], bank(5, n), [PB[5]], [kn])
            for g0 in range(0, NT, 4):
                g1 = min(NT, g0 + 4)
                pb = 6 + (g0 // 4) % 2
                for t in range(g0, g1):
                    for k in range(2):
                        mk.op("pe", lambda e: e.matmul(bank(pb, 128, (t - g0) * 128), lhsT=ckvn[:, k, t * 128:(t + 1) * 128], rhs=wk[:, k, 128:256],
                                                       start=(k == 0), stop=(k == 1)), [ckvn, wk], [PB[pb]])
                copy(evac_eng(), vh[:, g0:g1, :], bank(pb, (g1 - g0) * 128).rearrange("p (t c) -> p t c", c=128), [PB[pb]], [vh])
            a_ = aT[h % 2]
            units = list(range(t0, NT))

            def unit_bufs(i):
                return Ps[i % 2], PTs[i % 2], sm[i % 2], ob[i % 2]

            def emit_S(i):
                t = units[i]
                nk = 256 if t < 2 else T
                P_, PT_, s_, o_ = unit_bufs(i)
                nb = (nk + 511) // 512
                for kb, (a, b) in enumerate(TB):
                    if a >= nk:
                        break
                    b = min(b, nk)
                    mk.op("pe", lambda e: e.matmul(PS[:, a:b], lhsT=qn[:, t * 128:(t + 1) * 128], rhs=kn[:, a:b], start=True, stop=False), [qn, kn], [PB[kb]])
                    mk.op("pe", lambda e: e.matmul(PS[:, a:b], lhsT=qr[:, t * 128:(t + 1) * 128], rhs=krb[:, a:b], start=False, stop=True), [qr, krb], [PB[kb]])
                    mk.op("dve", lambda e: e.reduce_max(out=s_[:, kb:kb + 1], in_=PS[:, a:b], axis=AX), [PB[kb]], [s_])
                if nb > 1:
                    mk.op("dve", lambda e: e.reduce_max(out=s_[:, 5:6], in_=s_[:, 0:nb], axis=AX), [s_], [s_])
                    mx = s_[:, 5:6]
                else:
                    mx = s_[:, 0:1]
                mk.op("dve", lambda e: e.tensor_scalar_mul(out=s_[:, 6:7], in0=mx, scalar1=-ATTN_SCALE), [s_], [s_])
                mk.op("dve", lambda e: e.memset(s_[:, 8:13], 0.0), [], [s_])

            def emit_exp(i):
                t = units[i]
                nk = 256 if t < 2 else T
                P_, PT_, s_, o_ = unit_bufs(i)
                for kb, (a, b) in enumerate(TB):
                    if a >= nk:
                        break
                    b = min(b, nk)
                    mk.op("act", lambda e: e.activation(out=P_[:, a:b], in_=PS[:, a:b], func=AF.Exp, bias=s_[:, 6:7], scale=ATTN_SCALE,
                                                        accum_out=s_[:, 8 + kb:9 + kb]), [PB[kb], s_], [P_, s_])

            def emit_T(i):
                t = units[i]
                nk = 256 if t < 2 else T
                nkt = nk // 128
                P_, PT_, s_, o_ = unit_bufs(i)
                for g0 in range(0, nkt, 8):
                    g1 = min(nkt, g0 + 8)
                    pb = 5 + (g0 // 8) % 2
                    for kt in range(g0, g1):
                        mk.op("pe", lambda e: e.transpose(out=bankb(pb)[:, (kt - g0) * 128:(kt - g0 + 1) * 128], in_=P_[:, kt * 128:(kt + 1) * 128], identity=identb),
                              [P_, cstb], [PB[pb]])
                    copy("dve" if (g0 // 8) % 2 == 0 else "act", PT_[:, g0:g1, :], bankb(pb)[:, 0:(g1 - g0) * 128].rearrange("p (t c) -> p t c", c=128), [PB[pb]], [PT_])

            def emit_PV(i):
                t = units[i]
                nk = 256 if t < 2 else T
                nkt = nk // 128
                P_, PT_, s_, o_ = unit_bufs(i)
                for kt in range(nkt):
                    mk.op("pe", lambda e: e.matmul(bank(7, 128), lhsT=PT_[:, kt, :], rhs=vh[:, kt, :], start=(kt == 0), stop=(kt == nkt - 1)), [PT_, vh], [PB[7]])
                mk.op("dve", lambda e: e.reduce_sum(out=s_[:, 13:14], in_=s_[:, 8:13], axis=AX), [s_], [s_])
                mk.op("dve", lambda e: e.reciprocal(out=s_[:, 14:15], in_=s_[:, 13:14]), [s_], [s_])
                mk.op("act", lambda e: e.activation(out=o_[:], in_=bank(7, 128), func=AF.Identity, scale=s_[:, 14:15]), [PB[7], s_], [o_])
                mk.op("pe", lambda e: e.transpose(out=bankb(7)[:, 512:640], in_=o_[:], identity=identb), [o_, cstb], [PB7b])
                copy("dve", a_[:, t * 128:(t + 1) * 128], bankb(7)[:, 512:640], [PB7b], [a_])

            emit_S(0)
            emit_exp(0)
            for i in range(len(units)):
                if i + 1 < len(units):
                    emit_S(i + 1)
                emit_T(i)
                if i + 1 < len(units):
                    emit_exp(i + 1)
                emit_PV(i)
            mk.dma("sp", attT[h * 128:(h + 1) * 128, t0 * 128:T], a_[:, t0 * 128:T], a_, load=False)
        mk.end_stage(es)

    def stage_ssd(l, last):
        es = ExitStack()
        t0 = 2 if last else 0
        dt = mk.tile(es, "dt", [128, NT, 32], F32)
        dta = mk.tile(es, "dtA", [128, NT, 32], F32)
        ndta = mk.tile(es, "ndta", [128, NT, 32], F32)
        tmpa = mk.tile(es, "tmpa", [128, NT, 32], F32)
        aneg = mk.tile(es, "aneg", [128, 32], F32)
        mk.dma("sp", dt[:], dtr.rearrange("(t p) c -> p t c", p=128), dt)
        bias_b = SROW(R_DTB, 32).unsqueeze(1).to_broadcast([128, NT, 32])
        mk.op("dve", lambda e: e.tensor_tensor(out=dt[:], in0=dt[:], in1=bias_b, op=ALU.add), [dt, srow], [dt])
        mk.op("act", lambda e: e.activation(out=tmpa[:], in_=dt[:], func=AF.Abs), [dt], [tmpa])
        mk.op("act", lambda e: e.activation(out=tmpa[:], in_=tmpa[:], func=AF.Exp, scale=-1.0), [tmpa], [tmpa])
        mk.op("act", lambda e: e.activation(out=tmpa[:], in_=tmpa[:], func=AF.Ln, bias=1.0, scale=1.0), [tmpa], [tmpa])
        mk.op("dve", lambda e: e.tensor_scalar_max(out=dt[:], in0=dt[:], scalar1=0.0), [dt], [dt])
        mk.op("dve", lambda e: e.tensor_tensor(out=dt[:], in0=dt[:], in1=tmpa[:], op=ALU.add), [dt, tmpa], [dt])
        mk.op("act", lambda e: e.activation(out=aneg[:], in_=SROW(R_ALOG, 32), func=AF.Exp), [srow], [aneg])
        mk.op("dve", lambda e: e.tensor_scalar_mul(out=aneg[:], in0=aneg[:], scalar1=-1.0), [aneg], [aneg])
        mk.op("dve", lambda e: e.tensor_tensor(out=dta[:], in0=dt[:], in1=aneg[:].unsqueeze(1).to_broadcast([128, NT, 32]), op=ALU.mult), [dt, aneg], [dta])
        mk.op("dve", lambda e: e.tensor_scalar_mul(out=ndta[:], in0=dta[:], scalar1=-1.0), [dta], [ndta])
        tri = [triU, triL]

        def small_stats(t, d, sst):
            mk.op("pe", lambda e: e.matmul(bank(7, 16), lhsT=tri[d], rhs=dta[:, t, d * 16:(d + 1) * 16], start=True, stop=True), [cst, dta], [PB[7]])
            mk.op("pe", lambda e: e.matmul(bank(7, 16, 16), lhsT=onesf, rhs=dta[:, t, d * 16:(d + 1) * 16], start=True, stop=True), [cst, dta], [PB[7]])
            copy("act", sst[:, 0:32], bank(7, 32), [PB[7]], [sst])

        xss = [mk.tile(es, "xs", [128, D], BF16) for _ in range(2)]
        bts = [mk.tile(es, "bt", [128, 512], BF16) for _ in range(2)]
        ssts = [mk.tile(es, "sst", [128, 64], F32) for _ in range(2)]
        xgd = [mk.tile(es, "xgd", [128, D], BF16) for _ in range(2)]
        hbf = [mk.tile(es, "hbf", [128, D], BF16) for _ in range(2)]
        St = mk.tile(es, "St", [128, D], F32)
        n1 = 0
        for d in range(2):
            order = list(range(NT)) if d == 0 else [1, 0] + list(range(NT - 1, 1, -1))
            mk.op("pool", lambda e: e.memset(St[:], 0.0), [], [St])
            for t in order:
                xs_, bt_, sst, xg_, hb_ = xss[n1 % 2], bts[n1 % 2], ssts[n1 % 2], xgd[n1 % 2], hbf[n1 % 2]
                n1 += 1
                mk.dma("sp", xs_[:], xs_tm[t * 128:(t + 1) * 128, :], xs_)
                mk.dma("sp", bt_[:], b_tm[t * 128:(t + 1) * 128, :], bt_)
                copy("act", hb_[:], St[:], [St], [hb_])
                mk.dma("sp", hprev[d, t], hb_[:], hb_, load=False)
                small_stats(t, d, sst)
                mk.op("dve", lambda e: e.tensor_tensor(out=sst[:, 32:48], in0=sst[:, 16:32], in1=sst[:, 0:16], op=ALU.subtract), [sst], [sst])
                mk.op("act", lambda e: e.activation(out=sst[:, 32:48], in_=sst[:, 32:48], func=AF.Exp), [sst], [sst])
                mk.op("dve", lambda e: e.tensor_tensor(out=sst[:, 32:48], in0=sst[:, 32:48], in1=dt[:, t, d * 16:(d + 1) * 16], op=ALU.mult), [sst, dt], [sst])
                mk.op("act", lambda e: e.activation(out=sst[:, 48:64], in_=sst[:, 16:32], func=AF.Exp), [sst], [sst])
                mk.op("dve", lambda e: e.tensor_tensor(out=xg_[:].rearrange("p (h j) -> p h j", j=64), in0=xs_[:].rearrange("p (h j) -> p h j", j=64),
                                                       in1=sst[:, 32:48].unsqueeze(2).to_broadcast([128, 16, 64]), op=ALU.mult), [xs_, sst], [xg_])
                for g in range(4):
                    pb = g // 2
                    mk.op("pe", lambda e: e.matmul(bank(pb, 256, (g % 2) * 256), lhsT=bt_[:, g * 128:(g + 1) * 128], rhs=xg_[:, g * 256:(g + 1) * 256],
                                                   start=True, stop=True), [bt_, xg_], [PB[pb]])
                mk.op("pool", lambda e: e.tensor_tensor(out=St[:].rearrange("p (h j) -> p h j", j=64), in0=St[:].rearrange("p (h j) -> p h j", j=64),
                                                        in1=sst[:, 48:64].unsqueeze(2).to_broadcast([128, 16, 64]), op=ALU.mult), [St, sst], [St])
                mk.op("dve", lambda e: e.tensor_tensor(out=St[:], in0=St[:], in1=PS[:, 0:1024], op=ALU.add), [St, PB[0], PB[1]], [St])
        mk.barrier()
        cts = [mk.tile(es, "ct", [128, 8, 128], BF16) for _ in range(2)]
        hps = [mk.tile(es, "hp", [128, 2, D], BF16) for _ in range(2)]
        zts = [mk.tile(es, "zt", [128, D], BF16) for _ in range(2)]
        cbm = mk.tile(es, "cbm", [128, 2, 512], BF16)
        rhs1 = mk.tile(es, "rhs1", [128, 16, 128], F32)
        rhs2 = mk.tile(es, "rhs2", [128, 16, 128], F32)
        Lm = mk.tile(es, "Lm", [128, 8, 128], F32)
        MT = mk.tile(es, "MT", [128, 16, 128], BF16)
        xg = mk.tile(es, "xg", [128, D], BF16)
        yo = mk.tile(es, "yo", [128, D], F32)
        yt = mk.tile(es, "yt", [128, D], F32)
        ynb = mk.tile(es, "ynb", [128, D], BF16)
        ys = [mk.tile(es, "ys", [128, 8, 128], BF16) for _ in range(2)]
        sq_ = mk.tile(es, "sq", [128, 256], F32)
        g4 = mk.tile(es, "g4", [128, 8], F32)
        bcv = bcT.rearrange("(c p) t -> p c t", p=128)
        for i, t in enumerate(range(t0, NT)):
            xs_, ct, hp, zt, sst, y_s = xss[i % 2], cts[i % 2], hps[i % 2], zts[i % 2], ssts[i % 2], ys[i % 2]
            mk.dma("sp", xs_[:], xs_tm[t * 128:(t + 1) * 128, :], xs_)
            mk.dma("sp", ct[:], bcv[:, :, t * 128:(t + 1) * 128], ct)
            mk.dma("sp", hp[:], hprev[:, t].rearrange("d p n -> p d n"), hp)
            mk.dma("sp", zt[:], zs[t * 128:(t + 1) * 128, :], zt)
            for g in range(4):
                mk.op("pe", lambda e: e.matmul(bank(0, 128, g * 128), lhsT=ct[:, g, :], rhs=ct[:, 4 + g, :], start=True, stop=True), [ct], [PB[0]])
            for d in range(2):
                mk.op("dve", lambda e: e.tensor_tensor(out=cbm[:, d, :].rearrange("p (g q) -> p g q", q=128), in0=bank(0).rearrange("p (g q) -> p g q", q=128),
                                                       in1=tri[d].unsqueeze(1).to_broadcast([128, 4, 128]), op=ALU.mult), [PB[0], cst], [cbm])
            for d in range(2):
                dsl = dta[:, t, d * 16:(d + 1) * 16]
                mk.op("dve", lambda e: e.tensor_tensor(out=rhs1[:], in0=tri[d].unsqueeze(1).to_broadcast([128, 16, 128]),
                                                       in1=dsl.unsqueeze(2).to_broadcast([128, 16, 128]), op=ALU.mult), [cst, dta], [rhs1])
                mk.op("pool", lambda e: e.tensor_copy(out=rhs2[:], in_=ndta[:, t, d * 16:(d + 1) * 16].unsqueeze(2).to_broadcast([128, 16, 128])), [ndta], [rhs2])
                small_stats(t, d, sst)
                mk.op("act", lambda e: e.activation(out=sst[:, 32:48], in_=sst[:, 0:16], func=AF.Exp), [sst], [sst])
                mk.op("dve", lambda e: e.tensor_tensor(out=xg[:].rearrange("p (h j) -> p h j", j=64), in0=xs_[:].rearrange("p (h j) -> p h j", j=64),
                                                       in1=dt[:, t, d * 16:(d + 1) * 16].unsqueeze(2).to_broadcast([128, 16, 64]), op=ALU.mult), [xs_, dt], [xg])
                for hf in range(2):
                    for q4 in range(2):
                        pb = 1 + q4
                        hs = hf * 8 + q4 * 4
                        mk.op("pe", lambda e: e.matmul(bank(pb), lhsT=onesf, rhs=rhs1[:, hs:hs + 4, :], start=True, stop=False), [cst, rhs1], [PB[pb]])
                        mk.op("pe", lambda e: e.matmul(bank(pb), lhsT=tri[d], rhs=rhs2[:, hs:hs + 4, :], start=False, stop=True), [cst, rhs2], [PB[pb]])
                    mk.op("dve", lambda e: e.tensor_scalar_min(out=Lm[:].rearrange("p h q -> p (h q)"), in0=PS[:, 512:1536], scalar1=0.0), [PB[1], PB[2]], [Lm])
                    mk.op("act", lambda e: e.activation(out=Lm[:], in_=Lm[:], func=AF.Exp), [Lm], [Lm])
                    mk.op("pool", lambda e: e.tensor_tensor(out=MT[:, hf * 8:(hf + 1) * 8, :].rearrange("p (g x) q -> p g x q", x=4),
                                                            in0=Lm[:].rearrange("p (g x) q -> p g x q", x=4),
                                                            in1=cbm[:, d, hf * 256:(hf + 1) * 256].rearrange("p (g q) -> p g q", q=128).unsqueeze(2).to_broadcast([128, 2, 4, 128]),
                                                            op=ALU.mult), [Lm, cbm], [MT])
                for h in range(16):
                    pb = 3 + h // 8
                    mk.op("pe", lambda e: e.matmul(bank(pb, 64, (h % 8) * 64), lhsT=MT[:, h, :], rhs=xg[:, h * 64:(h + 1) * 64], start=(d == 0 and h % 8 == 0), stop=(d == 1), skip_group_check=True),
                          [MT, xg], [PB[pb]])
                for g in range(4):
                    pb = 5 + g // 2
                    mk.op("pe", lambda e: e.matmul(bank(pb, 256, (g % 2) * 256), lhsT=ct[:, 4 + g, :], rhs=hp[:, d, g * 256:(g + 1) * 256], start=True, stop=True),
                          [ct, hp], [PB[pb]])
                if d == 0:
                    mk.op("dve", lambda e: e.tensor_tensor(out=yo[:].rearrange("p (h j) -> p h j", j=64), in0=PS[:, 2560:3584].rearrange("p (h j) -> p h j", j=64),
                                                           in1=sst[:, 32:48].unsqueeze(2).to_broadcast([128, 16, 64]), op=ALU.mult), [PB[5], PB[6], sst], [yo])
                else:
                    mk.op("dve", lambda e: e.tensor_tensor(out=yt[:].rearrange("p (h j) -> p h j", j=64), in0=PS[:, 2560:3584].rearrange("p (h j) -> p h j", j=64),
                                                           in1=sst[:, 32:48].unsqueeze(2).to_broadcast([128, 16, 64]), op=ALU.mult), [PB[5], PB[6], sst], [yt])
            mk.op("pool", lambda e: e.tensor_tensor(out=yo[:], in0=yo[:], in1=yt[:], op=ALU.add), [yo, yt], [yo])
            mk.op("dve", lambda e: e.tensor_tensor(out=yt[:], in0=PS[:, 1536:2560], in1=yo[:], op=ALU.add), [PB[3], PB[4], yo], [yt])
            mk.op("pool", lambda e: e.tensor_tensor(out=yo[:].rearrange("p (h j) -> p h j", j=64), in0=xs_[:].rearrange("p (h j) -> p h j", j=64),
                                                    in1=SROW(R_DSK, 16).unsqueeze(2).to_broadcast([128, 16, 64]), op=ALU.mult), [xs_, srow], [yo])
            mk.op("dve", lambda e: e.tensor_tensor(out=yt[:], in0=yt[:], in1=yo[:], op=ALU.add), [yt, yo], [yt])
            mk.op("dve", lambda e: e.tensor_tensor(out=yt[:], in0=yt[:], in1=zt[:], op=ALU.mult), [yt, zt], [yt])
            mk.op("pool", lambda e: e.memset(g4[:], 0.0), [], [g4])
            for g in range(4):
                mk.op("act", lambda e: e.activation(out=sq_[:], in_=yt[:, g * 256:(g + 1) * 256], func=AF.Square, accum_out=g4[:, g:g + 1]), [yt], [sq_, g4])
            mk.op("act", lambda e: e.activation(out=g4[:, 4:8], in_=g4[:, 0:4], func=AF.Sqrt, bias=RMS_EPS, scale=1.0 / 256.0), [g4], [g4])
            mk.op("dve", lambda e: e.reciprocal(out=g4[:, 4:8], in_=g4[:, 4:8]), [g4], [g4])
            mk.op("dve", lambda e: e.tensor_tensor(out=ynb[:].rearrange("p (g j) -> p g j", j=256), in0=yt[:].rearrange("p (g j) -> p g j", j=256),
                                                   in1=g4[:, 4:8].unsqueeze(2).to_broadcast([128, 4, 256]), op=ALU.mult), [yt, g4], [ynb])
            for c in range(8):
                mk.op("pe", lambda e: e.transpose(out=bankb(7)[:, c * 128:(c + 1) * 128], in_=ynb[:, c * 128:(c + 1) * 128], identity=identb), [ynb, cstb], [PB[7]])
            for c in range(8):
                mk.op("act", lambda e: e.activation(out=y_s[:, c, :], in_=bankb(7)[:, c * 128:(c + 1) * 128], func=AF.Identity,
                                                    scale=vec[:, V_SNW + c:V_SNW + c + 1]), [PB[7], vec], [y_s])
            mk.dma("sp", yssdT.rearrange("(c p) t -> p c t", p=128)[:, :, t * 128:(t + 1) * 128], y_s[:], y_s, load=False)
        mk.end_stage(es)

    def stage_merge(l, last, src, dst_fn):
        es = ExitStack()
        t0 = 2 if last else 0
        c0 = t0 * 128
        mT = mk.tile(es, "mT", [128, 8, T], BF16)
        brs = [mk.tile(es, "br", [128, 8, T], BF16) for _ in range(2)]
        wbs = [mk.tile(es, "wb", [128, 8, 512], BF16) for _ in range(2)]
        gts = [mk.tile(es, "gt", [128, T], BF16) for _ in range(2)]
        tmp = [mk.tile(es, "tmp", [128, 512], BF16) for _ in range(2)]
        srcs = [attT, yssdT, ylruT]
        n = 0
        for br in range(3):
            b_ = brs[br % 2]
            mk.dma("sp", b_[:, :, c0:T], srcs[br].rearrange("(c p) t -> p c t", p=128)[:, :, c0:T], b_)
            wv = w_branch[l, br].rearrange("(k p) n -> p k n", p=128)
            for dc in range(8):
                if dc % 4 == 0:
                    wb = wbs[n % 2]
                    n += 1
                    mk.dma("pool", wb[:], wv[:, :, dc * 128:dc * 128 + 512], wb)
                gt = gts[dc % 2]
                mk.dma("sp", gt[:, c0:T], gateT[(br * 8 + dc) * 128:(br * 8 + dc + 1) * 128, c0:T], gt)
                for bi, (a, b) in enumerate(TB):
                    a = max(a, c0)
                    if a >= b:
                        continue
                    rr["mm"] += 1
                    pb = rr["mm"] % 4
                    for k in range(8):
                        mk.op("pe", lambda e: e.matmul(bank(pb, b - a), lhsT=wb[:, k, (dc % 4) * 128:(dc % 4 + 1) * 128], rhs=b_[:, k, a:b],
                                                       start=(k == 0), stop=(k == 7)), [wb, b_], [PB[pb]])
                    if br == 0:
                        mk.op("dve", lambda e: e.tensor_tensor(out=mT[:, dc, a:b], in0=bank(pb, b - a), in1=gt[:, a:b], op=ALU.mult), [PB[pb], gt], [mT])
                    else:
                        tp = tmp[bi % 2]
                        mk.op("dve", lambda e: e.tensor_tensor(out=tp[:, 0:b - a], in0=bank(pb, b - a), in1=gt[:, a:b], op=ALU.mult), [PB[pb], gt], [tp])
                        mk.op("pool", lambda e: e.tensor_tensor(out=mT[:, dc, a:b], in0=mT[:, dc, a:b], in1=tp[:, 0:b - a], op=ALU.add), [mT, tp], [mT])
        wo = mk.tile(es, "wo", [128, 8, D], BF16)
        mk.dma("pool", wo[:], w_out[l].rearrange("(k p) n -> p k n", p=128), wo)
        grow = build_grow(es, 2)
        lnrow = mk.tile(es, "lnrow", [128, 2, D], F32)
        mk.dma("sp", lnrow[:], rows_d[l, R_LN1G:R_LN1G + 2048].rearrange("(a n) -> a n", a=2).partition_broadcast(128), lnrow)
        hts = [mk.tile(es, "ht", [128, D], F32) for _ in range(2)]
        vts = [mk.tile(es, "vt", [128, D], F32) for _ in range(2)]
        sts = [mk.tile(es, "st", [128, 12], F32) for _ in range(2)]
        mvs = [mk.tile(es, "mv", [128, 4], F32) for _ in range(2)]
        for i, t in enumerate(range(t0, NT)):
            ht = hts[i % 2]
            mk.dma("sp", ht[:], src[t * 128:(t + 1) * 128, :], ht)
            pbs = (4, 5) if i % 2 == 0 else (6, 7)
            for half in range(2):
                pb = pbs[half]
                for dc in range(8):
                    mk.op("pe", lambda e: e.matmul(bank(pb), lhsT=mT[:, dc, t * 128:(t + 1) * 128], rhs=wo[:, dc, half * 512:(half + 1) * 512],
                                                   start=(dc == 0), stop=(dc == 7)), [mT, wo], [PB[pb]])
            res_ln(t, PS[:, pbs[0] * 512:pbs[0] * 512 + 1024], [PB[pbs[0]], PB[pbs[1]]], src, grow, lnrow, dst_fn, (ht, vts[i % 2], sts[i % 2], mvs[i % 2]))
        mk.end_stage(es)

    def stage_moe(l, last, src, dst_fn):
        es = ExitStack()
        t0 = 2 if last else 0
        c0 = t0 * 128
        uT = mk.tile(es, "u2T", [128, 8, T], BF16)
        gw = mk.tile(es, "gw", [128, NT, 32], F32)
        wrt = mk.tile(es, "wrt", [128, 8, 36], F32)
        mk.dma("sp", wrt[:], wr[l].rearrange("(k p) n -> p k n", p=128), wrt)
        es1 = ExitStack()
        stage_ln_mod(es1, src, 3, 4, uT, t0, router=(wrt, gw))
        mk.end_stage(es1)
        mk.stage_tiles.extend([uT, gw, wrt])
        es2 = ExitStack()
        acc = mk.tile(es, "acc", [128, NT, D], F32)
        w1s = [mk.tile(es2, "w1", [128, 8, 512], BF16) for _ in range(2)]
        w3s = [mk.tile(es2, "w3", [128, 8, 512], BF16) for _ in range(2)]
        w2s = [mk.tile(es2, "w2", [128, 4, D], BF16) for _ in range(2)]
        hid = [mk.tile(es2, "hid", [128, 4, 512], BF16) for _ in range(2)]
        sas = [mk.tile(es2, "sa", [128, 512], BF16) for _ in range(2)]
        nb = 0
        for ex in range(32):
            w1, w3, w2 = w1s[ex % 2], w3s[ex % 2], w2s[ex % 2]
            mk.dma("pool", w1[:], exp_w1[l, ex].rearrange("(k p) n -> p k n", p=128), w1)
            mk.dma("pool", w3[:], exp_w3[l, ex].rearrange("(k p) n -> p k n", p=128), w3)
            mk.dma("pool", w2[:], exp_w2[l, ex].rearrange("(k p) n -> p k n", p=128), w2)
            for bi, (a, b) in enumerate(TB):
                a = max(a, c0)
                if a >= b:
                    continue
                n = b - a
                hd_ = hid[nb % 2]
                nb += 1
                for hc in range(4):
                    pa, pb = (0, 1) if hc % 2 == 0 else (2, 3)
                    for k in range(8):
                        mk.op("pe", lambda e: e.matmul(bank(pa, n), lhsT=w1[:, k, hc * 128:(hc + 1) * 128], rhs=uT[:, k, a:b], start=(k == 0), stop=(k == 7)),
                              [w1, uT], [PB[pa]])
                    for k in range(8):
                        mk.op("pe", lambda e: e.matmul(bank(pb, n), lhsT=w3[:, k, hc * 128:(hc + 1) * 128], rhs=uT[:, k, a:b], start=(k == 0), stop=(k == 7)),
                              [w3, uT], [PB[pb]])
                    sa = sas[hc % 2]
                    mk.op("act", lambda e: e.activation(out=sa[:, 0:n], in_=bank(pa, n), func=AF.Silu), [PB[pa]], [sa])
                    mk.op("dve", lambda e: e.tensor_tensor(out=hd_[:, hc, 0:n], in0=bank(pb, n), in1=sa[:, 0:n], op=ALU.mult), [PB[pb], sa], [hd_])
                for ti, t in enumerate(range(a // 128, b // 128)):
                    pbs = (4, 5) if ti % 2 == 0 else (6, 7)
                    for half in range(2):
                        for hc in range(4):
                            mk.op("pe", lambda e: e.matmul(bank(pbs[half]), lhsT=hd_[:, hc, (t * 128 - a):(t * 128 - a) + 128], rhs=w2[:, hc, half * 512:(half + 1) * 512],
                                                           start=(hc == 0), stop=(hc == 3)), [hd_, w2], [PB[pbs[half]]])
                    yps = PS[:, pbs[0] * 512:pbs[0] * 512 + 1024]
                    if ex == 0:
                        mk.op("dve", lambda e: e.tensor_scalar_mul(out=acc[:, t, :], in0=yps, scalar1=gw[:, t, ex:ex + 1]), [PB[pbs[0]], PB[pbs[1]], gw], [acc])
                    else:
                        mk.op("dve", lambda e: e.scalar_tensor_tensor(out=acc[:, t, :], in0=yps, scalar=gw[:, t, ex:ex + 1], in1=acc[:, t, :],
                                                                      op0=ALU.mult, op1=ALU.add), [PB[pbs[0]], PB[pbs[1]], gw, acc], [acc])
        mk.end_stage(es2)
        mk.stage_tiles.extend([uT, gw, wrt, acc])
        grow = build_grow(es, 5)
        lnrow = mk.tile(es, "lnrow", [128, 2, D], F32)
        mk.dma("sp", lnrow[:], rows_d[l, R_LN2G:R_LN2G + 2048].rearrange("(a n) -> a n", a=2).partition_broadcast(128), lnrow)
        hts = [mk.tile(es, "ht", [128, D], F32) for _ in range(2)]
        vts = [mk.tile(es, "vt", [128, D], F32) for _ in range(2)]
        sts = [mk.tile(es, "st", [128, 12], F32) for _ in range(2)]
        mvs = [mk.tile(es, "mv", [128, 4], F32) for _ in range(2)]
        for i, t in enumerate(range(t0, NT)):
            ht = hts[i % 2]
            mk.dma("sp", ht[:], src[t * 128:(t + 1) * 128, :], ht)
            res_ln(t, acc[:, t, :], [acc], src, grow, lnrow, dst_fn, (ht, vts[i % 2], sts[i % 2], mvs[i % 2]))
        mk.end_stage(es)

    def hd_tile(t):
        return hd[t * 128:(t + 1) * 128, :]

    def y_tile(t):
        return y_out[(t - 2) * 128:(t - 1) * 128, :]

    for l in range(n_layers):
        last = (l == n_layers - 1)
        src = h0 if l == 0 else hd
        stage_mod(l)
        stage_inproj(l, src)
        if stop_after == "inproj":
            break
        stage_mla(l, last)
        if stop_after == "mla":
            break
        stage_ssd(l, last)
        if stop_after == "ssd":
            break
        stage_merge(l, last, src, hd_tile)
        if stop_after == "merge":
            break
        stage_moe(l, last, hd, y_tile if last else hd_tile)
    mk.barrier()
    print("built: inst", mk.n_inst, "waits", mk.n_wait, "cnt", mk.cnt)
    return nc


def _prep_shared(inp):
    f = np.float32
    Lh = inp["w_in"].shape[0]
    sh = {}
    sh["w_mod"] = np.ascontiguousarray(inp["w_mod"], f)
    sh["w_in"] = np.ascontiguousarray(inp["w_in"], f)
    kr = inp["w_in"][:, :, C_KR:C_KR + 64].reshape(Lh, D, 2, 2, 16)
    sh["w_krsw"] = np.ascontiguousarray(kr[:, :, :, ::-1, :].reshape(Lh, D, 64), f)
    uq = inp["w_uq"].reshape(Lh, 512, 8, 192)
    qr = uq[..., 128:].reshape(Lh, 512, 8, 2, 2, 16)
    qsw = qr[:, :, :, :, ::-1, :].reshape(Lh, 512, 8, 64)
    sh["w_uqx"] = np.ascontiguousarray(np.concatenate([uq, qsw], axis=-1).reshape(Lh, 512, 2048), f)
    sh["w_ukv"] = np.ascontiguousarray(inp["w_ukv"], f)
    bd = np.zeros((Lh, 2, 2, 8, 128, 128), f)
    for gi, key in enumerate(("lru_wa", "lru_wx")):
        w = inp[key]
        for c in range(8):
            bd[:, :, gi, c, 0:64, 0:64] = w[:, :, 2 * c]
            bd[:, :, gi, c, 64:128, 64:128] = w[:, :, 2 * c + 1]
    sh["lru_bd"] = bd.reshape(Lh, 32, 128, 128)
    sh["w_branch"] = np.ascontiguousarray(inp["w_branch"], f)
    sh["w_out"] = np.ascontiguousarray(inp["w_out"], f)
    sh["wr"] = np.ascontiguousarray(np.concatenate([inp["router_wg"], inp["router_we"]], axis=-1), f)
    sh["exp_w1"] = np.ascontiguousarray(inp["exp_w1"], f)
    sh["exp_w3"] = np.ascontiguousarray(inp["exp_w3"], f)
    sh["exp_w2"] = np.ascontiguousarray(inp["exp_w2"], f)
    vecs = np.zeros((Lh, 128, NV), f)

    def colfmt(v, nchunk):
        return v.reshape(v.shape[:-1] + (nchunk, 128))

    for l in range(Lh):
        bm = inp["b_mod"][l].reshape(48, 128).T
        vecs[l, :, V_BMOD:V_BMOD + 96] = np.repeat(bm, 2, axis=1)
        vecs[l, :, V_QNW:V_QNW + 4] = inp["q_norm_w"][l].reshape(4, 128).T
        vecs[l, :, V_KVNW:V_KVNW + 2] = inp["kv_norm_w"][l].reshape(2, 128).T
        vecs[l, :, V_SCW:V_SCW + 64] = inp["ssd_conv_w"][l].reshape(4, 16, 128).transpose(2, 1, 0).reshape(128, 64)
        vecs[l, :, V_SCB:V_SCB + 16] = inp["ssd_conv_b"][l].reshape(16, 128).T
        vecs[l, :, V_LCW:V_LCW + 32] = inp["lru_conv_w"][l].reshape(4, 8, 128).transpose(2, 1, 0).reshape(128, 32)
        vecs[l, :, V_LCB:V_LCB + 8] = inp["lru_conv_b"][l].reshape(8, 128).T
        vecs[l, :, V_LBA:V_LBA + 16] = inp["lru_ba"][l].reshape(2, 8, 128).transpose(2, 0, 1).reshape(128, 16)
        vecs[l, :, V_LBX:V_LBX + 16] = inp["lru_bx"][l].reshape(2, 8, 128).transpose(2, 0, 1).reshape(128, 16)
        vecs[l, :, V_LLAM:V_LLAM + 16] = inp["lru_lambda"][l].reshape(2, 8, 128).transpose(2, 0, 1).reshape(128, 16)
        vecs[l, :, V_SNW:V_SNW + 8] = inp["ssd_norm_w"][l].reshape(8, 128).T
    sh["vecs"] = vecs
    rows = np.zeros((Lh, NR), f)
    rows[:, R_LN1G:R_LN1G + 1024] = inp["ln1_g"]
    rows[:, R_LN1B:R_LN1B + 1024] = inp["ln1_b"]
    rows[:, R_LN2G:R_LN2G + 1024] = inp["ln2_g"]
    rows[:, R_LN2B:R_LN2B + 1024] = inp["ln2_b"]
    rows[:, R_ALOG:R_ALOG + 32] = inp["ssd_a_log"].reshape(Lh, 32)
    rows[:, R_DTB:R_DTB + 32] = inp["ssd_dt_bias"].reshape(Lh, 32)
    rows[:, R_DSK:R_DSK + 16] = inp["ssd_d"]
    rows[:, R_RB:R_RB + 4] = inp["router_bg"]
    rows[:, R_RB + 4:R_RB + 36] = inp["router_be"]
    sh["rows"] = rows
    idx = np.arange(128)
    consts = np.zeros((4, 128, 128), f)
    consts[0] = np.eye(128)
    consts[1] = (idx[:, None] <= idx[None, :])
    consts[2] = (idx[:, None] >= idx[None, :])
    consts[3] = 1.0
    sh["consts"] = consts
    pos = np.arange(2048)
    rowp = (pos // 64).astype(f)
    colp = (pos % 64).astype(f)
    inv = (10000.0 ** (-np.arange(16, dtype=f) / 16)).astype(f)
    ang = np.stack([rowp[:, None] * inv, colp[:, None] * inv], axis=1).astype(f)
    cos = np.cos(ang).astype(f)
    sin = np.sin(ang).astype(f)
    ct = np.ones((64, T), f)
    st = np.zeros((64, T), f)
    for a in range(2):
        for j in range(2):
            r0 = a * 32 + j * 16
            ct[r0:r0 + 16, 256:] = cos[:, a, :].T
            st[r0:r0 + 16, 256:] = (-1.0 if j == 0 else 1.0) * sin[:, a, :].T
    sh["cossin"] = np.stack([ct, st]).astype(f)
    return sh


def _prep_core(inp, b):
    f = np.float32
    m = {}
    m["h0"] = np.ascontiguousarray(np.concatenate([inp["ctx"][b], inp["x"][b]], axis=0), f)
    cv = np.zeros((128, 16), f)
    cv[:, 0::2] = inp["c"][b].reshape(8, 128).T
    cv[:, 1::2] = inp["c_ctx"].reshape(8, 128).T
    m["cvec"] = cv
    return m


_NC_CACHE = {}


def kernel(**inputs):
    inp = {k: np.asarray(v) for k, v in inputs.items()}
    if "nc" not in _NC_CACHE:
        _NC_CACHE["nc"] = build()
    nc = _NC_CACHE["nc"]
    sh = _prep_shared(inp)
    in_maps = []
    for b in range(8):
        m = dict(sh)
        m.update(_prep_core(inp, b))
        in_maps.append(m)
    res = run_bass_kernel_spmd(nc, in_maps, core_ids=list(range(8)))
    out = np.stack([np.asarray(r["y"], dtype=np.float32) for r in res.results], axis=0)
    return out
```

```python
import math
from contextlib import ExitStack
import numpy as np
import concourse.bass as bass
import concourse.mybir as mybir
from concourse.bass_utils import run_bass_kernel_spmd

F32 = mybir.dt.float32
BF16 = mybir.dt.bfloat16
AF = mybir.ActivationFunctionType
ALU = mybir.AluOpType
AX = mybir.AxisListType.X

D = 1024
T = 2304
NT = 18
L = 4
TB = [(0, 512), (512, 1024), (1024, 1536), (1536, 2048), (2048, 2304)]
SEQS = [(0, 256), (256, 2304)]
ALPHA = 8.0 ** 0.25
LN_EPS = 1e-5
RMS_EPS = 1e-6
ATTN_SCALE = 192.0 ** -0.5
C_CQ, C_CKV, C_KR, C_Z, C_XBC, C_DT, C_LX, C_LG, C_GATE = 0, 512, 768, 832, 1856, 3904, 3936, 4960, 5984
NCOL = 9056
V_BMOD, V_QNW, V_KVNW, V_SCW, V_SCB, V_LCW, V_LCB, V_LBA, V_LBX, V_LLAM, V_SNW = 0, 96, 100, 102, 166, 182, 214, 222, 238, 254, 270
NV = 278
R_LN1G, R_LN1B, R_LN2G, R_LN2B, R_ALOG, R_DTB, R_DSK, R_RB = 0, 1024, 2048, 3072, 4096, 4128, 4160, 4176
NR = 4212


class Tile:
    __slots__ = ("t", "name", "last_w", "reads", "dsem", "dcount")

    def __init__(self, t, name):
        self.t = t
        self.name = name
        self.last_w = None
        self.reads = []
        self.dsem = None
        self.dcount = 0

    def __getitem__(self, k):
        return self.t[k]


class MK:
    def __init__(self, nc):
        self.nc = nc
        self.E = {"pe": nc.tensor, "act": nc.scalar, "dve": nc.vector, "pool": nc.gpsimd, "sp": nc.sync}
        self.sem = {k: nc.alloc_semaphore("s_" + k) for k in self.E}
        self.cnt = {k: 0 for k in self.E}
        self.waited = {}
        self.sem_pool = []
        self.stage_tiles = []
        self.dma_active = []
        self.uid = 0
        self.n_inst = 0
        self.n_wait = 0

    def tile(self, es, name, shape, dt):
        self.uid += 1
        t = es.enter_context(self.nc.sbuf_tensor("%s_%d" % (name, self.uid), list(shape), dt))
        tl = Tile(t, name)
        self.stage_tiles.append(tl)
        return tl

    def tracker(self, name):
        return Tile(None, name)

    def _wait(self, eng, tok):
        if tok is None:
            return
        sem, val, src = tok
        if src == eng and eng == "pe":
            return
        key = (eng, id(sem))
        if self.waited.get(key, 0) >= val:
            return
        self.waited[key] = val
        self.E[eng].wait_ge(sem, val)
        self.n_wait += 1

    def _deps(self, eng, reads, writes):
        for b in reads:
            self._wait(eng, b.last_w)
        for b in writes:
            self._wait(eng, b.last_w)
            for r in b.reads:
                self._wait(eng, r)

    def op(self, eng, fn, reads=(), writes=()):
        self._deps(eng, reads, writes)
        inst = fn(self.E[eng])
        self.cnt[eng] += 1
        inst.then_inc(self.sem[eng], 1)
        tok = (self.sem[eng], self.cnt[eng], eng)
        for b in reads:
            b.reads.append(tok)
        for b in writes:
            b.last_w = tok
            b.reads = []
        self.n_inst += 1
        return inst

    def dma(self, q, out, in_, sbuf, load=True):
        if sbuf.dsem is None:
            if self.sem_pool:
                sbuf.dsem, sbuf.dcount = self.sem_pool.pop()
            else:
                self.uid += 1
                sbuf.dsem, sbuf.dcount = self.nc.alloc_semaphore("d%d" % self.uid), 0
            self.dma_active.append(sbuf)
        if load:
            self._deps(q, [], [sbuf])
        else:
            self._deps(q, [sbuf], [])
        inst = self.E[q].dma_start(out=out, in_=in_)
        sbuf.dcount += 16
        inst.then_inc(sbuf.dsem, 16)
        tok = (sbuf.dsem, sbuf.dcount, "dma")
        if load:
            sbuf.last_w = tok
            sbuf.reads = []
        else:
            sbuf.reads.append(tok)
        self.n_inst += 1
        return inst

    def barrier(self):
        for b in self.dma_active:
            self._wait("sp", (b.dsem, b.dcount, "dma"))
        for k in self.E:
            if k != "sp" and self.cnt[k]:
                self._wait("sp", (self.sem[k], self.cnt[k], k))
        inst = self.E["sp"].nop()
        self.cnt["sp"] += 1
        inst.then_inc(self.sem["sp"], 1)
        tok = (self.sem["sp"], self.cnt["sp"], "sp")
        for k in self.E:
            if k != "sp":
                self._wait(k, tok)

    def end_stage(self, es):
        self.barrier()
        keep = []
        for tl in self.dma_active:
            if tl in self.stage_tiles:
                self.sem_pool.append((tl.dsem, tl.dcount))
                tl.dsem = None
            else:
                keep.append(tl)
        self.dma_active = keep
        self.stage_tiles = []
        es.close()


def build(n_layers=L, debug=False, stop_after=None):
    nc = bass.Bass("TRN2", target_bir_lowering=False)
    mk = MK(nc)

    def dram(name, shape, dt=F32, kind="ExternalInput"):
        return nc.dram_tensor(name, list(shape), dt, kind=kind).ap()

    skind = "ExternalOutput" if debug else "Internal"
    h0 = dram("h0", [T, D])
    cvec = dram("cvec", [128, 16])
    w_mod = dram("w_mod", [L, D, 6144])
    w_in = dram("w_in", [L, D, NCOL])
    w_krsw = dram("w_krsw", [L, D, 64])
    w_uqx = dram("w_uqx", [L, 512, 2048])
    w_ukv = dram("w_ukv", [L, 256, 2048])
    lru_bd = dram("lru_bd", [L, 32, 128, 128])
    w_branch = dram("w_branch", [L, 3, D, D])
    w_out = dram("w_out", [L, D, D])
    wr = dram("wr", [L, D, 36])
    exp_w1 = dram("exp_w1", [L, 32, D, 512])
    exp_w3 = dram("exp_w3", [L, 32, D, 512])
    exp_w2 = dram("exp_w2", [L, 32, 512, D])
    vecs_d = dram("vecs", [L, 128, NV])
    rows_d = dram("rows", [L, NR])
    consts_d = dram("consts", [4, 128, 128])
    cs_d = dram("cossin", [2, 64, T])
    y_out = dram("y", [2048, D], F32, "ExternalOutput")
    hd = dram("hd", [T, D], F32, skind)
    cqT = dram("cqT", [512, T], BF16, skind)
    ckvT = dram("ckvT", [256, T], BF16, skind)
    krT = dram("krT", [64, T], BF16, skind)
    bcT = dram("bcT", [1024, T], BF16, skind)
    xs_tm = dram("xs_tm", [T, D], BF16, skind)
    b_tm = dram("b_tm", [T, 512], BF16, skind)
    zs = dram("zs", [T, D], BF16, skind)
    dtr = dram("dtr", [T, 32], F32, skind)
    gateT = dram("gateT", [3072, T], BF16, skind)
    attT = dram("attT", [D, T], BF16, skind)
    yssdT = dram("yssdT", [D, T], BF16, skind)
    ylruT = dram("ylruT", [D, T], BF16, skind)
    hprev = dram("hprev", [2, NT, 128, D], BF16, skind)

    ges = ExitStack()
    PSt = ges.enter_context(nc.psum_tensor("psall", [128, 4096], F32))
    PS = PSt
    PB = [mk.tracker("pb%d" % i) for i in range(8)]

    def bank(i, n=512, off=0):
        return PS[:, i * 512 + off:i * 512 + off + n]

    def bankb(i):
        return PS[:, i * 512:(i + 1) * 512].bitcast(BF16)

    cst = mk.tile(ges, "cst", [128, 4, 128], F32)
    identf = cst[:, 0, :]
    triU = cst[:, 1, :]
    triL = cst[:, 2, :]
    onesf = cst[:, 3, :]
    cstb = mk.tile(ges, "cstb", [128, 2, 128], BF16)
    identb = cstb[:, 0, :]
    onesb = cstb[:, 1, :]
    vec = mk.tile(ges, "vec", [128, NV], F32)
    srow = mk.tile(ges, "srow", [128, NR - R_ALOG], F32)
    modT = mk.tile(ges, "modT", [128, 96], F32)
    scb = mk.tile(ges, "scb", [128, 16], BF16)
    cvt = mk.tile(ges, "cvt", [128, 16], F32)
    lcs = mk.tile(ges, "lcs", [128, 16], F32)
    qnws = mk.tile(ges, "qnws", [128, 6], F32)

    mk.dma("sp", cst[:], consts_d.rearrange("c p n -> p c n"), cst)
    mk.op("dve", lambda e: e.tensor_copy(out=cstb[:, 0, :], in_=cst[:, 0, :]), [cst], [cstb])
    mk.op("dve", lambda e: e.tensor_copy(out=cstb[:, 1, :], in_=cst[:, 3, :]), [cst], [cstb])
    mk.dma("sp", cvt[:], cvec, cvt)
    mk.op("act", lambda e: e.activation(out=scb[:], in_=cvt[:], func=AF.Silu), [cvt], [scb])

    def SROW(off, n):
        return srow[:, off - R_ALOG:off - R_ALOG + n]

    def modcol(sec, c, j):
        col = (sec * 8 + c) * 2 + j
        return modT[:, col:col + 1]

    rr = {"mm": 0, "ev": 0}

    def evac_eng():
        rr["ev"] += 1
        return "act" if rr["ev"] % 2 else "dve"

    def copy(eng, out, in_, r, w):
        if eng == "act":
            mk.op("act", lambda e: e.activation(out=out, in_=in_, func=AF.Identity), r, w)
        else:
            mk.op(eng, lambda e: e.tensor_copy(out=out, in_=in_), r, w)

    def stage_mod(l):
        es = ExitStack()
        mk.dma("sp", vec[:], vecs_d[l], vec)
        mk.dma("sp", srow[:], rows_d[l, R_ALOG:NR].partition_broadcast(128), srow)
        wmb = [mk.tile(es, "wmb", [128, 8, 512], BF16) for _ in range(2)]
        wv = w_mod[l].rearrange("(k p) n -> p k n", p=128)
        for cg in range(12):
            wb = wmb[cg % 2]
            mk.dma("pool", wb[:], wv[:, :, cg * 512:(cg + 1) * 512], wb)
            for f in range(4):
                fc = cg * 4 + f
                for k in range(8):
                    mk.op("pe", lambda e: e.matmul(bank(0, 2, fc * 2), lhsT=wb[:, k, f * 128:(f + 1) * 128],
                                                   rhs=scb[:, 2 * k:2 * k + 2], start=(k == 0), stop=(k == 7)),
                          [wb, scb], [PB[0]])
        mk.op("dve", lambda e: e.tensor_tensor(out=modT[:], in0=bank(0, 96), in1=vec[:, V_BMOD:V_BMOD + 96], op=ALU.add),
              [PB[0], vec], [modT])
        for sec in (1, 4):
            mk.op("dve", lambda e: e.tensor_scalar_add(out=modT[:, sec * 16:(sec + 1) * 16], in0=modT[:, sec * 16:(sec + 1) * 16],
                                                       scalar1=1.0), [modT], [modT])
        mk.op("act", lambda e: e.activation(out=lcs[:], in_=vec[:, V_LLAM:V_LLAM + 16], func=AF.Exp, scale=-1.0), [vec], [lcs])
        mk.op("act", lambda e: e.activation(out=lcs[:], in_=lcs[:], func=AF.Ln, bias=1.0, scale=1.0), [lcs], [lcs])
        mk.op("dve", lambda e: e.tensor_scalar_mul(out=lcs[:], in0=lcs[:], scalar1=-8.0), [lcs], [lcs])
        mk.op("dve", lambda e: e.tensor_scalar_mul(out=qnws[:, 0:4], in0=vec[:, V_QNW:V_QNW + 4], scalar1=math.sqrt(512.0)), [vec], [qnws])
        mk.op("dve", lambda e: e.tensor_scalar_mul(out=qnws[:, 4:6], in0=vec[:, V_KVNW:V_KVNW + 2], scalar1=math.sqrt(256.0)), [vec], [qnws])
        mk.end_stage(es)

    def build_grow(es, sec):
        grow = mk.tile(es, "grow", [128, 2, D], F32)
        dg = [mk.tile(es, "dg", [128, 128], F32) for _ in range(2)]
        n = 0
        for j in range(2):
            for c in range(8):
                d_ = dg[n % 2]
                pb = 6 + (n % 2)
                mk.op("dve", lambda e: e.tensor_scalar_mul(out=d_[:], in0=identf, scalar1=modcol(sec, c, j)), [cst, modT], [d_])
                mk.op("pe", lambda e: e.matmul(bank(pb, 128), lhsT=onesf, rhs=d_[:], start=True, stop=True), [cst, d_], [PB[pb]])
                copy("act", grow[:, j, c * 128:(c + 1) * 128], bank(pb, 128), [PB[pb]], [grow])
                n += 1
        return grow

    def layer_norm_stats(x_ap, xt, stt, mvt):
        mk.op("dve", lambda e: e.bn_stats(out=stt[:, 0:6], in_=x_ap[:, 0:512]), [xt], [stt])
        mk.op("dve", lambda e: e.bn_stats(out=stt[:, 6:12], in_=x_ap[:, 512:1024]), [xt], [stt])
        mk.op("dve", lambda e: e.bn_aggr(out=mvt[:, 2:4], in_=stt[:]), [stt], [mvt])
        mk.op("act", lambda e: e.activation(out=mvt[:, 0:1], in_=mvt[:, 3:4], func=AF.Sqrt, bias=LN_EPS, scale=1.0), [mvt], [mvt])
        mk.op("dve", lambda e: e.reciprocal(out=mvt[:, 0:1], in_=mvt[:, 0:1]), [mvt], [mvt])
        mk.op("dve", lambda e: e.tensor_scalar(out=mvt[:, 1:2], in0=mvt[:, 2:3], scalar1=mvt[:, 0:1], scalar2=-1.0,
                                               op0=ALU.mult, op1=ALU.mult), [mvt], [mvt])

    def stage_ln_mod(es, src, sec_shift, sec_scale, uT, t0, router=None):
        hts = [mk.tile(es, "ht", [128, D], F32) for _ in range(2)]
        xns = [mk.tile(es, "xn", [128, D], F32) for _ in range(2)]
        ufs = [mk.tile(es, "uf", [128, 8, 128], F32) for _ in range(2)]
        sts = [mk.tile(es, "st", [128, 12], F32) for _ in range(2)]
        mvs = [mk.tile(es, "mv", [128, 4], F32) for _ in range(2)]
        if router is not None:
            wrt, gw = router
            rt = [mk.tile(es, "rt", [128, 160], F32) for _ in range(2)]
        tiles = list(range(t0, NT))
        mk.dma("sp", hts[0][:], src[tiles[0] * 128:(tiles[0] + 1) * 128, :], hts[0])
        for i, t in enumerate(tiles):
            ht, xn, uf, st_, mv = hts[i % 2], xns[i % 2], ufs[i % 2], sts[i % 2], mvs[i % 2]
            if i + 1 < len(tiles):
                tn = tiles[i + 1]
                mk.dma("sp", hts[(i + 1) % 2][:], src[tn * 128:(tn + 1) * 128, :], hts[(i + 1) % 2])
            j = 1 if t < 2 else 0
            layer_norm_stats(ht, ht, st_, mv)
            mk.op("act", lambda e: e.activation(out=xn[:], in_=ht[:], func=AF.Identity, scale=mv[:, 0:1], bias=mv[:, 1:2]),
                  [ht, mv], [xn])
            for half in range(2):
                pb = 4 + (rr["mm"] % 2)
                rr["mm"] += 1
                for c4 in range(4):
                    c = half * 4 + c4
                    mk.op("pe", lambda e: e.transpose(out=bank(pb, 128, c4 * 128), in_=xn[:, c * 128:(c + 1) * 128], identity=identf),
                          [xn, cst], [PB[pb]])
                for c4 in range(4):
                    c = half * 4 + c4
                    mk.op("act", lambda e: e.activation(out=uf[:, c, :], in_=bank(pb, 128, c4 * 128), func=AF.Identity,
                                                        scale=modcol(sec_scale, c, j), bias=modcol(sec_shift, c, j)),
                          [PB[pb], modT], [uf])
            mk.op("dve", lambda e: e.tensor_copy(out=uT[:, :, t * 128:(t + 1) * 128], in_=uf[:]), [uf], [uT])
            if router is not None:
                r_ = rt[i % 2]
                for k in range(8):
                    mk.op("pe", lambda e: e.matmul(bank(6, 36), lhsT=uf[:, k, :], rhs=wrt[:, k, :], start=(k == 0), stop=(k == 7)),
                          [uf, wrt], [PB[6]])
                lg = r_[:, 0:36]
                mk.op("dve", lambda e: e.tensor_tensor(out=lg, in0=bank(6, 36), in1=SROW(R_RB, 36), op=ALU.add), [PB[6], srow], [r_])
                gm = r_[:, 40:41]
                mk.op("dve", lambda e: e.reduce_max(out=gm, in_=r_[:, 0:4], axis=AX), [r_], [r_])
                ngm = r_[:, 41:42]
                mk.op("dve", lambda e: e.tensor_scalar_mul(out=ngm, in0=gm, scalar1=-1.0), [r_], [r_])
                mk.op("dve", lambda e: e.memset(r_[:, 42:43], 0.0), [], [r_])
                mk.op("act", lambda e: e.activation(out=r_[:, 44:48], in_=r_[:, 0:4], func=AF.Exp, bias=ngm, scale=1.0,
                                                    accum_out=r_[:, 42:43]), [r_], [r_])
                gval = r_[:, 43:44]
                mk.op("dve", lambda e: e.reciprocal(out=gval, in_=r_[:, 42:43]), [r_], [r_])
                gmask = r_[:, 48:52]
                mk.op("dve", lambda e: e.tensor_scalar(out=gmask, in0=r_[:, 0:4], scalar1=gm, scalar2=None, op0=ALU.is_ge), [r_], [r_])
                pen = r_[:, 52:56]
                mk.op("dve", lambda e: e.tensor_scalar(out=pen, in0=gmask, scalar1=-1.0, scalar2=1e30, op0=ALU.add, op1=ALU.mult), [r_], [r_])
                ml = r_[:, 56:88]
                mk.op("dve", lambda e: e.tensor_tensor(out=ml.rearrange("p (g x) -> p g x", x=8),
                                                       in0=r_[:, 4:36].rearrange("p (g x) -> p g x", x=8),
                                                       in1=pen.unsqueeze(2).to_broadcast([128, 4, 8]), op=ALU.add), [r_], [r_])
                m1 = r_[:, 88:89]
                mk.op("dve", lambda e: e.reduce_max(out=m1, in_=ml, axis=AX), [r_], [r_])
                oh1 = r_[:, 96:128]
                mk.op("dve", lambda e: e.tensor_scalar(out=oh1, in0=ml, scalar1=m1, scalar2=None, op0=ALU.is_ge), [r_], [r_])
                ml2 = r_[:, 128:160]
                mk.op("dve", lambda e: e.scalar_tensor_tensor(out=ml2, in0=oh1, scalar=-1e30, in1=ml, op0=ALU.mult, op1=ALU.add), [r_], [r_])
                m2 = r_[:, 89:90]
                mk.op("dve", lambda e: e.reduce_max(out=m2, in_=ml2, axis=AX), [r_], [r_])
                dd = r_[:, 90:91]
                mk.op("dve", lambda e: e.tensor_tensor(out=dd, in0=m2, in1=m1, op=ALU.subtract), [r_], [r_])
                mk.op("act", lambda e: e.activation(out=dd, in_=dd, func=AF.Exp), [r_], [r_])
                mk.op("dve", lambda e: e.tensor_scalar_add(out=dd, in0=dd, scalar1=1.0), [r_], [r_])
                mk.op("dve", lambda e: e.reciprocal(out=dd, in_=dd), [r_], [r_])
                w1_ = r_[:, 91:92]
                mk.op("dve", lambda e: e.tensor_tensor(out=w1_, in0=dd, in1=gval, op=ALU.mult), [r_], [r_])
                w2_ = r_[:, 92:93]
                mk.op("dve", lambda e: e.tensor_tensor(out=w2_, in0=gval, in1=w1_, op=ALU.subtract), [r_], [r_])
                mk.op("dve", lambda e: e.tensor_scalar(out=ml, in0=ml2, scalar1=m2, scalar2=w2_, op0=ALU.is_ge, op1=ALU.mult), [r_], [r_])
                mk.op("dve", lambda e: e.scalar_tensor_tensor(out=gw[:, t, :], in0=oh1, scalar=w1_, in1=ml, op0=ALU.mult, op1=ALU.add),
                      [r_], [gw])

    def res_ln(t, mix_ap, mix_reads, h_src, grow, lnrow, dst_ap_fn, tmps):
        ht, vt, st_, mv = tmps
        j = 1 if t < 2 else 0
        mk.op("dve", lambda e: e.tensor_tensor(out=vt[:], in0=mix_ap, in1=grow[:, j, :], op=ALU.mult), list(mix_reads) + [grow], [vt])
        mk.op("dve", lambda e: e.scalar_tensor_tensor(out=vt[:], in0=ht[:], scalar=ALPHA, in1=vt[:], op0=ALU.mult, op1=ALU.add),
              [ht, vt], [vt])
        layer_norm_stats(vt, vt, st_, mv)
        mk.op("act", lambda e: e.activation(out=vt[:], in_=vt[:], func=AF.Identity, scale=mv[:, 0:1], bias=mv[:, 1:2]), [vt, mv], [vt])
        mk.op("pool", lambda e: e.tensor_tensor(out=vt[:], in0=vt[:], in1=lnrow[:, 0, :], op=ALU.mult), [vt, lnrow], [vt])
        mk.op("pool", lambda e: e.tensor_tensor(out=vt[:], in0=vt[:], in1=lnrow[:, 1, :], op=ALU.add), [vt, lnrow], [vt])
        mk.dma("sp", dst_ap_fn(t), vt[:], vt, load=False)

    def stage_inproj(l, src):
        es = ExitStack()
        uT = mk.tile(es, "uT", [128, 8, T], BF16)
        es1 = ExitStack()
        stage_ln_mod(es1, src, 0, 1, uT, 0)
        mk.end_stage(es1)
        wv = w_in[l].rearrange("(k p) n -> p k n", p=128)
        wgs = [mk.tile(es, "wg", [128, 8, 512], BF16) for _ in range(2)]
        keep = [uT] + wgs

        def sub_end(esx):
            mk.end_stage(esx)
            mk.stage_tiles.extend(keep)
            return ExitStack()

        mk.stage_tiles.extend(keep)
        es_outer = es
        es = ExitStack()
        wgi = [0]

        def load_w(col0, n, src_v=None):
            wg = wgs[wgi[0] % 2]
            wgi[0] += 1
            v = wv if src_v is None else src_v
            mk.dma("pool", wg[:, :, 0:n], v[:, :, col0:col0 + n], wg)
            return wg

        def proj_fm(wg, c0, M, a, b, pb):
            for k in range(8):
                mk.op("pe", lambda e: e.matmul(PS[0:M, pb * 512:pb * 512 + (b - a)], lhsT=wg[:, k, c0:c0 + M], rhs=uT[:, k, a:b],
                                               start=(k == 0), stop=(k == 7)), [wg, uT], [PB[pb]])

        def next_pb():
            rr["mm"] += 1
            return rr["mm"] % 4

        raws = [mk.tile(es, "raw", [128, 4, 512], BF16) for _ in range(2)]
        sqs = [mk.tile(es, "sq", [128, 512], BF16) for _ in range(2)]
        Rt = [mk.tile(es, "Rt", [128, 512], F32) for _ in range(2)]
        outb = [mk.tile(es, "outb", [128, 4, 512], BF16) for _ in range(2)]
        for (col0, nch, dim, nwoff, dst) in ((C_CQ, 4, 512, 0, cqT), (C_CKV, 2, 256, 4, ckvT)):
            wg = load_w(col0, nch * 128)
            for bi, (a, b) in enumerate(TB):
                n = b - a
                raw, R_, ob = raws[bi % 2], Rt[bi % 2], outb[bi % 2]
                for c in range(nch):
                    pb = next_pb()
                    proj_fm(wg, c * 128, 128, a, b, pb)
                    copy("act", raw[:, c, 0:n], bank(pb, n), [PB[pb]], [raw])
                    sq = sqs[c % 2]
                    mk.op("dve", lambda e: e.tensor_tensor(out=sq[:, 0:n], in0=bank(pb, n), in1=raw[:, c, 0:n], op=ALU.mult), [PB[pb], raw], [sq])
                    mk.op("pe", lambda e: e.matmul(bank(4, n), lhsT=onesb, rhs=sq[:, 0:n], start=(c == 0), stop=(c == nch - 1)),
                          [cstb, sq], [PB[4]])
                mk.op("act", lambda e: e.activation(out=R_[:, 0:n], in_=bank(4, n), func=AF.Sqrt, bias=dim * RMS_EPS, scale=1.0), [PB[4]], [R_])
                mk.op("dve", lambda e: e.reciprocal(out=R_[:, 0:n], in_=R_[:, 0:n]), [R_], [R_])
                for c in range(nch):
                    mk.op("dve", lambda e: e.scalar_tensor_tensor(out=ob[:, c, 0:n], in0=raw[:, c, 0:n], scalar=qnws[:, nwoff + c:nwoff + c + 1],
                                                                  in1=R_[:, 0:n], op0=ALU.mult, op1=ALU.mult), [raw, qnws, R_], [ob])
                mk.dma("sp", dst.rearrange("(c p) t -> p c t", p=128)[:, :, a:b], ob[:, 0:nch, 0:n], ob, load=False)
        cs = mk.tile(es, "cs", [64, 2, T], F32)
        mk.dma("sp", cs[:], cs_d.rearrange("c p t -> p c t"), cs)
        wg = load_w(C_KR, 64)
        wg2 = load_w(0, 64, w_krsw[l].rearrange("(k p) n -> p k n", p=128))
        krb = mk.tile(es, "krb", [64, T], BF16)
        kt1 = mk.tile(es, "kt1", [64, 512], F32)
        kt2 = mk.tile(es, "kt2", [64, 512], F32)
        for bi, (a, b) in enumerate(TB):
            n = b - a
            p1 = next_pb()
            proj_fm(wg, 0, 64, a, b, p1)
            p2 = next_pb()
            proj_fm(wg2, 0, 64, a, b, p2)
            mk.op("dve", lambda e: e.tensor_tensor(out=kt1[:, 0:n], in0=PS[0:64, p1 * 512:p1 * 512 + n], in1=cs[:, 0, a:b], op=ALU.mult), [PB[p1], cs], [kt1])
            mk.op("dve", lambda e: e.tensor_tensor(out=kt2[:, 0:n], in0=PS[0:64, p2 * 512:p2 * 512 + n], in1=cs[:, 1, a:b], op=ALU.mult), [PB[p2], cs], [kt2])
            mk.op("pool", lambda e: e.tensor_tensor(out=krb[:, a:b], in0=kt1[:, 0:n], in1=kt2[:, 0:n], op=ALU.add), [kt1, kt2], [krb])
        mk.dma("sp", krT, krb[:], krb, load=False)
        gbs = [mk.tile(es, "gb", [128, T], BF16) for _ in range(2)]
        for c in range(24):
            if c % 4 == 0:
                wg = load_w(C_GATE + c * 128, 512)
            gb = gbs[c % 2]
            for bi, (a, b) in enumerate(TB):
                pb = next_pb()
                proj_fm(wg, (c % 4) * 128, 128, a, b, pb)
                mk.op("act", lambda e: e.activation(out=gb[:, a:b], in_=bank(pb, b - a), func=AF.Sigmoid), [PB[pb]], [gb])
            mk.dma("sp", gateT[c * 128:(c + 1) * 128, :], gb[:], gb, load=False)
        es = sub_end(es)
        rawf = [mk.tile(es, "rawf", [128, T], F32) for _ in range(2)]
        accf = [mk.tile(es, "accf", [128, T], F32) for _ in range(2)]
        xbb = [mk.tile(es, "xbb", [128, T], BF16) for _ in range(2)]
        tmT = [mk.tile(es, "tmT", [128, NT, 128], BF16) for _ in range(2)]

        def conv(raw, acc, wcol, bcol):
            mk.op("act", lambda e: e.activation(out=acc[:], in_=raw[:], func=AF.Identity, scale=vec[:, wcol + 2:wcol + 3], bias=vec[:, bcol:bcol + 1]),
                  [raw, vec], [acc])
            for (s0, s1) in SEQS:
                for (eng, j, oa, ob_, ia, ib) in (("dve", 0, s0 + 2, s1, s0, s1 - 2), ("dve", 1, s0 + 1, s1, s0, s1 - 1), ("dve", 3, s0, s1 - 1, s0 + 1, s1)):
                    mk.op(eng, lambda e: e.scalar_tensor_tensor(out=acc[:, oa:ob_], in0=raw[:, ia:ib], scalar=vec[:, wcol + j:wcol + j + 1],
                                                                in1=acc[:, oa:ob_], op0=ALU.mult, op1=ALU.add), [raw, vec, acc], [acc])

        for c in range(16):
            if c % 4 == 0:
                wg = load_w(C_XBC + c * 128, 512)
            raw, acc, xb = rawf[c % 2], accf[c % 2], xbb[c % 2]
            for bi, (a, b) in enumerate(TB):
                pb = next_pb()
                proj_fm(wg, (c % 4) * 128, 128, a, b, pb)
                copy(evac_eng(), raw[:, a:b], bank(pb, b - a), [PB[pb]], [raw])
            conv(raw, acc, V_SCW + c * 4, V_SCB + c)
            mk.op("act", lambda e: e.activation(out=xb[:], in_=acc[:], func=AF.Silu), [acc], [xb])
            if c >= 8:
                mk.dma("sp", bcT[(c - 8) * 128:(c - 7) * 128, :], xb[:], xb, load=False)
            if c < 12:
                tm = tmT[c % 2]
                for g0 in range(0, NT, 8):
                    g1 = min(NT, g0 + 8)
                    pb = 4 + (g0 // 8) % 2
                    for t in range(g0, g1):
                        mk.op("pe", lambda e: e.transpose(out=bankb(pb)[:, (t - g0) * 128:(t - g0 + 1) * 128], in_=xb[:, t * 128:(t + 1) * 128], identity=identb),
                              [xb, cstb], [PB[pb]])
                    copy(evac_eng(), tm[:, g0:g1, :], bankb(pb)[:, 0:(g1 - g0) * 128].rearrange("p (t c) -> p t c", c=128), [PB[pb]], [tm])
                if c < 8:
                    mk.dma("sp", xs_tm.rearrange("(t p) c -> p t c", p=128)[:, :, c * 128:(c + 1) * 128], tm[:], tm, load=False)
                else:
                    mk.dma("sp", b_tm.rearrange("(t p) c -> p t c", p=128)[:, :, (c - 8) * 128:(c - 7) * 128], tm[:], tm, load=False)
        es = sub_end(es)
        rawf = [mk.tile(es, "rawf", [128, T], F32) for _ in range(2)]
        bd = mk.tile(es, "bd", [128, 32, 128], BF16)
        mk.dma("pool", bd[:], lru_bd[l].rearrange("m p n -> p m n"), bd)
        xc = mk.tile(es, "xc", [128, T], F32)
        xcb = mk.tile(es, "xcb", [128, T], BF16)
        ra = mk.tile(es, "ra", [128, T], F32)
        ib_ = mk.tile(es, "ib", [128, T], F32)
        tq = mk.tile(es, "tq", [128, T], F32)
        hh = [mk.tile(es, "hh", [128, T], F32) for _ in range(2)]
        gg = mk.tile(es, "gg", [128, T], F32)
        yb = [mk.tile(es, "yb", [128, T], BF16) for _ in range(2)]
        for c in range(8):
            if c % 4 == 0:
                wgx = load_w(C_LX + c * 128, 512)
                wgg = load_w(C_LG + c * 128, 512)
            raw = rawf[c % 2]
            for bi, (a, b) in enumerate(TB):
                pb = next_pb()
                proj_fm(wgx, (c % 4) * 128, 128, a, b, pb)
                copy(evac_eng(), raw[:, a:b], bank(pb, b - a), [PB[pb]], [raw])
            conv(raw, xc, V_LCW + c * 4, V_LCB + c)
            mk.op("pool", lambda e: e.tensor_copy(out=xcb[:], in_=xc[:]), [xc], [xcb])
            for d in range(2):
                for gi, (gt, boff) in enumerate(((ra, V_LBA), (ib_, V_LBX))):
                    m = (d * 2 + gi) * 8 + c
                    for bi, (a, b) in enumerate(TB):
                        pb = next_pb()
                        mk.op("pe", lambda e: e.matmul(bank(pb, b - a), lhsT=bd[:, m, :], rhs=xcb[:, a:b], start=True, stop=True), [bd, xcb], [PB[pb]])
                        mk.op("act", lambda e: e.activation(out=gt[:, a:b], in_=bank(pb, b - a), func=AF.Sigmoid,
                                                            bias=vec[:, boff + d * 8 + c:boff + d * 8 + c + 1], scale=1.0), [PB[pb], vec], [gt])
                mk.op("act", lambda e: e.activation(out=ra[:], in_=ra[:], func=AF.Exp, scale=lcs[:, d * 8 + c:d * 8 + c + 1]), [ra, lcs], [ra])
                mk.op("pool", lambda e: e.tensor_tensor(out=tq[:], in0=ra[:], in1=ra[:], op=ALU.mult), [ra], [tq])
                mk.op("act", lambda e: e.activation(out=tq[:], in_=tq[:], func=AF.Sqrt, bias=1.0, scale=-1.0), [tq], [tq])
                mk.op("dve", lambda e: e.tensor_tensor(out=ib_[:], in0=ib_[:], in1=tq[:], op=ALU.mult), [ib_, tq], [ib_])
                mk.op("dve", lambda e: e.tensor_tensor(out=ib_[:], in0=ib_[:], in1=xc[:], op=ALU.mult), [ib_, xc], [ib_])
                h_ = hh[d]
                if d == 0:
                    mk.op("dve", lambda e: e.tensor_tensor_scan(out=h_[:, 0:256], data0=ra[:, 0:256], data1=ib_[:, 0:256], initial=0.0,
                                                                op0=ALU.mult, op1=ALU.add), [ra, ib_], [h_])
                    mk.op("dve", lambda e: e.tensor_tensor_scan(out=h_[:, 256:T], data0=ra[:, 256:T], data1=ib_[:, 256:T], initial=h_[:, 255:256],
                                                                op0=ALU.mult, op1=ALU.add), [ra, ib_, h_], [h_])
                else:
                    mk.op("dve", lambda e: e.tensor_tensor_scan(out=h_[:, 0:256][:, ::-1], data0=ra[:, 0:256][:, ::-1], data1=ib_[:, 0:256][:, ::-1],
                                                                initial=0.0, op0=ALU.mult, op1=ALU.add), [ra, ib_], [h_])
                    mk.op("dve", lambda e: e.tensor_tensor_scan(out=h_[:, 256:T][:, ::-1], data0=ra[:, 256:T][:, ::-1], data1=ib_[:, 256:T][:, ::-1],
                                                                initial=h_[:, 0:1], op0=ALU.mult, op1=ALU.add), [ra, ib_, h_], [h_])
            for bi, (a, b) in enumerate(TB):
                pb = next_pb()
                proj_fm(wgg, (c % 4) * 128, 128, a, b, pb)
                mk.op("act", lambda e: e.activation(out=gg[:, a:b], in_=bank(pb, b - a), func=AF.Gelu_apprx_tanh), [PB[pb]], [gg])
            mk.op("pool", lambda e: e.tensor_tensor(out=hh[0][:], in0=hh[0][:], in1=hh[1][:], op=ALU.add), [hh[0], hh[1]], [hh[0]])
            y_ = yb[c % 2]
            mk.op("dve", lambda e: e.tensor_tensor(out=y_[:], in0=hh[0][:], in1=gg[:], op=ALU.mult), [hh[0], gg], [y_])
            mk.dma("sp", ylruT[c * 128:(c + 1) * 128, :], y_[:], y_, load=False)
        es = sub_end(es)
        wz = mk.tile(es, "wz", [128, 8, 1024], BF16)
        mk.dma("pool", wz[:], wv[:, :, C_Z:C_Z + 1024], wz)
        wdt = mk.tile(es, "wdt", [128, 8, 32], BF16)
        mk.dma("pool", wdt[:], wv[:, :, C_DT:C_DT + 32], wdt)
        dta = mk.tile(es, "dta", [128, NT, 32], F32)
        zts = [mk.tile(es, "zt", [128, D], BF16) for _ in range(2)]
        for t in range(NT):
            zt = zts[t % 2]
            for half in range(2):
                pb = next_pb()
                for k in range(8):
                    mk.op("pe", lambda e: e.matmul(bank(pb), lhsT=uT[:, k, t * 128:(t + 1) * 128], rhs=wz[:, k, half * 512:(half + 1) * 512],
                                                   start=(k == 0), stop=(k == 7)), [uT, wz], [PB[pb]])
                mk.op("act", lambda e: e.activation(out=zt[:, half * 512:(half + 1) * 512], in_=bank(pb), func=AF.Silu), [PB[pb]], [zt])
            mk.dma("sp", zs[t * 128:(t + 1) * 128, :], zt[:], zt, load=False)
            pb = 4 + t % 2
            for k in range(8):
                mk.op("pe", lambda e: e.matmul(bank(pb, 32), lhsT=uT[:, k, t * 128:(t + 1) * 128], rhs=wdt[:, k, :], start=(k == 0), stop=(k == 7)),
                      [uT, wdt], [PB[pb]])
            mk.op("dve", lambda e: e.tensor_copy(out=dta[:, t, :], in_=bank(pb, 32)), [PB[pb]], [dta])
        mk.dma("sp", dtr.rearrange("(t p) c -> p t c", p=128), dta[:], dta, load=False)
        mk.end_stage(es)
        mk.end_stage(es_outer)

    def stage_mla(l, last):
        es = ExitStack()
        cqn = mk.tile(es, "cqn", [128, 4, T], BF16)
        ckvn = mk.tile(es, "ckvn", [128, 2, T], BF16)
        krb = mk.tile(es, "krb", [64, T], BF16)
        cs = mk.tile(es, "cs", [64, 2, T], F32)
        mk.dma("sp", cqn[:], cqT.rearrange("(c p) t -> p c t", p=128), cqn)
        mk.dma("sp", ckvn[:], ckvT.rearrange("(c p) t -> p c t", p=128), ckvn)
        mk.dma("sp", krb[:], krT, krb)
        mk.dma("sp", cs[:], cs_d.rearrange("c p t -> p c t"), cs)
        wqs = [mk.tile(es, "wq", [128, 4, 256], BF16) for _ in range(2)]
        wks = [mk.tile(es, "wk", [128, 2, 256], BF16) for _ in range(2)]
        qn = mk.tile(es, "qn", [128, T], BF16)
        qr = mk.tile(es, "qr", [64, T], BF16)
        kn = mk.tile(es, "kn", [128, T], BF16)
        vh = mk.tile(es, "vh", [128, NT, 128], BF16)
        kt1 = mk.tile(es, "kt1", [64, 512], F32)
        kt2 = mk.tile(es, "kt2", [64, 512], F32)
        Ps = [mk.tile(es, "P", [128, T], BF16) for _ in range(2)]
        PTs = [mk.tile(es, "PT", [128, NT, 128], BF16) for _ in range(2)]
        sm = [mk.tile(es, "sm", [128, 16], F32) for _ in range(2)]
        PB7b = mk.tracker("pb7b")
        PTg = [[mk.tracker("ptg%d_%d" % (i_, g_)) for g_ in range(3)] for i_ in range(2)]
        ob = [mk.tile(es, "ob", [128, 128], BF16) for _ in range(2)]
        aT = [mk.tile(es, "aT", [128, T], BF16) for _ in range(2)]
        wqv = w_uqx[l].rearrange("(k p) n -> p k n", p=128)
        wkv = w_ukv[l].rearrange("(k p) n -> p k n", p=128)
        t0 = 2 if last else 0
        it = 0
        for h in range(8):
            wq, wk = wqs[h % 2], wks[h % 2]
            mk.dma("pool", wq[:], wqv[:, :, h * 256:(h + 1) * 256], wq)
            mk.dma("pool", wk[:], wkv[:, :, h * 256:(h + 1) * 256], wk)
            for bi, (a, b) in enumerate(TB):
                n = b - a
                for k in range(4):
                    mk.op("pe", lambda e: e.matmul(bank(5, n), lhsT=wq[:, k, 0:128], rhs=cqn[:, k, a:b], start=(k == 0), stop=(k == 3)), [wq, cqn], [PB[5]])
                copy("act", qn[:, a:b], bank(5, n), [PB[5]], [qn])
                for k in range(4):
                    mk.op("pe", lambda e: e.matmul(PS[0:64, 6 * 512:6 * 512 + n], lhsT=wq[:, k, 128:192], rhs=cqn[:, k, a:b], start=(k == 0), stop=(k == 3)), [wq, cqn], [PB[6]])
                for k in range(4):
                    mk.op("pe", lambda e: e.matmul(PS[0:64, 7 * 512:7 * 512 + n], lhsT=wq[:, k, 192:256], rhs=cqn[:, k, a:b], start=(k == 0), stop=(k == 3)), [wq, cqn], [PB[7]])
                mk.op("dve", lambda e: e.tensor_tensor(out=kt1[:, 0:n], in0=PS[0:64, 6 * 512:6 * 512 + n], in1=cs[:, 0, a:b], op=ALU.mult), [PB[6], cs], [kt1])
                mk.op("dve", lambda e: e.tensor_tensor(out=kt2[:, 0:n], in0=PS[0:64, 7 * 512:7 * 512 + n], in1=cs[:, 1, a:b], op=ALU.mult), [PB[7], cs], [kt2])
                mk.op("pool", lambda e: e.tensor_tensor(out=qr[:, a:b], in0=kt1[:, 0:n], in1=kt2[:, 0:n], op=ALU.add), [kt1, kt2], [qr])
                for k in range(2):
                    mk.op("pe", lambda e: e.matmul(bank(5, n), lhsT=wk[:, k, 0:128], rhs=ckvn[:, k, a:b], start=(k == 0), stop=(k == 1)), [wk, ckvn], [PB[5]])
                copy("dve", kn[:, a:b], bank(5, n), [PB[5]], [kn])
            for g0 in range(0, NT, 4):
                g1 = min(NT, g0 + 4)
                pb = 6 + (g0 // 4) % 2
                for t in range(g0, g1):
                    for k in range(2):
                        mk.op("pe", lambda e: e.matmul(bank(pb, 128, (t - g0) * 128), lhsT=ckvn[:, k, t * 128:(t + 1) * 128], rhs=wk[:, k, 128:256],
                                                       start=(k == 0), stop=(k == 1)), [ckvn, wk], [PB[pb]])
                copy(evac_eng(), vh[:, g0:g1, :], bank(pb, (g1 - g0) * 128).rearrange("p (t c) -> p t c", c=128), [PB[pb]], [vh])
            a_ = aT[h % 2]
            units = list(range(t0, NT))

            def unit_bufs(i):
                return Ps[i % 2], PTs[i % 2], sm[i % 2], ob[i % 2]

            pending = []

            def emit_S(i):
                t = units[i]
                nk = 256 if t < 2 else T
                P_, PT_, s_, o_ = unit_bufs(i)
                nb = (nk + 511) // 512
                for kb, (a, b) in enumerate(TB):
                    if a >= nk:
                        break
                    b = min(b, nk)
                    mk.op("pe", lambda e: e.matmul(PS[:, a:b], lhsT=qn[:, t * 128:(t + 1) * 128], rhs=kn[:, a:b], start=True, stop=False), [qn, kn], [PB[kb]])
                    mk.op("pe", lambda e: e.matmul(PS[:, a:b], lhsT=qr[:, t * 128:(t + 1) * 128], rhs=krb[:, a:b], start=False, stop=True), [qr, krb], [PB[kb]])
                    mk.op("dve", lambda e: e.reduce_max(out=s_[:, kb:kb + 1], in_=PS[:, a:b], axis=AX), [PB[kb]], [s_])
                if nb > 1:
                    mk.op("dve", lambda e: e.reduce_max(out=s_[:, 5:6], in_=s_[:, 0:nb], axis=AX), [s_], [s_])
                    mx = s_[:, 5:6]
                else:
                    mx = s_[:, 0:1]
                mk.op("dve", lambda e: e.tensor_scalar_mul(out=s_[:, 6:7], in0=mx, scalar1=-ATTN_SCALE), [s_], [s_])
                mk.op("dve", lambda e: e.memset(s_[:, 8:13], 0.0), [], [s_])

            def emit_exp(i):
                t = units[i]
                nk = 256 if t < 2 else T
                P_, PT_, s_, o_ = unit_bufs(i)
                for kb, (a, b) in enumerate(TB):
                    if a >= nk:
                        break
                    b = min(b, nk)
                    mk.op("act", lambda e: e.activation(out=P_[:, a:b], in_=PS[:, a:b], func=AF.Exp, bias=s_[:, 6:7], scale=ATTN_SCALE,
                                                        accum_out=s_[:, 8 + kb:9 + kb]), [PB[kb], s_], [P_, s_])

            def emit_T(i):
                t = units[i]
                nk = 256 if t < 2 else T
                nkt = nk // 128
                P_, PT_, s_, o_ = unit_bufs(i)
                for g0 in range(0, nkt, 8):
                    g1 = min(nkt, g0 + 8)
                    pb = 5 + (g0 // 8) % 2
                    for kt in range(g0, g1):
                        mk.op("pe", lambda e: e.transpose(out=bankb(pb)[:, (kt - g0) * 128:(kt - g0 + 1) * 128], in_=P_[:, kt * 128:(kt + 1) * 128], identity=identb),
                              [P_, cstb], [PB[pb]])
                    copy("dve" if (g0 // 8) % 2 == 0 else "act", PT_[:, g0:g1, :], bankb(pb)[:, 0:(g1 - g0) * 128].rearrange("p (t c) -> p t c", c=128), [PB[pb]], [PTg[i % 2][g0 // 8]])

            def emit_OT():
                while pending:
                    t_, o2 = pending.pop(0)
                    mk.op("pe", lambda e: e.transpose(out=bankb(7)[:, 512:640], in_=o2[:], identity=identb), [o2, cstb], [PB7b])
                    copy("dve", a_[:, t_ * 128:(t_ + 1) * 128], bankb(7)[:, 512:640], [PB7b], [a_])

            def emit_PV(i):
                t = units[i]
                nk = 256 if t < 2 else T
                nkt = nk // 128
                P_, PT_, s_, o_ = unit_bufs(i)
                for kt in range(nkt):
                    mk.op("pe", lambda e: e.matmul(bank(7, 128), lhsT=PT_[:, kt, :], rhs=vh[:, kt, :], start=(kt == 0), stop=(kt == nkt - 1)), [PTg[i % 2][kt // 8], vh], [PB[7]])
                mk.op("dve", lambda e: e.reduce_sum(out=s_[:, 13:14], in_=s_[:, 8:13], axis=AX), [s_], [s_])
                mk.op("dve", lambda e: e.reciprocal(out=s_[:, 14:15], in_=s_[:, 13:14]), [s_], [s_])
                mk.op("act", lambda e: e.activation(out=o_[:], in_=bank(7, 128), func=AF.Identity, scale=s_[:, 14:15]), [PB[7], s_], [o_])
                pending.append((t, o_))

            emit_S(0)
            emit_exp(0)
            for i in range(len(units)):
                if i + 1 < len(units):
                    emit_S(i + 1)
                emit_OT()
                emit_T(i)
                if i + 1 < len(units):
                    emit_exp(i + 1)
                emit_PV(i)
            emit_OT()
            mk.dma("sp", attT[h * 128:(h + 1) * 128, t0 * 128:T], a_[:, t0 * 128:T], a_, load=False)
        mk.end_stage(es)

    def stage_ssd(l, last):
        es = ExitStack()
        t0 = 2 if last else 0
        dt = mk.tile(es, "dt", [128, NT, 32], F32)
        dta = mk.tile(es, "dtA", [128, NT, 32], F32)
        ndta = mk.tile(es, "ndta", [128, NT, 32], F32)
        tmpa = mk.tile(es, "tmpa", [128, NT, 32], F32)
        aneg = mk.tile(es, "aneg", [128, 32], F32)
        mk.dma("sp", dt[:], dtr.rearrange("(t p) c -> p t c", p=128), dt)
        bias_b = SROW(R_DTB, 32).unsqueeze(1).to_broadcast([128, NT, 32])
        mk.op("dve", lambda e: e.tensor_tensor(out=dt[:], in0=dt[:], in1=bias_b, op=ALU.add), [dt, srow], [dt])
        mk.op("act", lambda e: e.activation(out=tmpa[:], in_=dt[:], func=AF.Abs), [dt], [tmpa])
        mk.op("act", lambda e: e.activation(out=tmpa[:], in_=tmpa[:], func=AF.Exp, scale=-1.0), [tmpa], [tmpa])
        mk.op("act", lambda e: e.activation(out=tmpa[:], in_=tmpa[:], func=AF.Ln, bias=1.0, scale=1.0), [tmpa], [tmpa])
        mk.op("dve", lambda e: e.tensor_scalar_max(out=dt[:], in0=dt[:], scalar1=0.0), [dt], [dt])
        mk.op("dve", lambda e: e.tensor_tensor(out=dt[:], in0=dt[:], in1=tmpa[:], op=ALU.add), [dt, tmpa], [dt])
        mk.op("act", lambda e: e.activation(out=aneg[:], in_=SROW(R_ALOG, 32), func=AF.Exp), [srow], [aneg])
        mk.op("dve", lambda e: e.tensor_scalar_mul(out=aneg[:], in0=aneg[:], scalar1=-1.0), [aneg], [aneg])
        mk.op("dve", lambda e: e.tensor_tensor(out=dta[:], in0=dt[:], in1=aneg[:].unsqueeze(1).to_broadcast([128, NT, 32]), op=ALU.mult), [dt, aneg], [dta])
        mk.op("dve", lambda e: e.tensor_scalar_mul(out=ndta[:], in0=dta[:], scalar1=-1.0), [dta], [ndta])
        tri = [triU, triL]

        def small_stats(t, d, sst):
            mk.op("pe", lambda e: e.matmul(bank(7, 16), lhsT=tri[d], rhs=dta[:, t, d * 16:(d + 1) * 16], start=True, stop=True), [cst, dta], [PB[7]])
            mk.op("pe", lambda e: e.matmul(bank(7, 16, 16), lhsT=onesf, rhs=dta[:, t, d * 16:(d + 1) * 16], start=True, stop=True), [cst, dta], [PB[7]])
            copy("act", sst[:, 0:32], bank(7, 32), [PB[7]], [sst])

        xss = [mk.tile(es, "xs", [128, D], BF16) for _ in range(2)]
        bts = [mk.tile(es, "bt", [128, 512], BF16) for _ in range(2)]
        ssts = [mk.tile(es, "sst", [128, 64], F32) for _ in range(2)]
        xgd = [mk.tile(es, "xgd", [128, D], BF16) for _ in range(2)]
        hbf = [mk.tile(es, "hbf", [128, D], BF16) for _ in range(2)]
        St = mk.tile(es, "St", [128, D], F32)
        n1 = 0
        for d in range(2):
            order = list(range(NT)) if d == 0 else [1, 0] + list(range(NT - 1, 1, -1))
            mk.op("pool", lambda e: e.memset(St[:], 0.0), [], [St])
            for t in order:
                xs_, bt_, sst, xg_, hb_ = xss[n1 % 2], bts[n1 % 2], ssts[n1 % 2], xgd[n1 % 2], hbf[n1 % 2]
                n1 += 1
                mk.dma("sp", xs_[:], xs_tm[t * 128:(t + 1) * 128, :], xs_)
                mk.dma("sp", bt_[:], b_tm[t * 128:(t + 1) * 128, :], bt_)
                copy("act", hb_[:], St[:], [St], [hb_])
                mk.dma("sp", hprev[d, t], hb_[:], hb_, load=False)
                small_stats(t, d, sst)
                mk.op("dve", lambda e: e.tensor_tensor(out=sst[:, 32:48], in0=sst[:, 16:32], in1=sst[:, 0:16], op=ALU.subtract), [sst], [sst])
                mk.op("act", lambda e: e.activation(out=sst[:, 32:48], in_=sst[:, 32:48], func=AF.Exp), [sst], [sst])
                mk.op("dve", lambda e: e.tensor_tensor(out=sst[:, 32:48], in0=sst[:, 32:48], in1=dt[:, t, d * 16:(d + 1) * 16], op=ALU.mult), [sst, dt], [sst])
                mk.op("act", lambda e: e.activation(out=sst[:, 48:64], in_=sst[:, 16:32], func=AF.Exp), [sst], [sst])
                mk.op("dve", lambda e: e.tensor_tensor(out=xg_[:].rearrange("p (h j) -> p h j", j=64), in0=xs_[:].rearrange("p (h j) -> p h j", j=64),
                                                       in1=sst[:, 32:48].unsqueeze(2).to_broadcast([128, 16, 64]), op=ALU.mult), [xs_, sst], [xg_])
                for g in range(4):
                    pb = g // 2
                    mk.op("pe", lambda e: e.matmul(bank(pb, 256, (g % 2) * 256), lhsT=bt_[:, g * 128:(g + 1) * 128], rhs=xg_[:, g * 256:(g + 1) * 256],
                                                   start=True, stop=True), [bt_, xg_], [PB[pb]])
                mk.op("pool", lambda e: e.tensor_tensor(out=St[:].rearrange("p (h j) -> p h j", j=64), in0=St[:].rearrange("p (h j) -> p h j", j=64),
                                                        in1=sst[:, 48:64].unsqueeze(2).to_broadcast([128, 16, 64]), op=ALU.mult), [St, sst], [St])
                mk.op("dve", lambda e: e.tensor_tensor(out=St[:], in0=St[:], in1=PS[:, 0:1024], op=ALU.add), [St, PB[0], PB[1]], [St])
        mk.barrier()
        cts = [mk.tile(es, "ct", [128, 8, 128], BF16) for _ in range(2)]
        hps = [mk.tile(es, "hp", [128, 2, D], BF16) for _ in range(2)]
        zts = [mk.tile(es, "zt", [128, D], BF16) for _ in range(2)]
        cbm = mk.tile(es, "cbm", [128, 2, 512], BF16)
        rhs1 = mk.tile(es, "rhs1", [128, 16, 128], F32)
        rhs2 = mk.tile(es, "rhs2", [128, 16, 128], F32)
        Lm = mk.tile(es, "Lm", [128, 8, 128], F32)
        MT = mk.tile(es, "MT", [128, 16, 128], BF16)
        xg = mk.tile(es, "xg", [128, D], BF16)
        yo = mk.tile(es, "yo", [128, D], F32)
        yt = mk.tile(es, "yt", [128, D], F32)
        ynb = mk.tile(es, "ynb", [128, D], BF16)
        ys = [mk.tile(es, "ys", [128, 8, 128], BF16) for _ in range(2)]
        sq_ = mk.tile(es, "sq", [128, 256], F32)
        g4 = mk.tile(es, "g4", [128, 8], F32)
        bcv = bcT.rearrange("(c p) t -> p c t", p=128)
        for i, t in enumerate(range(t0, NT)):
            xs_, ct, hp, zt, sst, y_s = xss[i % 2], cts[i % 2], hps[i % 2], zts[i % 2], ssts[i % 2], ys[i % 2]
            mk.dma("sp", xs_[:], xs_tm[t * 128:(t + 1) * 128, :], xs_)
            mk.dma("sp", ct[:], bcv[:, :, t * 128:(t + 1) * 128], ct)
            mk.dma("sp", hp[:], hprev[:, t].rearrange("d p n -> p d n"), hp)
            mk.dma("sp", zt[:], zs[t * 128:(t + 1) * 128, :], zt)
            for g in range(4):
                mk.op("pe", lambda e: e.matmul(bank(0, 128, g * 128), lhsT=ct[:, g, :], rhs=ct[:, 4 + g, :], start=True, stop=True), [ct], [PB[0]])
            for d in range(2):
                mk.op("dve", lambda e: e.tensor_tensor(out=cbm[:, d, :].rearrange("p (g q) -> p g q", q=128), in0=bank(0).rearrange("p (g q) -> p g q", q=128),
                                                       in1=tri[d].unsqueeze(1).to_broadcast([128, 4, 128]), op=ALU.mult), [PB[0], cst], [cbm])
            for d in range(2):
                dsl = dta[:, t, d * 16:(d + 1) * 16]
                mk.op("dve", lambda e: e.tensor_tensor(out=rhs1[:], in0=tri[d].unsqueeze(1).to_broadcast([128, 16, 128]),
                                                       in1=dsl.unsqueeze(2).to_broadcast([128, 16, 128]), op=ALU.mult), [cst, dta], [rhs1])
                mk.op("pool", lambda e: e.tensor_copy(out=rhs2[:], in_=ndta[:, t, d * 16:(d + 1) * 16].unsqueeze(2).to_broadcast([128, 16, 128])), [ndta], [rhs2])
                small_stats(t, d, sst)
                mk.op("act", lambda e: e.activation(out=sst[:, 32:48], in_=sst[:, 0:16], func=AF.Exp), [sst], [sst])
                mk.op("dve", lambda e: e.tensor_tensor(out=xg[:].rearrange("p (h j) -> p h j", j=64), in0=xs_[:].rearrange("p (h j) -> p h j", j=64),
                                                       in1=dt[:, t, d * 16:(d + 1) * 16].unsqueeze(2).to_broadcast([128, 16, 64]), op=ALU.mult), [xs_, dt], [xg])
                for hf in range(2):
                    for q4 in range(2):
                        pb = 1 + q4
                        hs = hf * 8 + q4 * 4
                        mk.op("pe", lambda e: e.matmul(bank(pb), lhsT=onesf, rhs=rhs1[:, hs:hs + 4, :], start=True, stop=False), [cst, rhs1], [PB[pb]])
                        mk.op("pe", lambda e: e.matmul(bank(pb), lhsT=tri[d], rhs=rhs2[:, hs:hs + 4, :], start=False, stop=True), [cst, rhs2], [PB[pb]])
                    mk.op("dve", lambda e: e.tensor_scalar_min(out=Lm[:].rearrange("p h q -> p (h q)"), in0=PS[:, 512:1536], scalar1=0.0), [PB[1], PB[2]], [Lm])
                    mk.op("act", lambda e: e.activation(out=Lm[:], in_=Lm[:], func=AF.Exp), [Lm], [Lm])
                    mk.op("pool", lambda e: e.tensor_tensor(out=MT[:, hf * 8:(hf + 1) * 8, :].rearrange("p (g x) q -> p g x q", x=4),
                                                            in0=Lm[:].rearrange("p (g x) q -> p g x q", x=4),
                                                            in1=cbm[:, d, hf * 256:(hf + 1) * 256].rearrange("p (g q) -> p g q", q=128).unsqueeze(2).to_broadcast([128, 2, 4, 128]),
                                                            op=ALU.mult), [Lm, cbm], [MT])
                for h in range(16):
                    pb = 3 + h // 8
                    mk.op("pe", lambda e: e.matmul(bank(pb, 64, (h % 8) * 64), lhsT=MT[:, h, :], rhs=xg[:, h * 64:(h + 1) * 64], start=(d == 0 and h % 8 == 0), stop=(d == 1), skip_group_check=True),
                          [MT, xg], [PB[pb]])
                for g in range(4):
                    pb = 5 + g // 2
                    mk.op("pe", lambda e: e.matmul(bank(pb, 256, (g % 2) * 256), lhsT=ct[:, 4 + g, :], rhs=hp[:, d, g * 256:(g + 1) * 256], start=True, stop=True),
                          [ct, hp], [PB[pb]])
                if d == 0:
                    mk.op("dve", lambda e: e.tensor_tensor(out=yo[:].rearrange("p (h j) -> p h j", j=64), in0=PS[:, 2560:3584].rearrange("p (h j) -> p h j", j=64),
                                                           in1=sst[:, 32:48].unsqueeze(2).to_broadcast([128, 16, 64]), op=ALU.mult), [PB[5], PB[6], sst], [yo])
                else:
                    mk.op("dve", lambda e: e.tensor_tensor(out=yt[:].rearrange("p (h j) -> p h j", j=64), in0=PS[:, 2560:3584].rearrange("p (h j) -> p h j", j=64),
                                                           in1=sst[:, 32:48].unsqueeze(2).to_broadcast([128, 16, 64]), op=ALU.mult), [PB[5], PB[6], sst], [yt])
            mk.op("pool", lambda e: e.tensor_tensor(out=yo[:], in0=yo[:], in1=yt[:], op=ALU.add), [yo, yt], [yo])
            mk.op("dve", lambda e: e.tensor_tensor(out=yt[:], in0=PS[:, 1536:2560], in1=yo[:], op=ALU.add), [PB[3], PB[4], yo], [yt])
            mk.op("pool", lambda e: e.tensor_tensor(out=yo[:].rearrange("p (h j) -> p h j", j=64), in0=xs_[:].rearrange("p (h j) -> p h j", j=64),
                                                    in1=SROW(R_DSK, 16).unsqueeze(2).to_broadcast([128, 16, 64]), op=ALU.mult), [xs_, srow], [yo])
            mk.op("dve", lambda e: e.tensor_tensor(out=yt[:], in0=yt[:], in1=yo[:], op=ALU.add), [yt, yo], [yt])
            mk.op("dve", lambda e: e.tensor_tensor(out=yt[:], in0=yt[:], in1=zt[:], op=ALU.mult), [yt, zt], [yt])
            mk.op("pool", lambda e: e.memset(g4[:], 0.0), [], [g4])
            for g in range(4):
                mk.op("act", lambda e: e.activation(out=sq_[:], in_=yt[:, g * 256:(g + 1) * 256], func=AF.Square, accum_out=g4[:, g:g + 1]), [yt], [sq_, g4])
            mk.op("act", lambda e: e.activation(out=g4[:, 4:8], in_=g4[:, 0:4], func=AF.Sqrt, bias=RMS_EPS, scale=1.0 / 256.0), [g4], [g4])
            mk.op("dve", lambda e: e.reciprocal(out=g4[:, 4:8], in_=g4[:, 4:8]), [g4], [g4])
            mk.op("dve", lambda e: e.tensor_tensor(out=ynb[:].rearrange("p (g j) -> p g j", j=256), in0=yt[:].rearrange("p (g j) -> p g j", j=256),
                                                   in1=g4[:, 4:8].unsqueeze(2).to_broadcast([128, 4, 256]), op=ALU.mult), [yt, g4], [ynb])
            for c in range(8):
                mk.op("pe", lambda e: e.transpose(out=bankb(7)[:, c * 128:(c + 1) * 128], in_=ynb[:, c * 128:(c + 1) * 128], identity=identb), [ynb, cstb], [PB[7]])
            for c in range(8):
                mk.op("act", lambda e: e.activation(out=y_s[:, c, :], in_=bankb(7)[:, c * 128:(c + 1) * 128], func=AF.Identity,
                                                    scale=vec[:, V_SNW + c:V_SNW + c + 1]), [PB[7], vec], [y_s])
            mk.dma("sp", yssdT.rearrange("(c p) t -> p c t", p=128)[:, :, t * 128:(t + 1) * 128], y_s[:], y_s, load=False)
        mk.end_stage(es)

    def stage_merge(l, last, src, dst_fn):
        es = ExitStack()
        t0 = 2 if last else 0
        c0 = t0 * 128
        mT = mk.tile(es, "mT", [128, 8, T], BF16)
        brs = [mk.tile(es, "br", [128, 8, T], BF16) for _ in range(2)]
        wbs = [mk.tile(es, "wb", [128, 8, 512], BF16) for _ in range(2)]
        gts = [mk.tile(es, "gt", [128, T], BF16) for _ in range(2)]
        tmp = [mk.tile(es, "tmp", [128, 512], BF16) for _ in range(2)]
        srcs = [attT, yssdT, ylruT]
        n = 0
        for br in range(3):
            b_ = brs[br % 2]
            mk.dma("sp", b_[:, :, c0:T], srcs[br].rearrange("(c p) t -> p c t", p=128)[:, :, c0:T], b_)
            wv = w_branch[l, br].rearrange("(k p) n -> p k n", p=128)
            for dc in range(8):
                if dc % 4 == 0:
                    wb = wbs[n % 2]
                    n += 1
                    mk.dma("pool", wb[:], wv[:, :, dc * 128:dc * 128 + 512], wb)
                gt = gts[dc % 2]
                mk.dma("sp", gt[:, c0:T], gateT[(br * 8 + dc) * 128:(br * 8 + dc + 1) * 128, c0:T], gt)
                for bi, (a, b) in enumerate(TB):
                    a = max(a, c0)
                    if a >= b:
                        continue
                    rr["mm"] += 1
                    pb = rr["mm"] % 4
                    for k in range(8):
                        mk.op("pe", lambda e: e.matmul(bank(pb, b - a), lhsT=wb[:, k, (dc % 4) * 128:(dc % 4 + 1) * 128], rhs=b_[:, k, a:b],
                                                       start=(k == 0), stop=(k == 7)), [wb, b_], [PB[pb]])
                    if br == 0:
                        mk.op("dve", lambda e: e.tensor_tensor(out=mT[:, dc, a:b], in0=bank(pb, b - a), in1=gt[:, a:b], op=ALU.mult), [PB[pb], gt], [mT])
                    else:
                        tp = tmp[bi % 2]
                        mk.op("dve", lambda e: e.tensor_tensor(out=tp[:, 0:b - a], in0=bank(pb, b - a), in1=gt[:, a:b], op=ALU.mult), [PB[pb], gt], [tp])
                        mk.op("pool", lambda e: e.tensor_tensor(out=mT[:, dc, a:b], in0=mT[:, dc, a:b], in1=tp[:, 0:b - a], op=ALU.add), [mT, tp], [mT])
        wo = mk.tile(es, "wo", [128, 8, D], BF16)
        mk.dma("pool", wo[:], w_out[l].rearrange("(k p) n -> p k n", p=128), wo)
        grow = build_grow(es, 2)
        lnrow = mk.tile(es, "lnrow", [128, 2, D], F32)
        mk.dma("sp", lnrow[:], rows_d[l, R_LN1G:R_LN1G + 2048].rearrange("(a n) -> a n", a=2).partition_broadcast(128), lnrow)
        hts = [mk.tile(es, "ht", [128, D], F32) for _ in range(2)]
        vts = [mk.tile(es, "vt", [128, D], F32) for _ in range(2)]
        sts = [mk.tile(es, "st", [128, 12], F32) for _ in range(2)]
        mvs = [mk.tile(es, "mv", [128, 4], F32) for _ in range(2)]
        for i, t in enumerate(range(t0, NT)):
            ht = hts[i % 2]
            mk.dma("sp", ht[:], src[t * 128:(t + 1) * 128, :], ht)
            pbs = (4, 5) if i % 2 == 0 else (6, 7)
            for half in range(2):
                pb = pbs[half]
                for dc in range(8):
                    mk.op("pe", lambda e: e.matmul(bank(pb), lhsT=mT[:, dc, t * 128:(t + 1) * 128], rhs=wo[:, dc, half * 512:(half + 1) * 512],
                                                   start=(dc == 0), stop=(dc == 7)), [mT, wo], [PB[pb]])
            res_ln(t, PS[:, pbs[0] * 512:pbs[0] * 512 + 1024], [PB[pbs[0]], PB[pbs[1]]], src, grow, lnrow, dst_fn, (ht, vts[i % 2], sts[i % 2], mvs[i % 2]))
        mk.end_stage(es)

    def stage_moe(l, last, src, dst_fn):
        es = ExitStack()
        t0 = 2 if last else 0
        c0 = t0 * 128
        uT = mk.tile(es, "u2T", [128, 8, T], BF16)
        gw = mk.tile(es, "gw", [128, NT, 32], F32)
        wrt = mk.tile(es, "wrt", [128, 8, 36], F32)
        mk.dma("sp", wrt[:], wr[l].rearrange("(k p) n -> p k n", p=128), wrt)
        es1 = ExitStack()
        stage_ln_mod(es1, src, 3, 4, uT, t0, router=(wrt, gw))
        mk.end_stage(es1)
        mk.stage_tiles.extend([uT, gw, wrt])
        es2 = ExitStack()
        acc = mk.tile(es, "acc", [128, NT, D], F32)
        w1s = [mk.tile(es2, "w1", [128, 8, 512], BF16) for _ in range(2)]
        w3s = [mk.tile(es2, "w3", [128, 8, 512], BF16) for _ in range(2)]
        w2s = [mk.tile(es2, "w2", [128, 4, D], BF16) for _ in range(2)]
        hid = [mk.tile(es2, "hid", [128, 4, 512], BF16) for _ in range(2)]
        sas = [mk.tile(es2, "sa", [128, 512], BF16) for _ in range(2)]
        nb = 0
        for ex in range(32):
            w1, w3, w2 = w1s[ex % 2], w3s[ex % 2], w2s[ex % 2]
            mk.dma("pool", w1[:], exp_w1[l, ex].rearrange("(k p) n -> p k n", p=128), w1)
            mk.dma("pool", w3[:], exp_w3[l, ex].rearrange("(k p) n -> p k n", p=128), w3)
            mk.dma("pool", w2[:], exp_w2[l, ex].rearrange("(k p) n -> p k n", p=128), w2)
            for bi, (a, b) in enumerate(TB):
                a = max(a, c0)
                if a >= b:
                    continue
                n = b - a
                hd_ = hid[nb % 2]
                nb += 1
                for hc in range(4):
                    pa, pb = (0, 1) if hc % 2 == 0 else (2, 3)
                    for k in range(8):
                        mk.op("pe", lambda e: e.matmul(bank(pa, n), lhsT=w1[:, k, hc * 128:(hc + 1) * 128], rhs=uT[:, k, a:b], start=(k == 0), stop=(k == 7)),
                              [w1, uT], [PB[pa]])
                    for k in range(8):
                        mk.op("pe", lambda e: e.matmul(bank(pb, n), lhsT=w3[:, k, hc * 128:(hc + 1) * 128], rhs=uT[:, k, a:b], start=(k == 0), stop=(k == 7)),
                              [w3, uT], [PB[pb]])
                    sa = sas[hc % 2]
                    mk.op("act", lambda e: e.activation(out=sa[:, 0:n], in_=bank(pa, n), func=AF.Silu), [PB[pa]], [sa])
                    mk.op("dve", lambda e: e.tensor_tensor(out=hd_[:, hc, 0:n], in0=bank(pb, n), in1=sa[:, 0:n], op=ALU.mult), [PB[pb], sa], [hd_])
                for ti, t in enumerate(range(a // 128, b // 128)):
                    pbs = (4, 5) if ti % 2 == 0 else (6, 7)
                    for half in range(2):
                        for hc in range(4):
                            mk.op("pe", lambda e: e.matmul(bank(pbs[half]), lhsT=hd_[:, hc, (t * 128 - a):(t * 128 - a) + 128], rhs=w2[:, hc, half * 512:(half + 1) * 512],
                                                           start=(hc == 0), stop=(hc == 3)), [hd_, w2], [PB[pbs[half]]])
                    yps = PS[:, pbs[0] * 512:pbs[0] * 512 + 1024]
                    if ex == 0:
                        mk.op("dve", lambda e: e.tensor_scalar_mul(out=acc[:, t, :], in0=yps, scalar1=gw[:, t, ex:ex + 1]), [PB[pbs[0]], PB[pbs[1]], gw], [acc])
                    else:
                        mk.op("dve", lambda e: e.scalar_tensor_tensor(out=acc[:, t, :], in0=yps, scalar=gw[:, t, ex:ex + 1], in1=acc[:, t, :],
                                                                      op0=ALU.mult, op1=ALU.add), [PB[pbs[0]], PB[pbs[1]], gw, acc], [acc])
        mk.end_stage(es2)
        mk.stage_tiles.extend([uT, gw, wrt, acc])
        grow = build_grow(es, 5)
        lnrow = mk.tile(es, "lnrow", [128, 2, D], F32)
        mk.dma("sp", lnrow[:], rows_d[l, R_LN2G:R_LN2G + 2048].rearrange("(a n) -> a n", a=2).partition_broadcast(128), lnrow)
        hts = [mk.tile(es, "ht", [128, D], F32) for _ in range(2)]
        vts = [mk.tile(es, "vt", [128, D], F32) for _ in range(2)]
        sts = [mk.tile(es, "st", [128, 12], F32) for _ in range(2)]
        mvs = [mk.tile(es, "mv", [128, 4], F32) for _ in range(2)]
        for i, t in enumerate(range(t0, NT)):
            ht = hts[i % 2]
            mk.dma("sp", ht[:], src[t * 128:(t + 1) * 128, :], ht)
            res_ln(t, acc[:, t, :], [acc], src, grow, lnrow, dst_fn, (ht, vts[i % 2], sts[i % 2], mvs[i % 2]))
        mk.end_stage(es)

    def hd_tile(t):
        return hd[t * 128:(t + 1) * 128, :]

    def y_tile(t):
        return y_out[(t - 2) * 128:(t - 1) * 128, :]

    for l in range(n_layers):
        last = (l == n_layers - 1)
        src = h0 if l == 0 else hd
        stage_mod(l)
        stage_inproj(l, src)
        if stop_after == "inproj":
            break
        stage_mla(l, last)
        if stop_after == "mla":
            break
        stage_ssd(l, last)
        if stop_after == "ssd":
            break
        stage_merge(l, last, src, hd_tile)
        if stop_after == "merge":
            break
        stage_moe(l, last, hd, y_tile if last else hd_tile)
    mk.barrier()
    print("built: inst", mk.n_inst, "waits", mk.n_wait, "cnt", mk.cnt)
    return nc


def _prep_shared(inp):
    f = np.float32
    Lh = inp["w_in"].shape[0]
    sh = {}
    sh["w_mod"] = np.ascontiguousarray(inp["w_mod"], f)
    sh["w_in"] = np.ascontiguousarray(inp["w_in"], f)
    kr = inp["w_in"][:, :, C_KR:C_KR + 64].reshape(Lh, D, 2, 2, 16)
    sh["w_krsw"] = np.ascontiguousarray(kr[:, :, :, ::-1, :].reshape(Lh, D, 64), f)
    uq = inp["w_uq"].reshape(Lh, 512, 8, 192)
    qr = uq[..., 128:].reshape(Lh, 512, 8, 2, 2, 16)
    qsw = qr[:, :, :, :, ::-1, :].reshape(Lh, 512, 8, 64)
    sh["w_uqx"] = np.ascontiguousarray(np.concatenate([uq, qsw], axis=-1).reshape(Lh, 512, 2048), f)
    sh["w_ukv"] = np.ascontiguousarray(inp["w_ukv"], f)
    bd = np.zeros((Lh, 2, 2, 8, 128, 128), f)
    for gi, key in enumerate(("lru_wa", "lru_wx")):
        w = inp[key]
        for c in range(8):
            bd[:, :, gi, c, 0:64, 0:64] = w[:, :, 2 * c]
            bd[:, :, gi, c, 64:128, 64:128] = w[:, :, 2 * c + 1]
    sh["lru_bd"] = bd.reshape(Lh, 32, 128, 128)
    sh["w_branch"] = np.ascontiguousarray(inp["w_branch"], f)
    sh["w_out"] = np.ascontiguousarray(inp["w_out"], f)
    sh["wr"] = np.ascontiguousarray(np.concatenate([inp["router_wg"], inp["router_we"]], axis=-1), f)
    sh["exp_w1"] = np.ascontiguousarray(inp["exp_w1"], f)
    sh["exp_w3"] = np.ascontiguousarray(inp["exp_w3"], f)
    sh["exp_w2"] = np.ascontiguousarray(inp["exp_w2"], f)
    vecs = np.zeros((Lh, 128, NV), f)

    def colfmt(v, nchunk):
        return v.reshape(v.shape[:-1] + (nchunk, 128))

    for l in range(Lh):
        bm = inp["b_mod"][l].reshape(48, 128).T
        vecs[l, :, V_BMOD:V_BMOD + 96] = np.repeat(bm, 2, axis=1)
        vecs[l, :, V_QNW:V_QNW + 4] = inp["q_norm_w"][l].reshape(4, 128).T
        vecs[l, :, V_KVNW:V_KVNW + 2] = inp["kv_norm_w"][l].reshape(2, 128).T
        vecs[l, :, V_SCW:V_SCW + 64] = inp["ssd_conv_w"][l].reshape(4, 16, 128).transpose(2, 1, 0).reshape(128, 64)
        vecs[l, :, V_SCB:V_SCB + 16] = inp["ssd_conv_b"][l].reshape(16, 128).T
        vecs[l, :, V_LCW:V_LCW + 32] = inp["lru_conv_w"][l].reshape(4, 8, 128).transpose(2, 1, 0).reshape(128, 32)
        vecs[l, :, V_LCB:V_LCB + 8] = inp["lru_conv_b"][l].reshape(8, 128).T
        vecs[l, :, V_LBA:V_LBA + 16] = inp["lru_ba"][l].reshape(2, 8, 128).transpose(2, 0, 1).reshape(128, 16)
        vecs[l, :, V_LBX:V_LBX + 16] = inp["lru_bx"][l].reshape(2, 8, 128).transpose(2, 0, 1).reshape(128, 16)
        vecs[l, :, V_LLAM:V_LLAM + 16] = inp["lru_lambda"][l].reshape(2, 8, 128).transpose(2, 0, 1).reshape(128, 16)
        vecs[l, :, V_SNW:V_SNW + 8] = inp["ssd_norm_w"][l].reshape(8, 128).T
    sh["vecs"] = vecs
    rows = np.zeros((Lh, NR), f)
    rows[:, R_LN1G:R_LN1G + 1024] = inp["ln1_g"]
    rows[:, R_LN1B:R_LN1B + 1024] = inp["ln1_b"]
    rows[:, R_LN2G:R_LN2G + 1024] = inp["ln2_g"]
    rows[:, R_LN2B:R_LN2B + 1024] = inp["ln2_b"]
    rows[:, R_ALOG:R_ALOG + 32] = inp["ssd_a_log"].reshape(Lh, 32)
    rows[:, R_DTB:R_DTB + 32] = inp["ssd_dt_bias"].reshape(Lh, 32)
    rows[:, R_DSK:R_DSK + 16] = inp["ssd_d"]
    rows[:, R_RB:R_RB + 4] = inp["router_bg"]
    rows[:, R_RB + 4:R_RB + 36] = inp["router_be"]
    sh["rows"] = rows
    idx = np.arange(128)
    consts = np.zeros((4, 128, 128), f)
    consts[0] = np.eye(128)
    consts[1] = (idx[:, None] <= idx[None, :])
    consts[2] = (idx[:, None] >= idx[None, :])
    consts[3] = 1.0
    sh["consts"] = consts
    pos = np.arange(2048)
    rowp = (pos // 64).astype(f)
    colp = (pos % 64).astype(f)
    inv = (10000.0 ** (-np.arange(16, dtype=f) / 16)).astype(f)
    ang = np.stack([rowp[:, None] * inv, colp[:, None] * inv], axis=1).astype(f)
    cos = np.cos(ang).astype(f)
    sin = np.sin(ang).astype(f)
    ct = np.ones((64, T), f)
    st = np.zeros((64, T), f)
    for a in range(2):
        for j in range(2):
            r0 = a * 32 + j * 16
            ct[r0:r0 + 16, 256:] = cos[:, a, :].T
            st[r0:r0 + 16, 256:] = (-1.0 if j == 0 else 1.0) * sin[:, a, :].T
    sh["cossin"] = np.stack([ct, st]).astype(f)
    return sh


def _prep_core(inp, b):
    f = np.float32
    m = {}
    m["h0"] = np.ascontiguousarray(np.concatenate([inp["ctx"][b], inp["x"][b]], axis=0), f)
    cv = np.zeros((128, 16), f)
    cv[:, 0::2] = inp["c"][b].reshape(8, 128).T
    cv[:, 1::2] = inp["c_ctx"].reshape(8, 128).T
    m["cvec"] = cv
    return m


_NC_CACHE = {}


def kernel(**inputs):
    inp = {k: np.asarray(v) for k, v in inputs.items()}
    if "nc" not in _NC_CACHE:
        _NC_CACHE["nc"] = build()
    nc = _NC_CACHE["nc"]
    sh = _prep_shared(inp)
    in_maps = []
    for b in range(8):
        m = dict(sh)
        m.update(_prep_core(inp, b))
        in_maps.append(m)
    res = run_bass_kernel_spmd(nc, in_maps, core_ids=list(range(8)))
    out = np.stack([np.asarray(r["y"], dtype=np.float32) for r in res.results], axis=0)
    return out
```

```python
import math
from contextlib import ExitStack
import numpy as np
import concourse.bass as bass
import concourse.mybir as mybir
from concourse.bass_utils import run_bass_kernel_spmd

F32 = mybir.dt.float32
BF16 = mybir.dt.bfloat16
AF = mybir.ActivationFunctionType
ALU = mybir.AluOpType
AX = mybir.AxisListType.X

D = 1024
T = 2304
NT = 18
L = 4
TB = [(0, 512), (512, 1024), (1024, 1536), (1536, 2048), (2048, 2304)]
SEQS = [(0, 256), (256, 2304)]
ALPHA = 8.0 ** 0.25
LN_EPS = 1e-5
RMS_EPS = 1e-6
ATTN_SCALE = 192.0 ** -0.5
C_CQ, C_CKV, C_KR, C_Z, C_XBC, C_DT, C_LX, C_LG, C_GATE = 0, 512, 768, 832, 1856, 3904, 3936, 4960, 5984
NCOL = 9056
V_BMOD, V_QNW, V_KVNW, V_SCW, V_SCB, V_LCW, V_LCB, V_LBA, V_LBX, V_LLAM, V_SNW = 0, 96, 100, 102, 166, 182, 214, 222, 238, 254, 270
NV = 278
R_LN1G, R_LN1B, R_LN2G, R_LN2B, R_ALOG, R_DTB, R_DSK, R_RB = 0, 1024, 2048, 3072, 4096, 4128, 4160, 4176
NR = 4212


class Tile:
    __slots__ = ("t", "name", "last_w", "reads", "dsem", "dcount")

    def __init__(self, t, name):
        self.t = t
        self.name = name
        self.last_w = None
        self.reads = []
        self.dsem = None
        self.dcount = 0

    def __getitem__(self, k):
        return self.t[k]


class MK:
    def __init__(self, nc):
        self.nc = nc
        self.E = {"pe": nc.tensor, "act": nc.scalar, "dve": nc.vector, "pool": nc.gpsimd, "sp": nc.sync}
        self.sem = {k: nc.alloc_semaphore("s_" + k) for k in self.E}
        self.cnt = {k: 0 for k in self.E}
        self.waited = {}
        self.sem_pool = []
        self.stage_tiles = []
        self.dma_active = []
        self.uid = 0
        self.n_inst = 0
        self.n_wait = 0

    def tile(self, es, name, shape, dt):
        self.uid += 1
        t = es.enter_context(self.nc.sbuf_tensor("%s_%d" % (name, self.uid), list(shape), dt))
        tl = Tile(t, name)
        self.stage_tiles.append(tl)
        return tl

    def tracker(self, name):
        return Tile(None, name)

    def _wait(self, eng, tok):
        if tok is None:
            return
        sem, val, src = tok
        if src == eng and eng == "pe":
            return
        key = (eng, id(sem))
        if self.waited.get(key, 0) >= val:
            return
        self.waited[key] = val
        self.E[eng].wait_ge(sem, val)
        self.n_wait += 1

    def _deps(self, eng, reads, writes):
        for b in reads:
            self._wait(eng, b.last_w)
        for b in writes:
            self._wait(eng, b.last_w)
            for r in b.reads:
                self._wait(eng, r)

    def op(self, eng, fn, reads=(), writes=()):
        self._deps(eng, reads, writes)
        inst = fn(self.E[eng])
        self.cnt[eng] += 1
        inst.then_inc(self.sem[eng], 1)
        tok = (self.sem[eng], self.cnt[eng], eng)
        for b in reads:
            b.reads.append(tok)
        for b in writes:
            b.last_w = tok
            b.reads = []
        self.n_inst += 1
        return inst

    def dma(self, q, out, in_, sbuf, load=True):
        if sbuf.dsem is None:
            if self.sem_pool:
                sbuf.dsem, sbuf.dcount = self.sem_pool.pop()
            else:
                self.uid += 1
                sbuf.dsem, sbuf.dcount = self.nc.alloc_semaphore("d%d" % self.uid), 0
            self.dma_active.append(sbuf)
        if load:
            self._deps(q, [], [sbuf])
        else:
            self._deps(q, [sbuf], [])
        inst = self.E[q].dma_start(out=out, in_=in_)
        sbuf.dcount += 16
        inst.then_inc(sbuf.dsem, 16)
        tok = (sbuf.dsem, sbuf.dcount, "dma")
        if load:
            sbuf.last_w = tok
            sbuf.reads = []
        else:
            sbuf.reads.append(tok)
        self.n_inst += 1
        return inst

    def barrier(self):
        for b in self.dma_active:
            self._wait("sp", (b.dsem, b.dcount, "dma"))
        for k in self.E:
            if k != "sp" and self.cnt[k]:
                self._wait("sp", (self.sem[k], self.cnt[k], k))
        inst = self.E["sp"].nop()
        self.cnt["sp"] += 1
        inst.then_inc(self.sem["sp"], 1)
        tok = (self.sem["sp"], self.cnt["sp"], "sp")
        for k in self.E:
            if k != "sp":
                self._wait(k, tok)

    def end_stage(self, es):
        self.barrier()
        keep = []
        for tl in self.dma_active:
            if tl in self.stage_tiles:
                self.sem_pool.append((tl.dsem, tl.dcount))
                tl.dsem = None
            else:
                keep.append(tl)
        self.dma_active = keep
        self.stage_tiles = []
        es.close()


def build(n_layers=L, debug=False, stop_after=None):
    nc = bass.Bass("TRN2", target_bir_lowering=False)
    mk = MK(nc)

    def dram(name, shape, dt=F32, kind="ExternalInput"):
        return nc.dram_tensor(name, list(shape), dt, kind=kind).ap()

    skind = "ExternalOutput" if debug else "Internal"
    h0 = dram("h0", [T, D])
    cvec = dram("cvec", [128, 16])
    w_mod = dram("w_mod", [L, D, 6144])
    w_in = dram("w_in", [L, D, NCOL])
    w_krsw = dram("w_krsw", [L, D, 64])
    w_uqx = dram("w_uqx", [L, 512, 2048])
    w_ukv = dram("w_ukv", [L, 256, 2048])
    lru_bd = dram("lru_bd", [L, 32, 128, 128])
    w_branch = dram("w_branch", [L, 3, D, D])
    w_out = dram("w_out", [L, D, D])
    wr = dram("wr", [L, D, 36])
    exp_w1 = dram("exp_w1", [L, 32, D, 512])
    exp_w3 = dram("exp_w3", [L, 32, D, 512])
    exp_w2 = dram("exp_w2", [L, 32, 512, D])
    vecs_d = dram("vecs", [L, 128, NV])
    rows_d = dram("rows", [L, NR])
    consts_d = dram("consts", [4, 128, 128])
    cs_d = dram("cossin", [2, 64, T])
    y_out = dram("y", [2048, D], F32, "ExternalOutput")
    hd = dram("hd", [T, D], F32, skind)
    cqT = dram("cqT", [512, T], BF16, skind)
    ckvT = dram("ckvT", [256, T], BF16, skind)
    krT = dram("krT", [64, T], BF16, skind)
    bcT = dram("bcT", [1024, T], BF16, skind)
    xs_tm = dram("xs_tm", [T, D], BF16, skind)
    b_tm = dram("b_tm", [T, 512], BF16, skind)
    zs = dram("zs", [T, D], BF16, skind)
    dtr = dram("dtr", [T, 32], F32, skind)
    gateT = dram("gateT", [3072, T], BF16, skind)
    attT = dram("attT", [D, T], BF16, skind)
    yssdT = dram("yssdT", [D, T], BF16, skind)
    ylruT = dram("ylruT", [D, T], BF16, skind)
    hprev = dram("hprev", [2, NT, 128, D], BF16, skind)

    ges = ExitStack()
    PSt = ges.enter_context(nc.psum_tensor("psall", [128, 4096], F32))
    PS = PSt
    PB = [mk.tracker("pb%d" % i) for i in range(8)]

    def bank(i, n=512, off=0):
        return PS[:, i * 512 + off:i * 512 + off + n]

    def bankb(i):
        return PS[:, i * 512:(i + 1) * 512].bitcast(BF16)

    cst = mk.tile(ges, "cst", [128, 4, 128], F32)
    identf = cst[:, 0, :]
    triU = cst[:, 1, :]
    triL = cst[:, 2, :]
    onesf = cst[:, 3, :]
    cstb = mk.tile(ges, "cstb", [128, 2, 128], BF16)
    identb = cstb[:, 0, :]
    onesb = cstb[:, 1, :]
    vec = mk.tile(ges, "vec", [128, NV], F32)
    srow = mk.tile(ges, "srow", [128, NR - R_ALOG], F32)
    modT = mk.tile(ges, "modT", [128, 96], F32)
    scb = mk.tile(ges, "scb", [128, 16], BF16)
    cvt = mk.tile(ges, "cvt", [128, 16], F32)
    lcs = mk.tile(ges, "lcs", [128, 16], F32)
    qnws = mk.tile(ges, "qnws", [128, 6], F32)

    mk.dma("sp", cst[:], consts_d.rearrange("c p n -> p c n"), cst)
    mk.op("dve", lambda e: e.tensor_copy(out=cstb[:, 0, :], in_=cst[:, 0, :]), [cst], [cstb])
    mk.op("dve", lambda e: e.tensor_copy(out=cstb[:, 1, :], in_=cst[:, 3, :]), [cst], [cstb])
    mk.dma("sp", cvt[:], cvec, cvt)
    mk.op("act", lambda e: e.activation(out=scb[:], in_=cvt[:], func=AF.Silu), [cvt], [scb])

    def SROW(off, n):
        return srow[:, off - R_ALOG:off - R_ALOG + n]

    def modcol(sec, c, j):
        col = (sec * 8 + c) * 2 + j
        return modT[:, col:col + 1]

    rr = {"mm": 0, "ev": 0}

    def evac_eng():
        rr["ev"] += 1
        return "act" if rr["ev"] % 2 else "dve"

    def copy(eng, out, in_, r, w):
        if eng == "act":
            mk.op("act", lambda e: e.activation(out=out, in_=in_, func=AF.Identity), r, w)
        else:
            mk.op(eng, lambda e: e.tensor_copy(out=out, in_=in_), r, w)

    def stage_mod(l):
        es = ExitStack()
        mk.dma("sp", vec[:], vecs_d[l], vec)
        mk.dma("sp", srow[:], rows_d[l, R_ALOG:NR].partition_broadcast(128), srow)
        wmb = [mk.tile(es, "wmb", [128, 8, 512], BF16) for _ in range(2)]
        wv = w_mod[l].rearrange("(k p) n -> p k n", p=128)
        for cg in range(12):
            wb = wmb[cg % 2]
            mk.dma("pool", wb[:], wv[:, :, cg * 512:(cg + 1) * 512], wb)
            for f in range(4):
                fc = cg * 4 + f
                for k in range(8):
                    mk.op("pe", lambda e: e.matmul(bank(0, 2, fc * 2), lhsT=wb[:, k, f * 128:(f + 1) * 128],
                                                   rhs=scb[:, 2 * k:2 * k + 2], start=(k == 0), stop=(k == 7)),
                          [wb, scb], [PB[0]])
        mk.op("dve", lambda e: e.tensor_tensor(out=modT[:], in0=bank(0, 96), in1=vec[:, V_BMOD:V_BMOD + 96], op=ALU.add),
              [PB[0], vec], [modT])
        for sec in (1, 4):
            mk.op("dve", lambda e: e.tensor_scalar_add(out=modT[:, sec * 16:(sec + 1) * 16], in0=modT[:, sec * 16:(sec + 1) * 16],
                                                       scalar1=1.0), [modT], [modT])
        mk.op("act", lambda e: e.activation(out=lcs[:], in_=vec[:, V_LLAM:V_LLAM + 16], func=AF.Exp, scale=-1.0), [vec], [lcs])
        mk.op("act", lambda e: e.activation(out=lcs[:], in_=lcs[:], func=AF.Ln, bias=1.0, scale=1.0), [lcs], [lcs])
        mk.op("dve", lambda e: e.tensor_scalar_mul(out=lcs[:], in0=lcs[:], scalar1=-8.0), [lcs], [lcs])
        mk.op("dve", lambda e: e.tensor_scalar_mul(out=qnws[:, 0:4], in0=vec[:, V_QNW:V_QNW + 4], scalar1=math.sqrt(512.0)), [vec], [qnws])
        mk.op("dve", lambda e: e.tensor_scalar_mul(out=qnws[:, 4:6], in0=vec[:, V_KVNW:V_KVNW + 2], scalar1=math.sqrt(256.0)), [vec], [qnws])
        mk.end_stage(es)

    def build_grow(es, sec):
        grow = mk.tile(es, "grow", [128, 2, D], F32)
        dg = [mk.tile(es, "dg", [128, 128], F32) for _ in range(2)]
        n = 0
        for j in range(2):
            for c in range(8):
                d_ = dg[n % 2]
                pb = 6 + (n % 2)
                mk.op("dve", lambda e: e.tensor_scalar_mul(out=d_[:], in0=identf, scalar1=modcol(sec, c, j)), [cst, modT], [d_])
                mk.op("pe", lambda e: e.matmul(bank(pb, 128), lhsT=onesf, rhs=d_[:], start=True, stop=True), [cst, d_], [PB[pb]])
                copy("act", grow[:, j, c * 128:(c + 1) * 128], bank(pb, 128), [PB[pb]], [grow])
                n += 1
        return grow

    def layer_norm_stats(x_ap, xt, stt, mvt):
        mk.op("dve", lambda e: e.bn_stats(out=stt[:, 0:6], in_=x_ap[:, 0:512]), [xt], [stt])
        mk.op("dve", lambda e: e.bn_stats(out=stt[:, 6:12], in_=x_ap[:, 512:1024]), [xt], [stt])
        mk.op("dve", lambda e: e.bn_aggr(out=mvt[:, 2:4], in_=stt[:]), [stt], [mvt])
        mk.op("act", lambda e: e.activation(out=mvt[:, 0:1], in_=mvt[:, 3:4], func=AF.Sqrt, bias=LN_EPS, scale=1.0), [mvt], [mvt])
        mk.op("dve", lambda e: e.reciprocal(out=mvt[:, 0:1], in_=mvt[:, 0:1]), [mvt], [mvt])
        mk.op("dve", lambda e: e.tensor_scalar(out=mvt[:, 1:2], in0=mvt[:, 2:3], scalar1=mvt[:, 0:1], scalar2=-1.0,
                                               op0=ALU.mult, op1=ALU.mult), [mvt], [mvt])

    def stage_ln_mod(es, src, sec_shift, sec_scale, uT, t0, router=None):
        hts = [mk.tile(es, "ht", [128, D], F32) for _ in range(2)]
        xns = [mk.tile(es, "xn", [128, D], F32) for _ in range(2)]
        ufs = [mk.tile(es, "uf", [128, 8, 128], F32) for _ in range(2)]
        sts = [mk.tile(es, "st", [128, 12], F32) for _ in range(2)]
        mvs = [mk.tile(es, "mv", [128, 4], F32) for _ in range(2)]
        if router is not None:
            wrt, gw = router
            rt = [mk.tile(es, "rt", [128, 160], F32) for _ in range(2)]
        tiles = list(range(t0, NT))
        mk.dma("sp", hts[0][:], src[tiles[0] * 128:(tiles[0] + 1) * 128, :], hts[0])
        for i, t in enumerate(tiles):
            ht, xn, uf, st_, mv = hts[i % 2], xns[i % 2], ufs[i % 2], sts[i % 2], mvs[i % 2]
            if i + 1 < len(tiles):
                tn = tiles[i + 1]
                mk.dma("sp", hts[(i + 1) % 2][:], src[tn * 128:(tn + 1) * 128, :], hts[(i + 1) % 2])
            j = 1 if t < 2 else 0
            layer_norm_stats(ht, ht, st_, mv)
            mk.op("act", lambda e: e.activation(out=xn[:], in_=ht[:], func=AF.Identity, scale=mv[:, 0:1], bias=mv[:, 1:2]),
                  [ht, mv], [xn])
            for half in range(2):
                pb = 4 + (rr["mm"] % 2)
                rr["mm"] += 1
                for c4 in range(4):
                    c = half * 4 + c4
                    mk.op("pe", lambda e: e.transpose(out=bank(pb, 128, c4 * 128), in_=xn[:, c * 128:(c + 1) * 128], identity=identf),
                          [xn, cst], [PB[pb]])
                for c4 in range(4):
                    c = half * 4 + c4
                    mk.op("act", lambda e: e.activation(out=uf[:, c, :], in_=bank(pb, 128, c4 * 128), func=AF.Identity,
                                                        scale=modcol(sec_scale, c, j), bias=modcol(sec_shift, c, j)),
                          [PB[pb], modT], [uf])
            mk.op("dve", lambda e: e.tensor_copy(out=uT[:, :, t * 128:(t + 1) * 128], in_=uf[:]), [uf], [uT])
            if router is not None:
                r_ = rt[i % 2]
                for k in range(8):
                    mk.op("pe", lambda e: e.matmul(bank(6, 36), lhsT=uf[:, k, :], rhs=wrt[:, k, :], start=(k == 0), stop=(k == 7)),
                          [uf, wrt], [PB[6]])
                lg = r_[:, 0:36]
                mk.op("dve", lambda e: e.tensor_tensor(out=lg, in0=bank(6, 36), in1=SROW(R_RB, 36), op=ALU.add), [PB[6], srow], [r_])
                gm = r_[:, 40:41]
                mk.op("dve", lambda e: e.reduce_max(out=gm, in_=r_[:, 0:4], axis=AX), [r_], [r_])
                ngm = r_[:, 41:42]
                mk.op("dve", lambda e: e.tensor_scalar_mul(out=ngm, in0=gm, scalar1=-1.0), [r_], [r_])
                mk.op("dve", lambda e: e.memset(r_[:, 42:43], 0.0), [], [r_])
                mk.op("act", lambda e: e.activation(out=r_[:, 44:48], in_=r_[:, 0:4], func=AF.Exp, bias=ngm, scale=1.0,
                                                    accum_out=r_[:, 42:43]), [r_], [r_])
                gval = r_[:, 43:44]
                mk.op("dve", lambda e: e.reciprocal(out=gval, in_=r_[:, 42:43]), [r_], [r_])
                gmask = r_[:, 48:52]
                mk.op("dve", lambda e: e.tensor_scalar(out=gmask, in0=r_[:, 0:4], scalar1=gm, scalar2=None, op0=ALU.is_ge), [r_], [r_])
                pen = r_[:, 52:56]
                mk.op("dve", lambda e: e.tensor_scalar(out=pen, in0=gmask, scalar1=-1.0, scalar2=1e30, op0=ALU.add, op1=ALU.mult), [r_], [r_])
                ml = r_[:, 56:88]
                mk.op("dve", lambda e: e.tensor_tensor(out=ml.rearrange("p (g x) -> p g x", x=8),
                                                       in0=r_[:, 4:36].rearrange("p (g x) -> p g x", x=8),
                                                       in1=pen.unsqueeze(2).to_broadcast([128, 4, 8]), op=ALU.add), [r_], [r_])
                m1 = r_[:, 88:89]
                mk.op("dve", lambda e: e.reduce_max(out=m1, in_=ml, axis=AX), [r_], [r_])
                oh1 = r_[:, 96:128]
                mk.op("dve", lambda e: e.tensor_scalar(out=oh1, in0=ml, scalar1=m1, scalar2=None, op0=ALU.is_ge), [r_], [r_])
                ml2 = r_[:, 128:160]
                mk.op("dve", lambda e: e.scalar_tensor_tensor(out=ml2, in0=oh1, scalar=-1e30, in1=ml, op0=ALU.mult, op1=ALU.add), [r_], [r_])
                m2 = r_[:, 89:90]
                mk.op("dve", lambda e: e.reduce_max(out=m2, in_=ml2, axis=AX), [r_], [r_])
                dd = r_[:, 90:91]
                mk.op("dve", lambda e: e.tensor_tensor(out=dd, in0=m2, in1=m1, op=ALU.subtract), [r_], [r_])
                mk.op("act", lambda e: e.activation(out=dd, in_=dd, func=AF.Exp), [r_], [r_])
                mk.op("dve", lambda e: e.tensor_scalar_add(out=dd, in0=dd, scalar1=1.0), [r_], [r_])
                mk.op("dve", lambda e: e.reciprocal(out=dd, in_=dd), [r_], [r_])
                w1_ = r_[:, 91:92]
                mk.op("dve", lambda e: e.tensor_tensor(out=w1_, in0=dd, in1=gval, op=ALU.mult), [r_], [r_])
                w2_ = r_[:, 92:93]
                mk.op("dve", lambda e: e.tensor_tensor(out=w2_, in0=gval, in1=w1_, op=ALU.subtract), [r_], [r_])
                mk.op("dve", lambda e: e.tensor_scalar(out=ml, in0=ml2, scalar1=m2, scalar2=w2_, op0=ALU.is_ge, op1=ALU.mult), [r_], [r_])
                mk.op("dve", lambda e: e.scalar_tensor_tensor(out=gw[:, t, :], in0=oh1, scalar=w1_, in1=ml, op0=ALU.mult, op1=ALU.add),
                      [r_], [gw])

    def res_ln(t, mix_ap, mix_reads, h_src, grow, lnrow, dst_ap_fn, tmps):
        ht, vt, st_, mv = tmps
        j = 1 if t < 2 else 0
        mk.op("dve", lambda e: e.tensor_tensor(out=vt[:], in0=mix_ap, in1=grow[:, j, :], op=ALU.mult), list(mix_reads) + [grow], [vt])
        mk.op("dve", lambda e: e.scalar_tensor_tensor(out=vt[:], in0=ht[:], scalar=ALPHA, in1=vt[:], op0=ALU.mult, op1=ALU.add),
              [ht, vt], [vt])
        layer_norm_stats(vt, vt, st_, mv)
        mk.op("act", lambda e: e.activation(out=vt[:], in_=vt[:], func=AF.Identity, scale=mv[:, 0:1], bias=mv[:, 1:2]), [vt, mv], [vt])
        mk.op("pool", lambda e: e.tensor_tensor(out=vt[:], in0=vt[:], in1=lnrow[:, 0, :], op=ALU.mult), [vt, lnrow], [vt])
        mk.op("pool", lambda e: e.tensor_tensor(out=vt[:], in0=vt[:], in1=lnrow[:, 1, :], op=ALU.add), [vt, lnrow], [vt])
        mk.dma("pool", dst_ap_fn(t), vt[:], vt, load=False)

    def stage_inproj(l, src):
        es = ExitStack()
        uT = mk.tile(es, "uT", [128, 8, T], BF16)
        es1 = ExitStack()
        stage_ln_mod(es1, src, 0, 1, uT, 0)
        mk.end_stage(es1)
        wv = w_in[l].rearrange("(k p) n -> p k n", p=128)
        wgs = [mk.tile(es, "wg", [128, 8, 512], BF16) for _ in range(2)]
        keep = [uT] + wgs

        def sub_end(esx):
            mk.end_stage(esx)
            mk.stage_tiles.extend(keep)
            return ExitStack()

        mk.stage_tiles.extend(keep)
        es_outer = es
        es = ExitStack()
        wgi = [0]

        def load_w(col0, n, src_v=None):
            wg = wgs[wgi[0] % 2]
            wgi[0] += 1
            v = wv if src_v is None else src_v
            mk.dma("pool", wg[:, :, 0:n], v[:, :, col0:col0 + n], wg)
            return wg

        def proj_fm(wg, c0, M, a, b, pb):
            for k in range(8):
                mk.op("pe", lambda e: e.matmul(PS[0:M, pb * 512:pb * 512 + (b - a)], lhsT=wg[:, k, c0:c0 + M], rhs=uT[:, k, a:b],
                                               start=(k == 0), stop=(k == 7)), [wg, uT], [PB[pb]])

        def next_pb():
            rr["mm"] += 1
            return rr["mm"] % 4

        raws = [mk.tile(es, "raw", [128, 4, 512], BF16) for _ in range(2)]
        sqs = [mk.tile(es, "sq", [128, 512], BF16) for _ in range(2)]
        Rt = [mk.tile(es, "Rt", [128, 512], F32) for _ in range(2)]
        outb = [mk.tile(es, "outb", [128, 4, 512], BF16) for _ in range(2)]
        for (col0, nch, dim, nwoff, dst) in ((C_CQ, 4, 512, 0, cqT), (C_CKV, 2, 256, 4, ckvT)):
            wg = load_w(col0, nch * 128)
            for bi, (a, b) in enumerate(TB):
                n = b - a
                raw, R_, ob = raws[bi % 2], Rt[bi % 2], outb[bi % 2]
                for c in range(nch):
                    pb = next_pb()
                    proj_fm(wg, c * 128, 128, a, b, pb)
                    copy("act", raw[:, c, 0:n], bank(pb, n), [PB[pb]], [raw])
                    sq = sqs[c % 2]
                    mk.op("dve", lambda e: e.tensor_tensor(out=sq[:, 0:n], in0=bank(pb, n), in1=raw[:, c, 0:n], op=ALU.mult), [PB[pb], raw], [sq])
                    mk.op("pe", lambda e: e.matmul(bank(4, n), lhsT=onesb, rhs=sq[:, 0:n], start=(c == 0), stop=(c == nch - 1)),
                          [cstb, sq], [PB[4]])
                mk.op("act", lambda e: e.activation(out=R_[:, 0:n], in_=bank(4, n), func=AF.Sqrt, bias=dim * RMS_EPS, scale=1.0), [PB[4]], [R_])
                mk.op("dve", lambda e: e.reciprocal(out=R_[:, 0:n], in_=R_[:, 0:n]), [R_], [R_])
                for c in range(nch):
                    mk.op("dve", lambda e: e.scalar_tensor_tensor(out=ob[:, c, 0:n], in0=raw[:, c, 0:n], scalar=qnws[:, nwoff + c:nwoff + c + 1],
                                                                  in1=R_[:, 0:n], op0=ALU.mult, op1=ALU.mult), [raw, qnws, R_], [ob])
                mk.dma("sp", dst.rearrange("(c p) t -> p c t", p=128)[:, :, a:b], ob[:, 0:nch, 0:n], ob, load=False)
        cs = mk.tile(es, "cs", [64, 2, T], F32)
        mk.dma("sp", cs[:], cs_d.rearrange("c p t -> p c t"), cs)
        wg = load_w(C_KR, 64)
        wg2 = load_w(0, 64, w_krsw[l].rearrange("(k p) n -> p k n", p=128))
        krb = mk.tile(es, "krb", [64, T], BF16)
        kt1 = mk.tile(es, "kt1", [64, 512], F32)
        kt2 = mk.tile(es, "kt2", [64, 512], F32)
        for bi, (a, b) in enumerate(TB):
            n = b - a
            p1 = next_pb()
            proj_fm(wg, 0, 64, a, b, p1)
            p2 = next_pb()
            proj_fm(wg2, 0, 64, a, b, p2)
            mk.op("dve", lambda e: e.tensor_tensor(out=kt1[:, 0:n], in0=PS[0:64, p1 * 512:p1 * 512 + n], in1=cs[:, 0, a:b], op=ALU.mult), [PB[p1], cs], [kt1])
            mk.op("dve", lambda e: e.tensor_tensor(out=kt2[:, 0:n], in0=PS[0:64, p2 * 512:p2 * 512 + n], in1=cs[:, 1, a:b], op=ALU.mult), [PB[p2], cs], [kt2])
            mk.op("pool", lambda e: e.tensor_tensor(out=krb[:, a:b], in0=kt1[:, 0:n], in1=kt2[:, 0:n], op=ALU.add), [kt1, kt2], [krb])
        mk.dma("sp", krT, krb[:], krb, load=False)
        gbs = [mk.tile(es, "gb", [128, T], BF16) for _ in range(2)]
        for c in range(24):
            if c % 4 == 0:
                wg = load_w(C_GATE + c * 128, 512)
            gb = gbs[c % 2]
            for bi, (a, b) in enumerate(TB):
                pb = next_pb()
                proj_fm(wg, (c % 4) * 128, 128, a, b, pb)
                mk.op("act", lambda e: e.activation(out=gb[:, a:b], in_=bank(pb, b - a), func=AF.Sigmoid), [PB[pb]], [gb])
            mk.dma("sp", gateT[c * 128:(c + 1) * 128, :], gb[:], gb, load=False)
        es = sub_end(es)
        rawf = [mk.tile(es, "rawf", [128, T], F32) for _ in range(2)]
        accf = [mk.tile(es, "accf", [128, T], F32) for _ in range(2)]
        xbb = [mk.tile(es, "xbb", [128, T], BF16) for _ in range(2)]
        tmT = [mk.tile(es, "tmT", [128, NT, 128], BF16) for _ in range(2)]

        def conv(raw, acc, wcol, bcol):
            mk.op("act", lambda e: e.activation(out=acc[:], in_=raw[:], func=AF.Identity, scale=vec[:, wcol + 2:wcol + 3], bias=vec[:, bcol:bcol + 1]),
                  [raw, vec], [acc])
            for (s0, s1) in SEQS:
                for (eng, j, oa, ob_, ia, ib) in (("dve", 0, s0 + 2, s1, s0, s1 - 2), ("dve", 1, s0 + 1, s1, s0, s1 - 1), ("dve", 3, s0, s1 - 1, s0 + 1, s1)):
                    mk.op(eng, lambda e: e.scalar_tensor_tensor(out=acc[:, oa:ob_], in0=raw[:, ia:ib], scalar=vec[:, wcol + j:wcol + j + 1],
                                                                in1=acc[:, oa:ob_], op0=ALU.mult, op1=ALU.add), [raw, vec, acc], [acc])

        for c in range(16):
            if c % 4 == 0:
                wg = load_w(C_XBC + c * 128, 512)
            raw, acc, xb = rawf[c % 2], accf[c % 2], xbb[c % 2]
            for bi, (a, b) in enumerate(TB):
                pb = next_pb()
                proj_fm(wg, (c % 4) * 128, 128, a, b, pb)
                copy(evac_eng(), raw[:, a:b], bank(pb, b - a), [PB[pb]], [raw])
            conv(raw, acc, V_SCW + c * 4, V_SCB + c)
            mk.op("act", lambda e: e.activation(out=xb[:], in_=acc[:], func=AF.Silu), [acc], [xb])
            if c >= 8:
                mk.dma("sp", bcT[(c - 8) * 128:(c - 7) * 128, :], xb[:], xb, load=False)
            if c < 12:
                tm = tmT[c % 2]
                for g0 in range(0, NT, 8):
                    g1 = min(NT, g0 + 8)
                    pb = 4 + (g0 // 8) % 2
                    for t in range(g0, g1):
                        mk.op("pe", lambda e: e.transpose(out=bankb(pb)[:, (t - g0) * 128:(t - g0 + 1) * 128], in_=xb[:, t * 128:(t + 1) * 128], identity=identb),
                              [xb, cstb], [PB[pb]])
                    copy(evac_eng(), tm[:, g0:g1, :], bankb(pb)[:, 0:(g1 - g0) * 128].rearrange("p (t c) -> p t c", c=128), [PB[pb]], [tm])
                if c < 8:
                    mk.dma("sp", xs_tm.rearrange("(t p) c -> p t c", p=128)[:, :, c * 128:(c + 1) * 128], tm[:], tm, load=False)
                else:
                    mk.dma("sp", b_tm.rearrange("(t p) c -> p t c", p=128)[:, :, (c - 8) * 128:(c - 7) * 128], tm[:], tm, load=False)
        es = sub_end(es)
        rawf = [mk.tile(es, "rawf", [128, T], F32) for _ in range(2)]
        bd = mk.tile(es, "bd", [128, 32, 128], BF16)
        mk.dma("pool", bd[:], lru_bd[l].rearrange("m p n -> p m n"), bd)
        xc = mk.tile(es, "xc", [128, T], F32)
        xcb = mk.tile(es, "xcb", [128, T], BF16)
        ra_l = [mk.tile(es, "ra", [128, T], F32) for _ in range(2)]
        ib_l = [mk.tile(es, "ib", [128, T], F32) for _ in range(2)]
        tq_l = [mk.tile(es, "tq", [128, T], F32) for _ in range(2)]
        hh = [mk.tile(es, "hh", [128, T], F32) for _ in range(2)]
        gg = mk.tile(es, "gg", [128, T], F32)
        yb = [mk.tile(es, "yb", [128, T], BF16) for _ in range(2)]
        for c in range(8):
            if c % 4 == 0:
                wgx = load_w(C_LX + c * 128, 512)
                wgg = load_w(C_LG + c * 128, 512)
            raw = rawf[c % 2]
            for bi, (a, b) in enumerate(TB):
                pb = next_pb()
                proj_fm(wgx, (c % 4) * 128, 128, a, b, pb)
                copy(evac_eng(), raw[:, a:b], bank(pb, b - a), [PB[pb]], [raw])
            conv(raw, xc, V_LCW + c * 4, V_LCB + c)
            mk.op("pool", lambda e: e.tensor_copy(out=xcb[:], in_=xc[:]), [xc], [xcb])
            for d in range(2):
                ra, ib_, tq = ra_l[d], ib_l[d], tq_l[d]
                for gi, (gt, boff) in enumerate(((ra, V_LBA), (ib_, V_LBX))):
                    m = (d * 2 + gi) * 8 + c
                    for bi, (a, b) in enumerate(TB):
                        pb = next_pb()
                        mk.op("pe", lambda e: e.matmul(bank(pb, b - a), lhsT=bd[:, m, :], rhs=xcb[:, a:b], start=True, stop=True), [bd, xcb], [PB[pb]])
                        mk.op("act", lambda e: e.activation(out=gt[:, a:b], in_=bank(pb, b - a), func=AF.Sigmoid,
                                                            bias=vec[:, boff + d * 8 + c:boff + d * 8 + c + 1], scale=1.0), [PB[pb], vec], [gt])
                mk.op("act", lambda e: e.activation(out=ra[:], in_=ra[:], func=AF.Exp, scale=lcs[:, d * 8 + c:d * 8 + c + 1]), [ra, lcs], [ra])
                mk.op("pool", lambda e: e.tensor_tensor(out=tq[:], in0=ra[:], in1=ra[:], op=ALU.mult), [ra], [tq])
                mk.op("act", lambda e: e.activation(out=tq[:], in_=tq[:], func=AF.Sqrt, bias=1.0, scale=-1.0), [tq], [tq])
                mk.op("pool", lambda e: e.tensor_tensor(out=ib_[:], in0=ib_[:], in1=tq[:], op=ALU.mult), [ib_, tq], [ib_])
                mk.op("dve", lambda e: e.tensor_tensor(out=ib_[:], in0=ib_[:], in1=xc[:], op=ALU.mult), [ib_, xc], [ib_])
                h_ = hh[d]
                if d == 0:
                    mk.op("dve", lambda e: e.tensor_tensor_scan(out=h_[:, 0:256], data0=ra[:, 0:256], data1=ib_[:, 0:256], initial=0.0,
                                                                op0=ALU.mult, op1=ALU.add), [ra, ib_], [h_])
                    mk.op("dve", lambda e: e.tensor_tensor_scan(out=h_[:, 256:T], data0=ra[:, 256:T], data1=ib_[:, 256:T], initial=h_[:, 255:256],
                                                                op0=ALU.mult, op1=ALU.add), [ra, ib_, h_], [h_])
                else:
                    mk.op("dve", lambda e: e.tensor_tensor_scan(out=h_[:, 0:256][:, ::-1], data0=ra[:, 0:256][:, ::-1], data1=ib_[:, 0:256][:, ::-1],
                                                                initial=0.0, op0=ALU.mult, op1=ALU.add), [ra, ib_], [h_])
                    mk.op("dve", lambda e: e.tensor_tensor_scan(out=h_[:, 256:T][:, ::-1], data0=ra[:, 256:T][:, ::-1], data1=ib_[:, 256:T][:, ::-1],
                                                                initial=h_[:, 0:1], op0=ALU.mult, op1=ALU.add), [ra, ib_, h_], [h_])
            for bi, (a, b) in enumerate(TB):
                pb = next_pb()
                proj_fm(wgg, (c % 4) * 128, 128, a, b, pb)
                mk.op("act", lambda e: e.activation(out=gg[:, a:b], in_=bank(pb, b - a), func=AF.Gelu_apprx_tanh), [PB[pb]], [gg])
            mk.op("pool", lambda e: e.tensor_tensor(out=hh[0][:], in0=hh[0][:], in1=hh[1][:], op=ALU.add), [hh[0], hh[1]], [hh[0]])
            y_ = yb[c % 2]
            mk.op("dve", lambda e: e.tensor_tensor(out=y_[:], in0=hh[0][:], in1=gg[:], op=ALU.mult), [hh[0], gg], [y_])
            mk.dma("sp", ylruT[c * 128:(c + 1) * 128, :], y_[:], y_, load=False)
        es = sub_end(es)
        wz = mk.tile(es, "wz", [128, 8, 1024], BF16)
        mk.dma("pool", wz[:], wv[:, :, C_Z:C_Z + 1024], wz)
        wdt = mk.tile(es, "wdt", [128, 8, 32], BF16)
        mk.dma("pool", wdt[:], wv[:, :, C_DT:C_DT + 32], wdt)
        dta = mk.tile(es, "dta", [128, NT, 32], F32)
        zts = [mk.tile(es, "zt", [128, D], BF16) for _ in range(2)]
        for t in range(NT):
            zt = zts[t % 2]
            for half in range(2):
                pb = next_pb()
                for k in range(8):
                    mk.op("pe", lambda e: e.matmul(bank(pb), lhsT=uT[:, k, t * 128:(t + 1) * 128], rhs=wz[:, k, half * 512:(half + 1) * 512],
                                                   start=(k == 0), stop=(k == 7)), [uT, wz], [PB[pb]])
                mk.op("act", lambda e: e.activation(out=zt[:, half * 512:(half + 1) * 512], in_=bank(pb), func=AF.Silu), [PB[pb]], [zt])
            mk.dma("act", zs[t * 128:(t + 1) * 128, :], zt[:], zt, load=False)
            pb = 4 + t % 2
            for k in range(8):
                mk.op("pe", lambda e: e.matmul(bank(pb, 32), lhsT=uT[:, k, t * 128:(t + 1) * 128], rhs=wdt[:, k, :], start=(k == 0), stop=(k == 7)),
                      [uT, wdt], [PB[pb]])
            mk.op("dve", lambda e: e.tensor_copy(out=dta[:, t, :], in_=bank(pb, 32)), [PB[pb]], [dta])
        mk.dma("sp", dtr.rearrange("(t p) c -> p t c", p=128), dta[:], dta, load=False)
        mk.end_stage(es)
        mk.end_stage(es_outer)

    def stage_mla(l, last):
        es = ExitStack()
        cqn = mk.tile(es, "cqn", [128, 4, T], BF16)
        ckvn = mk.tile(es, "ckvn", [128, 2, T], BF16)
        krb = mk.tile(es, "krb", [64, T], BF16)
        cs = mk.tile(es, "cs", [64, 2, T], F32)
        mk.dma("sp", cqn[:], cqT.rearrange("(c p) t -> p c t", p=128), cqn)
        mk.dma("sp", ckvn[:], ckvT.rearrange("(c p) t -> p c t", p=128), ckvn)
        mk.dma("sp", krb[:], krT, krb)
        mk.dma("sp", cs[:], cs_d.rearrange("c p t -> p c t"), cs)
        wqs = [mk.tile(es, "wq", [128, 4, 256], BF16) for _ in range(2)]
        wks = [mk.tile(es, "wk", [128, 2, 256], BF16) for _ in range(2)]
        qn = mk.tile(es, "qn", [128, T], BF16)
        qr = mk.tile(es, "qr", [64, T], BF16)
        kn = mk.tile(es, "kn", [128, T], BF16)
        vh = mk.tile(es, "vh", [128, NT, 128], BF16)
        kt1 = mk.tile(es, "kt1", [64, 512], F32)
        kt2 = mk.tile(es, "kt2", [64, 512], F32)
        Ps = [mk.tile(es, "P", [128, T], BF16) for _ in range(2)]
        PTs = [mk.tile(es, "PT", [128, NT, 128], BF16) for _ in range(2)]
        sm = [mk.tile(es, "sm", [128, 16], F32) for _ in range(2)]
        PB7b = mk.tracker("pb7b")
        PTg = [[mk.tracker("ptg%d_%d" % (i_, g_)) for g_ in range(3)] for i_ in range(2)]
        ob = [mk.tile(es, "ob", [128, 128], BF16) for _ in range(2)]
        aT = [mk.tile(es, "aT", [128, T], BF16) for _ in range(2)]
        wqv = w_uqx[l].rearrange("(k p) n -> p k n", p=128)
        wkv = w_ukv[l].rearrange("(k p) n -> p k n", p=128)
        t0 = 2 if last else 0
        it = 0
        for h in range(8):
            wq, wk = wqs[h % 2], wks[h % 2]
            mk.dma("pool", wq[:], wqv[:, :, h * 256:(h + 1) * 256], wq)
            mk.dma("pool", wk[:], wkv[:, :, h * 256:(h + 1) * 256], wk)
            for bi, (a, b) in enumerate(TB):
                n = b - a
                for k in range(4):
                    mk.op("pe", lambda e: e.matmul(bank(5, n), lhsT=wq[:, k, 0:128], rhs=cqn[:, k, a:b], start=(k == 0), stop=(k == 3)), [wq, cqn], [PB[5]])
                copy("act", qn[:, a:b], bank(5, n), [PB[5]], [qn])
                for k in range(4):
                    mk.op("pe", lambda e: e.matmul(PS[0:64, 6 * 512:6 * 512 + n], lhsT=wq[:, k, 128:192], rhs=cqn[:, k, a:b], start=(k == 0), stop=(k == 3)), [wq, cqn], [PB[6]])
                for k in range(4):
                    mk.op("pe", lambda e: e.matmul(PS[0:64, 7 * 512:7 * 512 + n], lhsT=wq[:, k, 192:256], rhs=cqn[:, k, a:b], start=(k == 0), stop=(k == 3)), [wq, cqn], [PB[7]])
                mk.op("dve", lambda e: e.tensor_tensor(out=kt1[:, 0:n], in0=PS[0:64, 6 * 512:6 * 512 + n], in1=cs[:, 0, a:b], op=ALU.mult), [PB[6], cs], [kt1])
                mk.op("dve", lambda e: e.tensor_tensor(out=kt2[:, 0:n], in0=PS[0:64, 7 * 512:7 * 512 + n], in1=cs[:, 1, a:b], op=ALU.mult), [PB[7], cs], [kt2])
                mk.op("pool", lambda e: e.tensor_tensor(out=qr[:, a:b], in0=kt1[:, 0:n], in1=kt2[:, 0:n], op=ALU.add), [kt1, kt2], [qr])
                for k in range(2):
                    mk.op("pe", lambda e: e.matmul(bank(5, n), lhsT=wk[:, k, 0:128], rhs=ckvn[:, k, a:b], start=(k == 0), stop=(k == 1)), [wk, ckvn], [PB[5]])
                copy("dve", kn[:, a:b], bank(5, n), [PB[5]], [kn])
            for g0 in range(0, NT, 4):
                g1 = min(NT, g0 + 4)
                pb = 6 + (g0 // 4) % 2
                for t in range(g0, g1):
                    for k in range(2):
                        mk.op("pe", lambda e: e.matmul(bank(pb, 128, (t - g0) * 128), lhsT=ckvn[:, k, t * 128:(t + 1) * 128], rhs=wk[:, k, 128:256],
                                                       start=(k == 0), stop=(k == 1)), [ckvn, wk], [PB[pb]])
                copy(evac_eng(), vh[:, g0:g1, :], bank(pb, (g1 - g0) * 128).rearrange("p (t c) -> p t c", c=128), [PB[pb]], [vh])
            a_ = aT[h % 2]
            units = list(range(t0, NT))

            def unit_bufs(i):
                return Ps[i % 2], PTs[i % 2], sm[i % 2], ob[i % 2]

            pending = []

            def emit_S(i):
                t = units[i]
                nk = 256 if t < 2 else T
                P_, PT_, s_, o_ = unit_bufs(i)
                nb = (nk + 511) // 512
                for kb, (a, b) in enumerate(TB):
                    if a >= nk:
                        break
                    b = min(b, nk)
                    mk.op("pe", lambda e: e.matmul(PS[:, a:b], lhsT=qn[:, t * 128:(t + 1) * 128], rhs=kn[:, a:b], start=True, stop=False), [qn, kn], [PB[kb]])
                    mk.op("pe", lambda e: e.matmul(PS[:, a:b], lhsT=qr[:, t * 128:(t + 1) * 128], rhs=krb[:, a:b], start=False, stop=True), [qr, krb], [PB[kb]])
                    mk.op("dve", lambda e: e.reduce_max(out=s_[:, kb:kb + 1], in_=PS[:, a:b], axis=AX), [PB[kb]], [s_])
                if nb > 1:
                    mk.op("dve", lambda e: e.reduce_max(out=s_[:, 5:6], in_=s_[:, 0:nb], axis=AX), [s_], [s_])
                    mx = s_[:, 5:6]
                else:
                    mx = s_[:, 0:1]
                mk.op("dve", lambda e: e.tensor_scalar_mul(out=s_[:, 6:7], in0=mx, scalar1=-ATTN_SCALE), [s_], [s_])
                mk.op("dve", lambda e: e.memset(s_[:, 8:13], 0.0), [], [s_])

            def emit_exp(i):
                t = units[i]
                nk = 256 if t < 2 else T
                P_, PT_, s_, o_ = unit_bufs(i)
                for kb, (a, b) in enumerate(TB):
                    if a >= nk:
                        break
                    b = min(b, nk)
                    mk.op("act", lambda e: e.activation(out=P_[:, a:b], in_=PS[:, a:b], func=AF.Exp, bias=s_[:, 6:7], scale=ATTN_SCALE,
                                                        accum_out=s_[:, 8 + kb:9 + kb]), [PB[kb], s_], [P_, s_])

            def emit_T(i):
                t = units[i]
                nk = 256 if t < 2 else T
                nkt = nk // 128
                P_, PT_, s_, o_ = unit_bufs(i)
                for g0 in range(0, nkt, 8):
                    g1 = min(nkt, g0 + 8)
                    pb = 5 + (g0 // 8) % 2
                    for kt in range(g0, g1):
                        mk.op("pe", lambda e: e.transpose(out=bankb(pb)[:, (kt - g0) * 128:(kt - g0 + 1) * 128], in_=P_[:, kt * 128:(kt + 1) * 128], identity=identb),
                              [P_, cstb], [PB[pb]])
                    copy("dve" if (g0 // 8) % 2 == 0 else "act", PT_[:, g0:g1, :], bankb(pb)[:, 0:(g1 - g0) * 128].rearrange("p (t c) -> p t c", c=128), [PB[pb]], [PTg[i % 2][g0 // 8]])

            def emit_OT():
                while pending:
                    t_, o2 = pending.pop(0)
                    mk.op("pe", lambda e: e.transpose(out=bankb(7)[:, 512:640], in_=o2[:], identity=identb), [o2, cstb], [PB7b])
                    copy("dve", a_[:, t_ * 128:(t_ + 1) * 128], bankb(7)[:, 512:640], [PB7b], [a_])

            def emit_PV(i):
                t = units[i]
                nk = 256 if t < 2 else T
                nkt = nk // 128
                P_, PT_, s_, o_ = unit_bufs(i)
                for kt in range(nkt):
                    mk.op("pe", lambda e: e.matmul(bank(7, 128), lhsT=PT_[:, kt, :], rhs=vh[:, kt, :], start=(kt == 0), stop=(kt == nkt - 1)), [PTg[i % 2][kt // 8], vh], [PB[7]])
                mk.op("dve", lambda e: e.reduce_sum(out=s_[:, 13:14], in_=s_[:, 8:13], axis=AX), [s_], [s_])
                mk.op("dve", lambda e: e.reciprocal(out=s_[:, 14:15], in_=s_[:, 13:14]), [s_], [s_])
                mk.op("act", lambda e: e.activation(out=o_[:], in_=bank(7, 128), func=AF.Identity, scale=s_[:, 14:15]), [PB[7], s_], [o_])
                pending.append((t, o_))

            emit_S(0)
            emit_exp(0)
            for i in range(len(units)):
                if i + 1 < len(units):
                    emit_S(i + 1)
                emit_OT()
                emit_T(i)
                if i + 1 < len(units):
                    emit_exp(i + 1)
                emit_PV(i)
            emit_OT()
            mk.dma("sp", attT[h * 128:(h + 1) * 128, t0 * 128:T], a_[:, t0 * 128:T], a_, load=False)
        mk.end_stage(es)

    def stage_ssd(l, last):
        es = ExitStack()
        t0 = 2 if last else 0
        dt = mk.tile(es, "dt", [128, NT, 32], F32)
        dta = mk.tile(es, "dtA", [128, NT, 32], F32)
        ndta = mk.tile(es, "ndta", [128, NT, 32], F32)
        tmpa = mk.tile(es, "tmpa", [128, NT, 32], F32)
        aneg = mk.tile(es, "aneg", [128, 32], F32)
        mk.dma("sp", dt[:], dtr.rearrange("(t p) c -> p t c", p=128), dt)
        bias_b = SROW(R_DTB, 32).unsqueeze(1).to_broadcast([128, NT, 32])
        mk.op("dve", lambda e: e.tensor_tensor(out=dt[:], in0=dt[:], in1=bias_b, op=ALU.add), [dt, srow], [dt])
        mk.op("act", lambda e: e.activation(out=tmpa[:], in_=dt[:], func=AF.Abs), [dt], [tmpa])
        mk.op("act", lambda e: e.activation(out=tmpa[:], in_=tmpa[:], func=AF.Exp, scale=-1.0), [tmpa], [tmpa])
        mk.op("act", lambda e: e.activation(out=tmpa[:], in_=tmpa[:], func=AF.Ln, bias=1.0, scale=1.0), [tmpa], [tmpa])
        mk.op("dve", lambda e: e.tensor_scalar_max(out=dt[:], in0=dt[:], scalar1=0.0), [dt], [dt])
        mk.op("dve", lambda e: e.tensor_tensor(out=dt[:], in0=dt[:], in1=tmpa[:], op=ALU.add), [dt, tmpa], [dt])
        mk.op("act", lambda e: e.activation(out=aneg[:], in_=SROW(R_ALOG, 32), func=AF.Exp), [srow], [aneg])
        mk.op("dve", lambda e: e.tensor_scalar_mul(out=aneg[:], in0=aneg[:], scalar1=-1.0), [aneg], [aneg])
        mk.op("dve", lambda e: e.tensor_tensor(out=dta[:], in0=dt[:], in1=aneg[:].unsqueeze(1).to_broadcast([128, NT, 32]), op=ALU.mult), [dt, aneg], [dta])
        mk.op("dve", lambda e: e.tensor_scalar_mul(out=ndta[:], in0=dta[:], scalar1=-1.0), [dta], [ndta])
        tri = [triU, triL]

        def small_stats(t, d, sst):
            mk.op("pe", lambda e: e.matmul(bank(7, 16), lhsT=tri[d], rhs=dta[:, t, d * 16:(d + 1) * 16], start=True, stop=True), [cst, dta], [PB[7]])
            mk.op("pe", lambda e: e.matmul(bank(7, 16, 16), lhsT=onesf, rhs=dta[:, t, d * 16:(d + 1) * 16], start=True, stop=True), [cst, dta], [PB[7]])
            copy("act", sst[:, 0:32], bank(7, 32), [PB[7]], [sst])

        ACUM = mk.tile(es, "ACUM", [128, NT, 32], F32)
        TOT = mk.tile(es, "TOT", [128, NT, 32], F32)
        WDE = mk.tile(es, "WDE", [128, NT, 32], F32)
        CD = mk.tile(es, "CD", [128, NT, 32], F32)
        EAC = mk.tile(es, "EAC", [128, NT, 32], F32)
        for half in range(2):
            for t in range(half * 9, half * 9 + 9):
                for d in range(2):
                    off = (t - half * 9) * 32 + d * 16
                    mk.op("pe", lambda e: e.matmul(bank(4 + half, 16, off), lhsT=tri[d], rhs=dta[:, t, d * 16:(d + 1) * 16], start=True, stop=True), [cst, dta], [PB[4 + half]])
                    mk.op("pe", lambda e: e.matmul(bank(6 + half, 16, off), lhsT=onesf, rhs=dta[:, t, d * 16:(d + 1) * 16], start=True, stop=True), [cst, dta], [PB[6 + half]])
            copy("act", ACUM[:, half * 9:half * 9 + 9, :], bank(4 + half, 288).rearrange("p (t c) -> p t c", c=32), [PB[4 + half]], [ACUM])
            copy("dve", TOT[:, half * 9:half * 9 + 9, :], bank(6 + half, 288).rearrange("p (t c) -> p t c", c=32), [PB[6 + half]], [TOT])
        mk.op("dve", lambda e: e.tensor_tensor(out=WDE[:], in0=TOT[:], in1=ACUM[:], op=ALU.subtract), [TOT, ACUM], [WDE])
        mk.op("act", lambda e: e.activation(out=WDE[:], in_=WDE[:], func=AF.Exp), [WDE], [WDE])
        mk.op("dve", lambda e: e.tensor_tensor(out=WDE[:], in0=WDE[:], in1=dt[:], op=ALU.mult), [WDE, dt], [WDE])
        mk.op("act", lambda e: e.activation(out=CD[:], in_=TOT[:], func=AF.Exp), [TOT], [CD])
        mk.op("act", lambda e: e.activation(out=EAC[:], in_=ACUM[:], func=AF.Exp), [ACUM], [EAC])
        xss = [mk.tile(es, "xs", [128, D], BF16) for _ in range(2)]
        bts = [mk.tile(es, "bt", [128, 512], BF16) for _ in range(2)]
        ssts = [mk.tile(es, "sst", [128, 64], F32) for _ in range(2)]
        xgd = [mk.tile(es, "xgd", [128, D], BF16) for _ in range(2)]
        hbf = [mk.tile(es, "hbf", [128, D], BF16) for _ in range(2)]
        St = mk.tile(es, "St", [128, D], F32)
        n1 = 0
        for d in range(2):
            order = list(range(NT)) if d == 0 else [1, 0] + list(range(NT - 1, 1, -1))
            mk.op("pool", lambda e: e.memset(St[:], 0.0), [], [St])
            for t in order:
                xs_, bt_, sst, xg_, hb_ = xss[n1 % 2], bts[n1 % 2], ssts[n1 % 2], xgd[n1 % 2], hbf[n1 % 2]
                n1 += 1
                mk.dma("sp", xs_[:], xs_tm[t * 128:(t + 1) * 128, :], xs_)
                mk.dma("sp", bt_[:], b_tm[t * 128:(t + 1) * 128, :], bt_)
                copy("act", hb_[:], St[:], [St], [hb_])
                mk.dma("act", hprev[d, t], hb_[:], hb_, load=False)
                mk.op("dve", lambda e: e.tensor_tensor(out=xg_[:].rearrange("p (h j) -> p h j", j=64), in0=xs_[:].rearrange("p (h j) -> p h j", j=64),
                                                       in1=WDE[:, t, d * 16:(d + 1) * 16].unsqueeze(2).to_broadcast([128, 16, 64]), op=ALU.mult), [xs_, WDE], [xg_])
                for g in range(4):
                    pb = g // 2
                    mk.op("pe", lambda e: e.matmul(bank(pb, 256, (g % 2) * 256), lhsT=bt_[:, g * 128:(g + 1) * 128], rhs=xg_[:, g * 256:(g + 1) * 256],
                                                   start=True, stop=True), [bt_, xg_], [PB[pb]])
                mk.op("pool", lambda e: e.tensor_tensor(out=St[:].rearrange("p (h j) -> p h j", j=64), in0=St[:].rearrange("p (h j) -> p h j", j=64),
                                                        in1=CD[:, t, d * 16:(d + 1) * 16].unsqueeze(2).to_broadcast([128, 16, 64]), op=ALU.mult), [St, CD], [St])
                mk.op("dve", lambda e: e.tensor_tensor(out=St[:], in0=St[:], in1=PS[:, 0:1024], op=ALU.add), [St, PB[0], PB[1]], [St])
        mk.barrier()
        cts = [mk.tile(es, "ct", [128, 8, 128], BF16) for _ in range(2)]
        hps = [mk.tile(es, "hp", [128, 2, D], BF16) for _ in range(2)]
        zts = [mk.tile(es, "zt", [128, D], BF16) for _ in range(2)]
        cbm_l = [mk.tile(es, "cbm", [128, 2, 512], BF16) for _ in range(2)]
        rhs1_l = [mk.tile(es, "rhs1", [128, 16, 128], F32) for _ in range(2)]
        rhs2_l = [mk.tile(es, "rhs2", [128, 16, 128], F32) for _ in range(2)]
        Lm_l = [mk.tile(es, "Lm", [128, 8, 128], F32) for _ in range(2)]
        MT_l = [mk.tile(es, "MT", [128, 16, 128], BF16) for _ in range(2)]
        xg_l = [mk.tile(es, "xg", [128, D], BF16) for _ in range(2)]
        yo_l = [mk.tile(es, "yo", [128, D], F32) for _ in range(2)]
        yt_l = [mk.tile(es, "yt", [128, D], F32) for _ in range(2)]
        ynb_l = [mk.tile(es, "ynb", [128, D], BF16) for _ in range(2)]
        ys = [mk.tile(es, "ys", [128, 8, 128], BF16) for _ in range(2)]
        sq_l = [mk.tile(es, "sq", [128, 256], F32) for _ in range(2)]
        g4_l = [mk.tile(es, "g4", [128, 8], F32) for _ in range(2)]
        bcv = bcT.rearrange("(c p) t -> p c t", p=128)
        for i, t in enumerate(range(t0, NT)):
            xs_, ct, hp, zt, sst, y_s = xss[i % 2], cts[i % 2], hps[i % 2], zts[i % 2], ssts[i % 2], ys[i % 2]
            cbm, yo, yt, ynb, sq_, g4 = cbm_l[i % 2], yo_l[i % 2], yt_l[i % 2], ynb_l[i % 2], sq_l[i % 2], g4_l[i % 2]
            mk.dma("sp", xs_[:], xs_tm[t * 128:(t + 1) * 128, :], xs_)
            mk.dma("sp", ct[:], bcv[:, :, t * 128:(t + 1) * 128], ct)
            mk.dma("sp", hp[:], hprev[:, t].rearrange("d p n -> p d n"), hp)
            mk.dma("sp", zt[:], zs[t * 128:(t + 1) * 128, :], zt)
            for g in range(4):
                mk.op("pe", lambda e: e.matmul(bank(0, 128, g * 128), lhsT=ct[:, g, :], rhs=ct[:, 4 + g, :], start=True, stop=True), [ct], [PB[0]])
            for d in range(2):
                mk.op("dve", lambda e: e.tensor_tensor(out=cbm[:, d, :].rearrange("p (g q) -> p g q", q=128), in0=bank(0).rearrange("p (g q) -> p g q", q=128),
                                                       in1=tri[d].unsqueeze(1).to_broadcast([128, 4, 128]), op=ALU.mult), [PB[0], cst], [cbm])
            for d in range(2):
                rhs1, rhs2, MT, xg = rhs1_l[d], rhs2_l[d], MT_l[d], xg_l[d]
                dsl = dta[:, t, d * 16:(d + 1) * 16]
                mk.op("dve", lambda e: e.tensor_tensor(out=rhs1[:], in0=tri[d].unsqueeze(1).to_broadcast([128, 16, 128]),
                                                       in1=dsl.unsqueeze(2).to_broadcast([128, 16, 128]), op=ALU.mult), [cst, dta], [rhs1])
                mk.op("pool", lambda e: e.tensor_copy(out=rhs2[:], in_=ndta[:, t, d * 16:(d + 1) * 16].unsqueeze(2).to_broadcast([128, 16, 128])), [ndta], [rhs2])
                mk.op("dve", lambda e: e.tensor_tensor(out=xg[:].rearrange("p (h j) -> p h j", j=64), in0=xs_[:].rearrange("p (h j) -> p h j", j=64),
                                                       in1=dt[:, t, d * 16:(d + 1) * 16].unsqueeze(2).to_broadcast([128, 16, 64]), op=ALU.mult), [xs_, dt], [xg])
                for hf in range(2):
                    Lm = Lm_l[hf]
                    for q4 in range(2):
                        pb = 1 + q4
                        hs = hf * 8 + q4 * 4
                        mk.op("pe", lambda e: e.matmul(bank(pb), lhsT=onesf, rhs=rhs1[:, hs:hs + 4, :], start=True, stop=False), [cst, rhs1], [PB[pb]])
                        mk.op("pe", lambda e: e.matmul(bank(pb), lhsT=tri[d], rhs=rhs2[:, hs:hs + 4, :], start=False, stop=True), [cst, rhs2], [PB[pb]])
                    mk.op("act", lambda e: e.activation(out=Lm[:].rearrange("p h q -> p (h q)"), in_=PS[:, 512:1536], func=AF.Exp), [PB[1], PB[2]], [Lm])
                    for g2 in range(2):
                        gg_ = hf * 2 + g2
                        mk.op("dve", lambda e: e.scalar_tensor_tensor(out=MT[:, gg_ * 4:(gg_ + 1) * 4, :], in0=Lm[:, g2 * 4:(g2 + 1) * 4, :], scalar=1.0,
                                                                      in1=cbm[:, d, gg_ * 128:(gg_ + 1) * 128].unsqueeze(1).to_broadcast([128, 4, 128]),
                                                                      op0=ALU.min, op1=ALU.mult), [Lm, cbm], [MT])
                for h in range(16):
                    pb = 3 + h // 8
                    mk.op("pe", lambda e: e.matmul(bank(pb, 64, (h % 8) * 64), lhsT=MT[:, h, :], rhs=xg[:, h * 64:(h + 1) * 64], start=(d == 0 and h % 8 == 0), stop=(d == 1), skip_group_check=True),
                          [MT, xg], [PB[pb]])
                for g in range(4):
                    pb = 5 + g // 2
                    mk.op("pe", lambda e: e.matmul(bank(pb, 256, (g % 2) * 256), lhsT=ct[:, 4 + g, :], rhs=hp[:, d, g * 256:(g + 1) * 256], start=True, stop=True),
                          [ct, hp], [PB[pb]])
                if d == 0:
                    mk.op("dve", lambda e: e.tensor_tensor(out=yo[:].rearrange("p (h j) -> p h j", j=64), in0=PS[:, 2560:3584].rearrange("p (h j) -> p h j", j=64),
                                                           in1=EAC[:, t, d * 16:(d + 1) * 16].unsqueeze(2).to_broadcast([128, 16, 64]), op=ALU.mult), [PB[5], PB[6], EAC], [yo])
                else:
                    mk.op("dve", lambda e: e.tensor_tensor(out=yt[:].rearrange("p (h j) -> p h j", j=64), in0=PS[:, 2560:3584].rearrange("p (h j) -> p h j", j=64),
                                                           in1=EAC[:, t, d * 16:(d + 1) * 16].unsqueeze(2).to_broadcast([128, 16, 64]), op=ALU.mult), [PB[5], PB[6], EAC], [yt])
            mk.op("pool", lambda e: e.tensor_tensor(out=yo[:], in0=yo[:], in1=yt[:], op=ALU.add), [yo, yt], [yo])
            mk.op("dve", lambda e: e.tensor_tensor(out=yt[:], in0=PS[:, 1536:2560], in1=yo[:], op=ALU.add), [PB[3], PB[4], yo], [yt])
            mk.op("pool", lambda e: e.tensor_tensor(out=yo[:].rearrange("p (h j) -> p h j", j=64), in0=xs_[:].rearrange("p (h j) -> p h j", j=64),
                                                    in1=SROW(R_DSK, 16).unsqueeze(2).to_broadcast([128, 16, 64]), op=ALU.mult), [xs_, srow], [yo])
            mk.op("dve", lambda e: e.tensor_tensor(out=yt[:], in0=yt[:], in1=yo[:], op=ALU.add), [yt, yo], [yt])
            mk.op("dve", lambda e: e.tensor_tensor(out=yt[:], in0=yt[:], in1=zt[:], op=ALU.mult), [yt, zt], [yt])
            mk.op("pool", lambda e: e.memset(g4[:], 0.0), [], [g4])
            for g in range(4):
                mk.op("act", lambda e: e.activation(out=sq_[:], in_=yt[:, g * 256:(g + 1) * 256], func=AF.Square, accum_out=g4[:, g:g + 1]), [yt], [sq_, g4])
            mk.op("act", lambda e: e.activation(out=g4[:, 4:8], in_=g4[:, 0:4], func=AF.Sqrt, bias=RMS_EPS, scale=1.0 / 256.0), [g4], [g4])
            mk.op("dve", lambda e: e.reciprocal(out=g4[:, 4:8], in_=g4[:, 4:8]), [g4], [g4])
            mk.op("dve", lambda e: e.tensor_tensor(out=ynb[:].rearrange("p (g j) -> p g j", j=256), in0=yt[:].rearrange("p (g j) -> p g j", j=256),
                                                   in1=g4[:, 4:8].unsqueeze(2).to_broadcast([128, 4, 256]), op=ALU.mult), [yt, g4], [ynb])
            for c in range(8):
                mk.op("pe", lambda e: e.transpose(out=bankb(7)[:, c * 128:(c + 1) * 128], in_=ynb[:, c * 128:(c + 1) * 128], identity=identb), [ynb, cstb], [PB[7]])
            for c in range(8):
                mk.op("act", lambda e: e.activation(out=y_s[:, c, :], in_=bankb(7)[:, c * 128:(c + 1) * 128], func=AF.Identity,
                                                    scale=vec[:, V_SNW + c:V_SNW + c + 1]), [PB[7], vec], [y_s])
            mk.dma("act", yssdT.rearrange("(c p) t -> p c t", p=128)[:, :, t * 128:(t + 1) * 128], y_s[:], y_s, load=False)
        mk.end_stage(es)

    def stage_merge(l, last, src, dst_fn):
        es = ExitStack()
        t0 = 2 if last else 0
        c0 = t0 * 128
        mT = mk.tile(es, "mT", [128, 8, T], BF16)
        brs = [mk.tile(es, "br", [128, 8, T], BF16) for _ in range(2)]
        wbs = [mk.tile(es, "wb", [128, 8, 512], BF16) for _ in range(2)]
        gts = [mk.tile(es, "gt", [128, T], BF16) for _ in range(2)]
        tmp = [mk.tile(es, "tmp", [128, 512], BF16) for _ in range(2)]
        srcs = [attT, yssdT, ylruT]
        n = 0
        for br in range(3):
            b_ = brs[br % 2]
            mk.dma("sp", b_[:, :, c0:T], srcs[br].rearrange("(c p) t -> p c t", p=128)[:, :, c0:T], b_)
            wv = w_branch[l, br].rearrange("(k p) n -> p k n", p=128)
            for dc in range(8):
                if dc % 4 == 0:
                    wb = wbs[n % 2]
                    n += 1
                    mk.dma("pool", wb[:], wv[:, :, dc * 128:dc * 128 + 512], wb)
                gt = gts[dc % 2]
                mk.dma("sp", gt[:, c0:T], gateT[(br * 8 + dc) * 128:(br * 8 + dc + 1) * 128, c0:T], gt)
                for bi, (a, b) in enumerate(TB):
                    a = max(a, c0)
                    if a >= b:
                        continue
                    rr["mm"] += 1
                    pb = rr["mm"] % 4
                    for k in range(8):
                        mk.op("pe", lambda e: e.matmul(bank(pb, b - a), lhsT=wb[:, k, (dc % 4) * 128:(dc % 4 + 1) * 128], rhs=b_[:, k, a:b],
                                                       start=(k == 0), stop=(k == 7)), [wb, b_], [PB[pb]])
                    if br == 0:
                        mk.op("dve", lambda e: e.tensor_tensor(out=mT[:, dc, a:b], in0=bank(pb, b - a), in1=gt[:, a:b], op=ALU.mult), [PB[pb], gt], [mT])
                    else:
                        tp = tmp[bi % 2]
                        mk.op("dve", lambda e: e.tensor_tensor(out=tp[:, 0:b - a], in0=bank(pb, b - a), in1=gt[:, a:b], op=ALU.mult), [PB[pb], gt], [tp])
                        mk.op("pool", lambda e: e.tensor_tensor(out=mT[:, dc, a:b], in0=mT[:, dc, a:b], in1=tp[:, 0:b - a], op=ALU.add), [mT, tp], [mT])
        wo = mk.tile(es, "wo", [128, 8, D], BF16)
        mk.dma("pool", wo[:], w_out[l].rearrange("(k p) n -> p k n", p=128), wo)
        grow = build_grow(es, 2)
        lnrow = mk.tile(es, "lnrow", [128, 2, D], F32)
        mk.dma("sp", lnrow[:], rows_d[l, R_LN1G:R_LN1G + 2048].rearrange("(a n) -> a n", a=2).partition_broadcast(128), lnrow)
        hts = [mk.tile(es, "ht", [128, D], F32) for _ in range(3)]
        vts = [mk.tile(es, "vt", [128, D], F32) for _ in range(3)]
        sts = [mk.tile(es, "st", [128, 12], F32) for _ in range(3)]
        mvs = [mk.tile(es, "mv", [128, 4], F32) for _ in range(3)]
        for i, t in enumerate(range(t0, NT)):
            ht = hts[i % 3]
            mk.dma("sp", ht[:], src[t * 128:(t + 1) * 128, :], ht)
            pbs = (4, 5) if i % 2 == 0 else (6, 7)
            for half in range(2):
                pb = pbs[half]
                for dc in range(8):
                    mk.op("pe", lambda e: e.matmul(bank(pb), lhsT=mT[:, dc, t * 128:(t + 1) * 128], rhs=wo[:, dc, half * 512:(half + 1) * 512],
                                                   start=(dc == 0), stop=(dc == 7)), [mT, wo], [PB[pb]])
            res_ln(t, PS[:, pbs[0] * 512:pbs[0] * 512 + 1024], [PB[pbs[0]], PB[pbs[1]]], src, grow, lnrow, dst_fn, (ht, vts[i % 3], sts[i % 3], mvs[i % 3]))
        mk.end_stage(es)

    def stage_moe(l, last, src, dst_fn):
        es = ExitStack()
        t0 = 2 if last else 0
        c0 = t0 * 128
        uT = mk.tile(es, "u2T", [128, 8, T], BF16)
        gw = mk.tile(es, "gw", [128, NT, 32], F32)
        wrt = mk.tile(es, "wrt", [128, 8, 36], F32)
        mk.dma("sp", wrt[:], wr[l].rearrange("(k p) n -> p k n", p=128), wrt)
        es1 = ExitStack()
        stage_ln_mod(es1, src, 3, 4, uT, t0, router=(wrt, gw))
        mk.end_stage(es1)
        mk.stage_tiles.extend([uT, gw, wrt])
        es2 = ExitStack()
        acc = mk.tile(es, "acc", [128, NT, D], F32)
        w1s = [mk.tile(es2, "w1", [128, 8, 512], BF16) for _ in range(2)]
        w3s = [mk.tile(es2, "w3", [128, 8, 512], BF16) for _ in range(2)]
        w2s = [mk.tile(es2, "w2", [128, 4, D], BF16) for _ in range(2)]
        hid = [mk.tile(es2, "hid", [128, 4, 512], BF16) for _ in range(2)]
        sas = [mk.tile(es2, "sa", [128, 512], BF16) for _ in range(2)]
        nb = 0
        for ex in range(32):
            w1, w3, w2 = w1s[ex % 2], w3s[ex % 2], w2s[ex % 2]
            mk.dma("pool", w1[:], exp_w1[l, ex].rearrange("(k p) n -> p k n", p=128), w1)
            mk.dma("pool", w3[:], exp_w3[l, ex].rearrange("(k p) n -> p k n", p=128), w3)
            mk.dma("pool", w2[:], exp_w2[l, ex].rearrange("(k p) n -> p k n", p=128), w2)
            for bi, (a, b) in enumerate(TB):
                a = max(a, c0)
                if a >= b:
                    continue
                n = b - a
                hd_ = hid[nb % 2]
                nb += 1
                for hc in range(4):
                    pa, pb = (0, 1) if hc % 2 == 0 else (2, 3)
                    for k in range(8):
                        mk.op("pe", lambda e: e.matmul(bank(pa, n), lhsT=w1[:, k, hc * 128:(hc + 1) * 128], rhs=uT[:, k, a:b], start=(k == 0), stop=(k == 7)),
                              [w1, uT], [PB[pa]])
                    for k in range(8):
                        mk.op("pe", lambda e: e.matmul(bank(pb, n), lhsT=w3[:, k, hc * 128:(hc + 1) * 128], rhs=uT[:, k, a:b], start=(k == 0), stop=(k == 7)),
                              [w3, uT], [PB[pb]])
                    sa = sas[hc % 2]
                    mk.op("act", lambda e: e.activation(out=sa[:, 0:n], in_=bank(pa, n), func=AF.Silu), [PB[pa]], [sa])
                    mk.op("dve", lambda e: e.tensor_tensor(out=hd_[:, hc, 0:n], in0=bank(pb, n), in1=sa[:, 0:n], op=ALU.mult), [PB[pb], sa], [hd_])
                for ti, t in enumerate(range(a // 128, b // 128)):
                    pbs = (4, 5) if ti % 2 == 0 else (6, 7)
                    for half in range(2):
                        for hc in range(4):
                            mk.op("pe", lambda e: e.matmul(bank(pbs[half]), lhsT=hd_[:, hc, (t * 128 - a):(t * 128 - a) + 128], rhs=w2[:, hc, half * 512:(half + 1) * 512],
                                                           start=(hc == 0), stop=(hc == 3)), [hd_, w2], [PB[pbs[half]]])
                    yps = PS[:, pbs[0] * 512:pbs[0] * 512 + 1024]
                    if ex == 0:
                        mk.op("dve", lambda e: e.tensor_scalar_mul(out=acc[:, t, :], in0=yps, scalar1=gw[:, t, ex:ex + 1]), [PB[pbs[0]], PB[pbs[1]], gw], [acc])
                    else:
                        mk.op("dve", lambda e: e.scalar_tensor_tensor(out=acc[:, t, :], in0=yps, scalar=gw[:, t, ex:ex + 1], in1=acc[:, t, :],
                                                                      op0=ALU.mult, op1=ALU.add), [PB[pbs[0]], PB[pbs[1]], gw, acc], [acc])
        mk.end_stage(es2)
        mk.stage_tiles.extend([uT, gw, wrt, acc])
        grow = build_grow(es, 5)
        lnrow = mk.tile(es, "lnrow", [128, 2, D], F32)
        mk.dma("sp", lnrow[:], rows_d[l, R_LN2G:R_LN2G + 2048].rearrange("(a n) -> a n", a=2).partition_broadcast(128), lnrow)
        hts = [mk.tile(es, "ht", [128, D], F32) for _ in range(4)]
        vts = [mk.tile(es, "vt", [128, D], F32) for _ in range(4)]
        sts = [mk.tile(es, "st", [128, 12], F32) for _ in range(4)]
        mvs = [mk.tile(es, "mv", [128, 4], F32) for _ in range(4)]
        for i, t in enumerate(range(t0, NT)):
            ht = hts[i % 4]
            mk.dma("sp", ht[:], src[t * 128:(t + 1) * 128, :], ht)
            res_ln(t, acc[:, t, :], [acc], src, grow, lnrow, dst_fn, (ht, vts[i % 4], sts[i % 4], mvs[i % 4]))
        mk.end_stage(es)

    def hd_tile(t):
        return hd[t * 128:(t + 1) * 128, :]

    def y_tile(t):
        return y_out[(t - 2) * 128:(t - 1) * 128, :]

    for l in range(n_layers):
        last = (l == n_layers - 1)
        src = h0 if l == 0 else hd
        stage_mod(l)
        stage_inproj(l, src)
        if stop_after == "inproj":
            break
        stage_mla(l, last)
        if stop_after == "mla":
            break
        stage_ssd(l, last)
        if stop_after == "ssd":
            break
        stage_merge(l, last, src, hd_tile)
        if stop_after == "merge":
            break
        stage_moe(l, last, hd, y_tile if last else hd_tile)
    mk.barrier()
    print("built: inst", mk.n_inst, "waits", mk.n_wait, "cnt", mk.cnt)
    return nc


def _prep_shared(inp):
    f = np.float32
    Lh = inp["w_in"].shape[0]
    sh = {}
    sh["w_mod"] = np.ascontiguousarray(inp["w_mod"], f)
    sh["w_in"] = np.ascontiguousarray(inp["w_in"], f)
    kr = inp["w_in"][:, :, C_KR:C_KR + 64].reshape(Lh, D, 2, 2, 16)
    sh["w_krsw"] = np.ascontiguousarray(kr[:, :, :, ::-1, :].reshape(Lh, D, 64), f)
    uq = inp["w_uq"].reshape(Lh, 512, 8, 192)
    qr = uq[..., 128:].reshape(Lh, 512, 8, 2, 2, 16)
    qsw = qr[:, :, :, :, ::-1, :].reshape(Lh, 512, 8, 64)
    sh["w_uqx"] = np.ascontiguousarray(np.concatenate([uq, qsw], axis=-1).reshape(Lh, 512, 2048), f)
    sh["w_ukv"] = np.ascontiguousarray(inp["w_ukv"], f)
    bd = np.zeros((Lh, 2, 2, 8, 128, 128), f)
    for gi, key in enumerate(("lru_wa", "lru_wx")):
        w = inp[key]
        for c in range(8):
            bd[:, :, gi, c, 0:64, 0:64] = w[:, :, 2 * c]
            bd[:, :, gi, c, 64:128, 64:128] = w[:, :, 2 * c + 1]
    sh["lru_bd"] = bd.reshape(Lh, 32, 128, 128)
    sh["w_branch"] = np.ascontiguousarray(inp["w_branch"], f)
    sh["w_out"] = np.ascontiguousarray(inp["w_out"], f)
    sh["wr"] = np.ascontiguousarray(np.concatenate([inp["router_wg"], inp["router_we"]], axis=-1), f)
    sh["exp_w1"] = np.ascontiguousarray(inp["exp_w1"], f)
    sh["exp_w3"] = np.ascontiguousarray(inp["exp_w3"], f)
    sh["exp_w2"] = np.ascontiguousarray(inp["exp_w2"], f)
    vecs = np.zeros((Lh, 128, NV), f)

    def colfmt(v, nchunk):
        return v.reshape(v.shape[:-1] + (nchunk, 128))

    for l in range(Lh):
        bm = inp["b_mod"][l].reshape(48, 128).T
        vecs[l, :, V_BMOD:V_BMOD + 96] = np.repeat(bm, 2, axis=1)
        vecs[l, :, V_QNW:V_QNW + 4] = inp["q_norm_w"][l].reshape(4, 128).T
        vecs[l, :, V_KVNW:V_KVNW + 2] = inp["kv_norm_w"][l].reshape(2, 128).T
        vecs[l, :, V_SCW:V_SCW + 64] = inp["ssd_conv_w"][l].reshape(4, 16, 128).transpose(2, 1, 0).reshape(128, 64)
        vecs[l, :, V_SCB:V_SCB + 16] = inp["ssd_conv_b"][l].reshape(16, 128).T
        vecs[l, :, V_LCW:V_LCW + 32] = inp["lru_conv_w"][l].reshape(4, 8, 128).transpose(2, 1, 0).reshape(128, 32)
        vecs[l, :, V_LCB:V_LCB + 8] = inp["lru_conv_b"][l].reshape(8, 128).T
        vecs[l, :, V_LBA:V_LBA + 16] = inp["lru_ba"][l].reshape(2, 8, 128).transpose(2, 0, 1).reshape(128, 16)
        vecs[l, :, V_LBX:V_LBX + 16] = inp["lru_bx"][l].reshape(2, 8, 128).transpose(2, 0, 1).reshape(128, 16)
        vecs[l, :, V_LLAM:V_LLAM + 16] = inp["lru_lambda"][l].reshape(2, 8, 128).transpose(2, 0, 1).reshape(128, 16)
        vecs[l, :, V_SNW:V_SNW + 8] = inp["ssd_norm_w"][l].reshape(8, 128).T
    sh["vecs"] = vecs
    rows = np.zeros((Lh, NR), f)
    rows[:, R_LN1G:R_LN1G + 1024] = inp["ln1_g"]
    rows[:, R_LN1B:R_LN1B + 1024] = inp["ln1_b"]
    rows[:, R_LN2G:R_LN2G + 1024] = inp["ln2_g"]
    rows[:, R_LN2B:R_LN2B + 1024] = inp["ln2_b"]
    rows[:, R_ALOG:R_ALOG + 32] = inp["ssd_a_log"].reshape(Lh, 32)
    rows[:, R_DTB:R_DTB + 32] = inp["ssd_dt_bias"].reshape(Lh, 32)
    rows[:, R_DSK:R_DSK + 16] = inp["ssd_d"]
    rows[:, R_RB:R_RB + 4] = inp["router_bg"]
    rows[:, R_RB + 4:R_RB + 36] = inp["router_be"]
    sh["rows"] = rows
    idx = np.arange(128)
    consts = np.zeros((4, 128, 128), f)
    consts[0] = np.eye(128)
    consts[1] = (idx[:, None] <= idx[None, :])
    consts[2] = (idx[:, None] >= idx[None, :])
    consts[3] = 1.0
    sh["consts"] = consts
    pos = np.arange(2048)
    rowp = (pos // 64).astype(f)
    colp = (pos % 64).astype(f)
    inv = (10000.0 ** (-np.arange(16, dtype=f) / 16)).astype(f)
    ang = np.stack([rowp[:, None] * inv, colp[:, None] * inv], axis=1).astype(f)
    cos = np.cos(ang).astype(f)
    sin = np.sin(ang).astype(f)
    ct = np.ones((64, T), f)
    st = np.zeros((64, T), f)
    for a in range(2):
        for j in range(2):
            r0 = a * 32 + j * 16
            ct[r0:r0 + 16, 256:] = cos[:, a, :].T
            st[r0:r0 + 16, 256:] = (-1.0 if j == 0 else 1.0) * sin[:, a, :].T
    sh["cossin"] = np.stack([ct, st]).astype(f)
    return sh


def _prep_core(inp, b):
    f = np.float32
    m = {}
    m["h0"] = np.ascontiguousarray(np.concatenate([inp["ctx"][b], inp["x"][b]], axis=0), f)
    cv = np.zeros((128, 16), f)
    cv[:, 0::2] = inp["c"][b].reshape(8, 128).T
    cv[:, 1::2] = inp["c_ctx"].reshape(8, 128).T
    m["cvec"] = cv
    return m


_NC_CACHE = {}


def kernel(**inputs):
    inp = {k: np.asarray(v) for k, v in inputs.items()}
    if "nc" not in _NC_CACHE:
        _NC_CACHE["nc"] = build()
    nc = _NC_CACHE["nc"]
    sh = _prep_shared(inp)
    in_maps = []
    for b in range(8):
        m = dict(sh)
        m.update(_prep_core(inp, b))
        in_maps.append(m)
    res = run_bass_kernel_spmd(nc, in_maps, core_ids=list(range(8)))
    out = np.stack([np.asarray(r["y"], dtype=np.float32) for r in res.results], axis=0)
    return out
```
